# Optimizing a Trainium2 kernel written in Bass

```python
import math
import jax
import jax.numpy as jnp
from jax import lax
import numpy as np

D_MODEL = 1024
BATCH = 16
SEQ = 2048
DEPTH = 2

DIFF_HEADS = 4
DIFF_QK_DIM = 64
DIFF_V_DIM = 128
GLA_HEADS = 4
GLA_DK = 64
GLA_DV = 128
GLA_GATE_RANK = 16
GLA_GATE_NORM = 16.0
HGRN_HEADS = 4
HGRN_EXPAND = 128
HGRN_DV = 128
N_BRANCHES = 3
BRANCH_WIDTH = 512
D_FF = 4 * D_MODEL
CHUNK = 64
Q_BLOCK = 128
ALPHA = (2 * DEPTH) ** 0.25
BETA = (8 * DEPTH) ** -0.25
LN_EPS = 1e-5
MASK_VALUE = -1e30
LB_FLOOR = 1e-30
SPLIT_SIZES = (
    DIFF_HEADS * 2 * DIFF_QK_DIM,
    DIFF_HEADS * 2 * DIFF_QK_DIM,
    DIFF_HEADS * DIFF_V_DIM,
    GLA_HEADS * GLA_DK,
    GLA_HEADS * GLA_DK,
    GLA_HEADS * GLA_DV,
    GLA_GATE_RANK,
    GLA_HEADS * GLA_DV,
    HGRN_HEADS * HGRN_EXPAND,
    HGRN_HEADS * HGRN_EXPAND,
    HGRN_HEADS * HGRN_DV,
    HGRN_HEADS * HGRN_DV,
    N_BRANCHES * D_MODEL,
)
IN_WIDTH = sum(SPLIT_SIZES)

kernel_name = 'hybrid_diffattn_gla_hgrn2_deepnorm'


def layer_norm(x, w, b):
    xf = x.astype(jnp.float32)
    mu = jnp.mean(xf, axis=-1, keepdims=True)
    var = jnp.mean(jnp.square(xf - mu), axis=-1, keepdims=True)
    y = (xf - mu) * lax.rsqrt(var + LN_EPS) * w.astype(jnp.float32) + b.astype(jnp.float32)
    return y.astype(x.dtype)


def rms_norm(x, w):
    xf = x.astype(jnp.float32)
    y = xf * lax.rsqrt(jnp.mean(jnp.square(xf), axis=-1, keepdims=True) + LN_EPS)
    return y * w.astype(jnp.float32)


def split_columns(z):
    points = np.cumsum(np.array(SPLIT_SIZES))[:-1]
    return jnp.split(z, points, axis=-1)


def to_heads(a, n_heads):
    b, t, _ = a.shape
    return a.reshape(b, t, n_heads, -1).transpose(0, 2, 1, 3)


def from_heads(a):
    b, h, t, d = a.shape
    return a.transpose(0, 2, 1, 3).reshape(b, t, h * d)


def diff_attention(q, k, v, lam):
    h = q.shape[1]
    t = q.shape[3]
    d = q.shape[4]
    slopes = jnp.exp2(-8.0 * jnp.arange(1, h + 1, dtype=jnp.float32) / h)
    scale = d ** -0.5
    vf = v.astype(jnp.float32)
    outs = []
    for blk in range(t // Q_BLOCK):
        start = blk * Q_BLOCK
        end = start + Q_BLOCK
        qb = q[:, :, :, start:end].astype(jnp.float32)
        kb = k[:, :, :, :end].astype(jnp.float32)
        s = jnp.einsum('bhmqd,bhmkd->bhmqk', qb, kb) * scale
        dist = (jnp.arange(start, end)[:, None] - jnp.arange(end)[None, :]).astype(jnp.float32)
        s = s - slopes[:, None, None, None] * dist
        s = jnp.where(dist >= 0, s, MASK_VALUE)
        p = jax.nn.softmax(s, axis=-1)
        a = p[:, :, 0] - lam * p[:, :, 1]
        outs.append(jnp.einsum('bhqk,bhkd->bhqd', a, vf[:, :, :end]))
    return jnp.concatenate(outs, axis=2)


def chunked_gated_linear_attention(q, k, v, log_g):
    b, h, t, dk = q.shape
    dv = v.shape[-1]
    nc = t // CHUNK

    def to_chunks(a):
        return a.astype(jnp.float32).reshape(b, h, nc, CHUNK, a.shape[-1]).transpose(2, 0, 1, 3, 4)

    qc, kc, vc, gc = to_chunks(q), to_chunks(k), to_chunks(v), to_chunks(log_g)
    causal = jnp.tril(jnp.ones((CHUNK, CHUNK), dtype=bool))[:, :, None]

    def step(state, inp):
        qi, ki, vi, gi = inp
        cum = jnp.cumsum(gi, axis=-2)
        diff = cum[..., :, None, :] - cum[..., None, :, :]
        decay = jnp.where(causal, jnp.exp(jnp.where(causal, diff, 0.0)), 0.0)
        scores = jnp.einsum('bhid,bhjd,bhijd->bhij', qi, ki, decay)
        o = jnp.einsum('bhij,bhje->bhie', scores, vi)
        o = o + jnp.einsum('bhid,bhde->bhie', qi * jnp.exp(cum), state)
        last = cum[..., -1:, :]
        new_state = jnp.exp(last)[..., 0, :, None] * state + jnp.einsum(
            'bhjd,bhje->bhde', ki * jnp.exp(last - cum), vi)
        return new_state, o

    s0 = jnp.zeros((b, h, dk, dv), dtype=jnp.float32)
    _, o = lax.scan(step, s0, (qc, kc, vc, gc))
    return o.transpose(1, 2, 0, 3, 4).reshape(b, h, t, dv)


def setup_inputs(seed: int = 0) -> dict:
    key = jax.random.key(seed)
    ks = jax.random.split(key, 18)
    n = jax.random.normal
    f32 = jnp.float32
    return {
        'x': n(ks[0], (BATCH, SEQ, D_MODEL), f32),
        'w_in': n(ks[1], (DEPTH, D_MODEL, IN_WIDTH), f32) * D_MODEL ** -0.5,
        'gla_w_gate': n(ks[2], (DEPTH, GLA_GATE_RANK, GLA_HEADS * GLA_DK), f32) * GLA_GATE_RANK ** -0.5,
        'gla_b_gate': 0.01 * n(ks[3], (DEPTH, GLA_HEADS * GLA_DK), f32),
        'diff_lambda': 0.1 * n(ks[4], (DEPTH, 4, DIFF_QK_DIM), f32),
        'diff_norm_w': 1.0 + 0.02 * n(ks[5], (DEPTH, DIFF_V_DIM), f32),
        'gla_norm_w': 1.0 + 0.02 * n(ks[6], (DEPTH, GLA_DV), f32),
        'hgrn_norm_w': 1.0 + 0.02 * n(ks[7], (DEPTH, HGRN_DV), f32),
        'hgrn_lb': n(ks[8], (DEPTH, HGRN_HEADS * HGRN_EXPAND), f32),
        'w_branch': n(ks[9], (DEPTH, N_BRANCHES, BRANCH_WIDTH, D_MODEL), f32) * BRANCH_WIDTH ** -0.5 * BETA,
        'w_out': n(ks[10], (DEPTH, D_MODEL, D_MODEL), f32) * D_MODEL ** -0.5 * BETA,
        'ln1_w': 1.0 + 0.02 * n(ks[11], (DEPTH, D_MODEL), f32),
        'ln1_b': 0.02 * n(ks[12], (DEPTH, D_MODEL), f32),
        'w_up': n(ks[13], (DEPTH, D_MODEL, D_FF), f32) * D_MODEL ** -0.5 * BETA,
        'w_down': n(ks[14], (DEPTH, D_FF, D_MODEL), f32) * D_FF ** -0.5 * BETA,
        'ln2_w': 1.0 + 0.02 * n(ks[15], (DEPTH, D_MODEL), f32),
        'ln2_b': 0.02 * n(ks[16], (DEPTH, D_MODEL), f32),
    }


def reference(x, w_in, gla_w_gate, gla_b_gate, diff_lambda, diff_norm_w, gla_norm_w,
              hgrn_norm_w, hgrn_lb, w_branch, w_out, ln1_w, ln1_b, w_up, w_down, ln2_w, ln2_b):
    dt = x.dtype
    b, t, _ = x.shape
    lb_soft = jax.nn.softmax(hgrn_lb.astype(jnp.float32), axis=0)
    lb_all = jnp.cumsum(lb_soft, axis=0) - lb_soft[0:1]
    for l in range(DEPTH):
        z = x @ w_in[l]
        (qa, ka, va, qg, kg, vg, glr, gout, qh, fh, ih, hout, mg) = split_columns(z)

        qa = qa.reshape(b, t, DIFF_HEADS, 2, DIFF_QK_DIM).transpose(0, 2, 3, 1, 4)
        ka = ka.reshape(b, t, DIFF_HEADS, 2, DIFF_QK_DIM).transpose(0, 2, 3, 1, 4)
        va = to_heads(va, DIFF_HEADS)
        lam_init = 0.8 - 0.6 * math.exp(-0.3 * l)
        lp = diff_lambda[l].astype(jnp.float32)
        lam = jnp.exp(jnp.sum(lp[0] * lp[1])) - jnp.exp(jnp.sum(lp[2] * lp[3])) + lam_init
        oa = diff_attention(qa, ka, va, lam)
        oa = from_heads(rms_norm(oa, diff_norm_w[l]) * (1.0 - lam_init))

        log_g = jax.nn.log_sigmoid((glr @ gla_w_gate[l] + gla_b_gate[l]).astype(jnp.float32)) / GLA_GATE_NORM
        ob = chunked_gated_linear_attention(
            to_heads(qg, GLA_HEADS) * GLA_DK ** -0.5, to_heads(kg, GLA_HEADS),
            to_heads(vg, GLA_HEADS), to_heads(log_g, GLA_HEADS))
        ob = from_heads(rms_norm(ob, gla_norm_w[l]) * jax.nn.silu(to_heads(gout, GLA_HEADS).astype(jnp.float32)))

        lb = lb_all[l]
        log_f = jnp.logaddexp(jnp.log(jnp.maximum(lb, LB_FLOOR)),
                              jnp.log1p(-lb) + jax.nn.log_sigmoid(fh.astype(jnp.float32)))
        k_h = -jnp.expm1(log_f)
        oc = chunked_gated_linear_attention(
            to_heads(qh, HGRN_HEADS), to_heads(k_h, HGRN_HEADS),
            to_heads(ih, HGRN_HEADS), to_heads(log_f, HGRN_HEADS))
        oc = from_heads(rms_norm(oc, hgrn_norm_w[l]) * jax.nn.silu(to_heads(hout, HGRN_HEADS).astype(jnp.float32)))

        branches = jnp.stack([oa, ob, oc], axis=2).astype(dt)
        y = jnp.einsum('btnc,ncd->btnd', branches, w_branch[l])
        gates = jax.nn.sigmoid(mg.reshape(b, t, N_BRANCHES, D_MODEL))
        mix = jnp.einsum('btnd,btnd->btd', gates, y) @ w_out[l]
        x = layer_norm(ALPHA * x + mix, ln1_w[l], ln1_b[l])

        hmid = jnp.square(jax.nn.relu(x @ w_up[l]))
        x = layer_norm(ALPHA * x + hmid @ w_down[l], ln2_w[l], ln2_b[l])
    return x
```

```python
import contextlib
import numpy as np
import concourse.bass as bass
import concourse.mybir as mybir
from concourse.bass_utils import run_bass_kernel_spmd

F32 = mybir.dt.float32
BF16 = mybir.dt.bfloat16
AF = mybir.ActivationFunctionType
ALU = mybir.AluOpType
AX = mybir.AxisListType

ENGS = ("pe", "act", "dve", "pool", "sp")


class Buf:
    __slots__ = ("name", "w", "r")

    def __init__(self, name):
        self.name = name
        self.w = None
        self.r = {}


class Chan:
    __slots__ = ("key", "cnt")

    def __init__(self, key):
        self.key = key
        self.cnt = 0


class Sched:
    def __init__(self, nc, stack):
        self.nc = nc
        self.stack = stack
        self.stream = {e: [] for e in ENGS}
        self.cnt = {e: 0 for e in ENGS}
        self.seen = {e: {} for e in ENGS}
        self.sems = {}
        for e in ENGS:
            self.sems[e] = stack.enter_context(nc.semaphore("s_" + e))
        self.chans = []
        self.same_sync = True
        self._uid = 0

    def uid(self):
        self._uid += 1
        return self._uid

    def chan(self, name):
        c = Chan("ch_" + name)
        self.sems[c.key] = self.stack.enter_context(self.nc.semaphore(c.key))
        self.chans.append(c)
        return c

    def _deps(self, reads, writes):
        deps = {}

        def add(t):
            if t is None:
                return
            k, v = t
            if deps.get(k, 0) < v:
                deps[k] = v
        for b in reads:
            add(b.w)
        for b in writes:
            add(b.w)
            for k, v in b.r.items():
                add((k, v))
        return deps

    def _filter(self, eng, deps):
        waits = []
        seen = self.seen[eng]
        for k, v in deps.items():
            if k == eng and (eng == "pe" or eng == "sp" or not self.same_sync):
                continue
            if seen.get(k, 0) >= v:
                continue
            seen[k] = v
            waits.append((k, v))
        return waits

    def _mark(self, tok, reads, writes):
        k, v = tok
        for b in reads:
            if b.r.get(k, 0) < v:
                b.r[k] = v
        for b in writes:
            b.w = tok
            b.r = {}

    def op(self, eng, meth, reads=(), writes=(), *args, **kw):
        fn = (meth, args, kw)
        deps = self._deps(reads, writes)
        waits = self._filter(eng, deps)
        self.cnt[eng] += 1
        tok = (eng, self.cnt[eng])
        self.stream[eng].append((waits, fn, eng, 1))
        self._mark(tok, reads, writes)
        return tok

    def dma(self, q, chan, out, in_, reads=(), writes=(), **kw):
        deps = self._deps(reads, writes)
        if chan.cnt > 0:
            if deps.get(chan.key, 0) < chan.cnt:
                deps[chan.key] = chan.cnt
        waits = self._filter(q, deps)
        chan.cnt += 16
        tok = (chan.key, chan.cnt)
        self.stream[q].append((waits, ("dma_start", (), dict(out=out, in_=in_, **kw)), chan.key, 16))
        self._mark(tok, reads, writes)
        return tok

    def barrier(self):
        snap = {e: self.cnt[e] for e in ENGS if self.cnt[e] > 0}
        for c in self.chans:
            if c.cnt > 0:
                snap[c.key] = c.cnt
        for e in ENGS:
            deps = {k: v for k, v in snap.items() if k != e}
            waits = self._filter(e, deps)
            if waits:
                self.stream[e].append((waits, None, None, 0))

    def final_wait(self, eng, toks):
        deps = {}
        for k, v in toks:
            if deps.get(k, 0) < v:
                deps[k] = v
        waits = self._filter(eng, deps)
        if waits:
            self.stream[eng].append((waits, None, None, 0))

    def emit(self):
        nc = self.nc
        sems = self.sems
        streams = self.stream

        def run(e, lst):
            for waits, fn, key, n in lst:
                for k, v in waits:
                    e.wait_ge(sems[k], v)
                if fn is not None:
                    meth, args, kw = fn
                    ins = getattr(e, meth)(*args, **kw)
                    ins.then_inc(sems[key], n)

        with nc.Block() as block:
            @block.tensor
            def _(e):
                run(e, streams["pe"])

            @block.scalar
            def _(e):
                run(e, streams["act"])

            @block.vector
            def _(e):
                run(e, streams["dve"])

            @block.gpsimd
            def _(e):
                run(e, streams["pool"])

            @block.sync
            def _(e):
                run(e, streams["sp"])

D = 1024
T = 2048
DEPTH = 2
NSEQ = 2
IN_W = 8208
DFF = 4096
ALPHA_C = float((2 * DEPTH) ** 0.25)
EPS = 1e-5
OFF_QA, OFF_KA, OFF_VA = 0, 512, 1024
OFF_QG, OFF_KG, OFF_VG, OFF_GLR, OFF_GOUT = 1536, 1792, 2048, 2560, 2576
OFF_QH, OFF_FH, OFF_IH, OFF_HOUT, OFF_MG = 3088, 3600, 4112, 4624, 5136
SBUF_BYTES = 212800
NTAB = 19


def _prod(s):
    r = 1
    for v in s:
        r *= v
    return r


class Carver:
    def __init__(self, big, lo, hi):
        self.big, self.off, self.hi = big, lo, hi

    def alloc(self, shape, dt):
        sz = 4 if dt == F32 else 2
        nb = _prod(shape) * sz
        nb = (nb + 63) // 64 * 64
        assert self.off + nb <= self.hi, ("SBUF carve overflow", self.off, nb, self.hi)
        ap = self.big[:, self.off // 2:(self.off + nb) // 2]
        self.off += nb
        if dt == F32:
            ap = ap.bitcast(F32)
        ap = ap[:, 0:_prod(shape)]
        if len(shape) == 2:
            ap = ap.rearrange("p (a b) -> p a b", a=shape[0])
        elif len(shape) == 3:
            ap = ap.rearrange("p (a b c) -> p a b c", a=shape[0], b=shape[1])
        return ap


def make_consts():
    c = np.zeros((128, 1024), np.float32)
    c[:, 0:128] = np.eye(128, dtype=np.float32)
    j = np.arange(128)[:, None]
    i = np.arange(128)[None, :]
    c[:, 128:256] = np.where(i < j, -30000.0, 0.0)
    c[:, 256:384] = ((i >= j) & ((i // 64) == (j // 64))).astype(np.float32)
    slopes = [2.0 ** (-8.0 * (h + 1) / 4) for h in range(4)]
    for h in range(4):
        for d in range(NTAB):
            c[:, 384 + h * NTAB + d] = slopes[h] * (128.0 * (d - 15) + np.arange(128))
    c[:, 461] = -0.5
    c[:, 512:1024] = 1.0
    c[:, 512:1024:64] = 0.0
    return c


def build(n_seq=NSEQ, n_layers=DEPTH, dbg=False, stop_after=None, only=None):
    nc = bass.Bass("TRN2", target_bir_lowering=False)
    dram = {}

    def din(name, shape):
        dram[name] = nc.dram_tensor(name, list(shape), F32, kind="ExternalInput").ap()
        return dram[name]
    x_d = din("x", [NSEQ, T, D])
    w_in_d = din("w_in", [DEPTH, D, IN_W])
    wg_d = din("gla_w_gate", [DEPTH, 16, 256])
    bg_d = din("gla_b_gate", [DEPTH, 256])
    dl_d = din("diff_lambda", [DEPTH, 4, 64])
    dnw_d = din("diff_norm_w", [DEPTH, 128])
    gnw_d = din("gla_norm_w", [DEPTH, 128])
    hnw_d = din("hgrn_norm_w", [DEPTH, 128])
    hlb_d = din("hgrn_lb", [DEPTH, 512])
    wbr_d = din("w_branch", [DEPTH, 3, 512, D])
    wout_d = din("w_out", [DEPTH, D, D])
    ln1w_d = din("ln1_w", [DEPTH, D])
    ln1b_d = din("ln1_b", [DEPTH, D])
    wup_d = din("w_up", [DEPTH, D, DFF])
    wdn_d = din("w_down", [DEPTH, DFF, D])
    ln2w_d = din("ln2_w", [DEPTH, D])
    ln2b_d = din("ln2_b", [DEPTH, D])
    cst_d = din("consts", [128, 1024])
    out_d = nc.dram_tensor("out", [NSEQ, T, D], F32, kind="ExternalOutput").ap()
    dbg_d = {}

    with contextlib.ExitStack() as st:
        S = Sched(nc, st)
        big = st.enter_context(nc.sbuf_tensor("big", [128, SBUF_BYTES // 2], BF16))
        psum = [st.enter_context(nc.psum_tensor("ps%d" % i, [128, 512], F32)) for i in range(8)]
        PB = [Buf("psum%d" % i) for i in range(8)]

        def psb(i):
            return psum[i][:].bitcast(BF16)

        P = Carver(big, 0, SBUF_BYTES)
        xres = P.alloc([16, D], F32)
        xT = P.alloc([8, T], BF16)
        cst = P.alloc([1024], F32)
        identb = P.alloc([128], BF16)
        maskb = P.alloc([128], BF16)
        prm = P.alloc([64], F32)
        brT = P.alloc([12, T], BF16)
        SCR_LO = P.off
        XR = [Buf("xres%d" % i) for i in range(16)]
        XT = [Buf("xT%d" % i) for i in range(16)]
        BR = [Buf("brT%d" % i) for i in range(12)]
        CST = Buf("cst")
        PRM = Buf("prm")
        cmask = cst[:, 256:384]
        rmask = cst[:, 512:1024]
        nhalf = cst[:, 461:462]

        def tab(h, d):
            i = 384 + h * NTAB + d + 15
            return cst[:, i:i + 1]

        ch_x = [S.chan("x%d" % i) for i in range(2)]
        ch_o = [S.chan("o%d" % i) for i in range(2)]
        ch_c = S.chan("c")
        ch_p = S.chan("p")
        ch_dbg = S.chan("dbg")
        out_toks = []

        def dump(name, ap, shape, dt, reads):
            if not dbg:
                return
            t = nc.dram_tensor("dbg_" + name, [128] + list(shape), dt, kind="ExternalOutput").ap()
            dbg_d[name] = t
            out_toks.append(S.dma("sp", ch_dbg, t, ap, reads=reads))

        def ACT(reads, writes, **kw):
            S.op("act", "activation", reads, writes, **kw)

        def V(meth, reads, writes, *a, **kw):
            S.op("dve", meth, reads, writes, *a, **kw)

        def G(meth, reads, writes, *a, **kw):
            S.op("pool", meth, reads, writes, *a, **kw)

        def mm(out, lhsT, rhs, start, stop, reads, writes):
            S.op("pe", "matmul", reads, writes, out, lhsT=lhsT, rhs=rhs, start=start, stop=stop, skip_group_check=True)

        def transp(out, in_, reads, writes):
            S.op("pe", "transpose", list(reads) + [CST], writes, out=out, in_=in_, identity=identb)

        S.dma("sp", ch_c, cst, cst_d, writes=[CST])
        V("tensor_copy", [CST], [CST], out=identb, in_=cst[:, 0:128])
        V("tensor_copy", [CST], [CST], out=maskb, in_=cst[:, 128:256])
        SC0 = Carver(big, SCR_LO, SBUF_BYTES)
        dlt = SC0.alloc([2, 4, 64], F32)
        dlp = SC0.alloc([2, 2, 64], F32)
        lbt = SC0.alloc([2, 4], F32)
        SETUP = Buf("setup")
        S.dma("sp", ch_p, dlt.rearrange("p l a b -> p (l a b)"),
              dl_d.rearrange("l a b -> (l a b)").partition_broadcast(128), writes=[SETUP])
        S.dma("sp", ch_p, lbt, hlb_d.rearrange("l (h p) -> p l h", p=128), writes=[SETUP], allow_slow_non_contiguous=True)
        for l in range(DEPTH):
            b = 32 * l
            S.dma("sp", ch_p, prm[:, b + 1:b + 2], dnw_d[l].unsqueeze(1), writes=[PRM])
            S.dma("sp", ch_p, prm[:, b + 2:b + 3], gnw_d[l].unsqueeze(1), writes=[PRM])
            S.dma("sp", ch_p, prm[:, b + 3:b + 4], hnw_d[l].unsqueeze(1), writes=[PRM])
            S.dma("sp", ch_p, prm[:, b + 4:b + 6], bg_d[l].rearrange("(g p) -> p g", p=128), writes=[PRM], allow_slow_non_contiguous=True)
        for l in range(DEPTH):
            b = 32 * l
            lam_init = 0.8 - 0.6 * float(np.exp(-0.3 * l))
            V("tensor_tensor", [SETUP], [SETUP], out=dlp[:, l, 0, :], in0=dlt[:, l, 0, :], in1=dlt[:, l, 1, :], op=ALU.mult)
            V("tensor_tensor", [SETUP], [SETUP], out=dlp[:, l, 1, :], in0=dlt[:, l, 2, :], in1=dlt[:, l, 3, :], op=ALU.mult)
            V("reduce_sum", [SETUP, PRM], [PRM], out=prm[:, b + 16:b + 18], in_=dlp[:, l, :, :], axis=AX.X)
            ACT([PRM], [PRM], out=prm[:, b + 18:b + 20], in_=prm[:, b + 16:b + 18], func=AF.Exp)
            V("scalar_tensor_tensor", [PRM], [PRM], out=prm[:, b + 0:b + 1], in0=prm[:, b + 19:b + 20], scalar=-lam_init, in1=prm[:, b + 18:b + 19], op0=ALU.add, op1=ALU.subtract)
            V("tensor_scalar", [PRM], [PRM], out=prm[:, b + 1:b + 2], in0=prm[:, b + 1:b + 2], scalar1=1.0 - lam_init, scalar2=None, op0=ALU.mult)
            V("tensor_scalar", [PRM], [PRM], out=prm[:, b + 4:b + 6], in0=prm[:, b + 4:b + 6], scalar1=-1.0, scalar2=None, op0=ALU.mult)
        V("memset", [PRM], [PRM], prm[:, 6:10], 1.0)
        V("memset", [PRM], [PRM], prm[:, 10:14], 1e-30)
        V("tensor_tensor", [SETUP, PRM], [PRM], out=prm[:, 48:52], in0=lbt[:, 1, :], in1=lbt[:, 0, :], op=ALU.subtract)
        ACT([PRM], [PRM], out=prm[:, 52:56], in_=prm[:, 48:52], func=AF.Sigmoid)
        V("tensor_scalar", [PRM], [PRM], out=prm[:, 38:42], in0=prm[:, 52:56], scalar1=-1.0, scalar2=1.0, op0=ALU.mult, op1=ALU.add)
        V("tensor_scalar", [PRM], [PRM], out=prm[:, 42:46], in0=prm[:, 52:56], scalar1=1e-30, scalar2=None, op0=ALU.max)
        S.barrier()

        PEND = []

        def flush_pending():
            while PEND:
                PEND.pop(0)()

        class Slots:
            def __init__(self, carver, n, shape, name):
                self.aps = [carver.alloc(shape, BF16) for _ in range(n)]
                self.bufs = [Buf("%s%d" % (name, i)) for i in range(n)]
                self.chs = [S.chan("%s%d_%d" % (name, i, S.uid())) for i in range(n)]
                self.i = 0

            def next(self):
                k = self.i % len(self.aps)
                self.i += 1
                return k

        def run_items(items, depth):
            loads = [i for i, it in enumerate(items) if it[0] is not None]
            handles = {}
            state = {"p": 0, "out": 0}

            def pump():
                while state["p"] < len(loads) and state["out"] < depth:
                    i = loads[state["p"]]
                    handles[i] = items[i][0]()
                    state["p"] += 1
                    state["out"] += 1
            for i, it in enumerate(items):
                pump()
                it[1](handles.get(i))
                if it[0] is not None:
                    state["out"] -= 1

        def load_cols(slots, src3, ncols):
            k = slots.next()
            S.dma("pool", slots.chs[k], slots.aps[k][:, :, 0:ncols], src3, writes=[slots.bufs[k]])
            return k

        def win_cols(l, c0, n):
            return w_in_d[l][:, c0:c0 + n].rearrange("(k p) n -> p k n", p=128)

        def proj_fm(wap, wbuf, M, banks, evac):
            for j in range(4):
                b = banks[j % len(banks)]
                for kc in range(8):
                    mm(psum[b][0:M, :], wap[:, kc, 0:M], xT[:, kc, j * 512:(j + 1) * 512], kc == 0, kc == 7,
                       [wbuf] + XT[4 * j:4 * j + 4], [PB[b]])
                evac(j, b)
                if j == 1:
                    flush_pending()

        def proj_tm(wap, wbuf, N, banks, evac):
            for g in range(4):
                b = banks[g % len(banks)]
                for q in range(4):
                    tt = 4 * g + q
                    for kc in range(8):
                        mm(psum[b][:, q * 128:q * 128 + N], xT[:, kc, tt * 128:(tt + 1) * 128], wap[:, kc, 0:N], kc == 0, kc == 7,
                           [wbuf, XT[tt]], [PB[b]])
                evac(g, b)
                if g == 1:
                    flush_pending()

        def to_xT(tt, tbank, xb, XB):
            ACT([XR[tt]], [XB], out=xb, in_=xres[:, tt, :], func=AF.Copy)
            pv = psb(tbank)
            for k in range(8):
                transp(pv[:, k * 128:(k + 1) * 128], xb[:, k * 128:(k + 1) * 128], [XB], [PB[tbank]])
            ACT([PB[tbank]], [XT[tt]], out=xT[:, :, tt * 128:(tt + 1) * 128], in_=pv.rearrange("p (k n) -> p k n", k=8), func=AF.Copy)

        def ln_pre(tt, hf, bank, st6t):
            sl = slice(hf * 512, (hf + 1) * 512)
            xr = xres[:, tt, :]
            V("scalar_tensor_tensor", [PB[bank]], [XR[tt]], out=xr[:, sl], in0=xr[:, sl], scalar=ALPHA_C, in1=psum[bank][:], op0=ALU.mult, op1=ALU.add)
            V("bn_stats", [XR[tt]], [XR[tt]], out=st6t[:, hf, :], in_=xr[:, sl])

        def ln_finish_a(tt, st6t, lnw, lnb, LNP, mv, xb, LNB):
            xr = xres[:, tt, :]
            V("bn_aggr", [XR[tt]], [LNB], out=mv[:, 0:2], in_=st6t.rearrange("p a b -> p (a b)"))
            V("tensor_scalar", [LNB], [LNB], out=mv[:, 2:3], in0=mv[:, 1:2], scalar1=EPS, scalar2=None, op0=ALU.add)
            ACT([LNB], [LNB], out=mv[:, 3:4], in_=mv[:, 2:3], func=AF.Sqrt)
            V("reciprocal", [LNB], [LNB], out=mv[:, 4:5], in_=mv[:, 3:4])
            V("scalar_tensor_tensor", [LNB, LNP], [XR[tt]], out=xr, in0=xr, scalar=mv[:, 0:1], in1=lnw, op0=ALU.subtract, op1=ALU.mult)
            V("scalar_tensor_tensor", [LNB, LNP], [XR[tt]], out=xr, in0=xr, scalar=mv[:, 4:5], in1=lnb, op0=ALU.mult, op1=ALU.add)
            ACT([XR[tt]], [LNB], out=xb, in_=xres[:, tt, :], func=AF.Copy)

        def ln_finish_b(tt, tbank, xb, LNB):
            pv = psb(tbank)
            for k in range(8):
                transp(pv[:, k * 128:(k + 1) * 128], xb[:, k * 128:(k + 1) * 128], [LNB], [PB[tbank]])
            ACT([PB[tbank]], [XT[tt]], out=xT[:, :, tt * 128:(tt + 1) * 128], in_=pv.rearrange("p (k n) -> p k n", k=8), func=AF.Copy)

        for s in range(n_seq):
            C = Carver(big, SCR_LO, SBUF_BYTES)
            xb0 = C.alloc([D], BF16)
            XB0 = Buf("xb0")
            for tt in range(16):
                S.dma("sp", ch_x[tt % 2], xres[:, tt, :], x_d[s, tt * 128:(tt + 1) * 128, :], writes=[XR[tt]])
            for tt in range(16):
                to_xT(tt, tt % 2, xb0, XB0)
            S.barrier()
            stop = False
            for l in range(n_layers):
                pb = 32 * l
                C = Carver(big, SCR_LO, SBUF_BYTES)
                WS = Slots(C, 3, [8, 128], "ws")
                vbuf = [C.alloc([16, 130], BF16) for _ in range(2)]
                VB = [Buf("v0"), Buf("v1")]
                qT = C.alloc([T], BF16)
                kT = C.alloc([T], BF16)
                QT, KT = Buf("qT"), Buf("kT")
                sm = C.alloc([64], F32)
                SMB = [Buf("sm%d" % i) for i in range(4)]
                C_shared = C.off
                CA = Carver(big, C_shared, SBUF_BYTES)
                onb = CA.alloc([4, 128], BF16)
                ONBB = [Buf("onb%d" % i) for i in range(4)]
                qTp = CA.alloc([2, T], BF16)
                QTP = Buf("qTp")
                PT = [CA.alloc([512], BF16) for _ in range(3)]
                PTB = [Buf("pt%d" % i) for i in range(3)]
                ost = [CA.alloc([2, 129], F32) for _ in range(4)]
                OST = [Buf("ost%d" % i) for i in range(4)]
                o1 = [CA.alloc([128], F32) for _ in range(4)]
                sqj4 = [CA.alloc([128], F32) for _ in range(4)]
                OFB = [Buf("ofin%d" % i) for i in range(4)]
                CG = Carver(big, C_shared, SBUF_BYTES)
                cum = CG.alloc([T], F32)
                tmpA = CG.alloc([512], F32)
                tmpB = CG.alloc([512], F32)
                tmpC = CG.alloc([512], F32)
                khT0 = CG.alloc([512], BF16)
                khat = CG.alloc([16, 128], BF16)
                Sbf = CG.alloc([32, 128], BF16)
                Sf = CG.alloc([4, 128], F32)
                sgT = CG.alloc([T], BF16)
                elast = CG.alloc([32], F32)
                wgb = CG.alloc([256], BF16)
                PTg4 = CG.alloc([4, 128], BF16)
                onb4 = [CG.alloc([4, 128], BF16) for _ in range(2)]
                ONB4 = [Buf("onb4_0"), Buf("onb4_1")]
                sm4 = [sm[:, 0:16], sm[:, 16:32]]
                CUM, TA, TB, TC, KHT, KHAT, SBF, SGT, EL, GLR, WG, SQG = [Buf(n) for n in
                    ["cum", "tA", "tB", "tC", "khT", "khat", "sbf", "sgT", "elast", "glrT", "wgb", "sqg"]]
                glrT = sgT
                GLR = SGT
                SFB = [Buf("sf0"), Buf("sf1")]
                PTGB = Buf("ptg4")
                khT2 = [khT0, PTg4.rearrange("p a n -> p (a n)")]
                KHTB = [Buf("khT0"), PTGB]
                ch_wg = S.chan("wg_%d" % S.uid())

                for vb in range(2):
                    V("memset", [], [VB[vb]], vbuf[vb][:, :, 128:130], 1.0)
                V("memset", [], [QTP], qTp[64:128, 0, :], 0.0)
                V("memset", [], [QTP], qTp[0:64, 1, :], 0.0)

                items = []

                def evac_copy_bf16(dst, DB, eng):
                    def f(j, b):
                        if eng == "act":
                            ACT([PB[b]], [DB], out=dst[:, j * 512:(j + 1) * 512], in_=psum[b][:], func=AF.Copy)
                        else:
                            V("tensor_copy", [PB[b]], [DB], out=dst[:, j * 512:(j + 1) * 512], in_=psum[b][:])
                    return f

                def evac_v(vb):
                    def f(g, b):
                        ACT([PB[b]], [VB[vb]], out=vbuf[vb][:, 4 * g:4 * g + 4, 0:128], in_=psum[b][:].rearrange("p (a n) -> p a n", a=4), func=AF.Copy)
                    return f

                def rms_chain(src, SRC, qs, k0, junk):
                    SM = SMB[qs]
                    ACT([SRC], [SM], out=junk, in_=src, func=AF.Square, accum_out=sm[:, k0:k0 + 1])
                    V("tensor_scalar", [SM], [SM], out=sm[:, k0 + 1:k0 + 2], in0=sm[:, k0:k0 + 1], scalar1=1.0 / 128, scalar2=EPS, op0=ALU.mult, op1=ALU.add)
                    ACT([SM], [SM], out=sm[:, k0 + 2:k0 + 3], in_=sm[:, k0 + 1:k0 + 2], func=AF.Sqrt)
                    V("reciprocal", [SM], [SM], out=sm[:, k0 + 3:k0 + 4], in_=sm[:, k0 + 2:k0 + 3])
                    V("tensor_scalar", [SRC, SM], [ONBB[qs]], out=onb[:, qs, :], in0=src, scalar1=sm[:, k0 + 3:k0 + 4], scalar2=None, op0=ALU.mult)

                def onb_transpose(qs, tb):
                    transp(psb(tb)[:, qs * 128:(qs + 1) * 128], onb[:, qs, :], [ONBB[qs]], [PB[tb]])

                def finish_T(dst, DB, wcol, gate, tb=0):
                    src = psb(tb)[:, 0:512]
                    if gate is None:
                        V("tensor_scalar", [PB[tb], PRM], [DB], out=dst, in0=src, scalar1=wcol, scalar2=None, op0=ALU.mult)
                    else:
                        gap, GB = gate
                        V("scalar_tensor_tensor", [PB[tb], PRM, GB], [DB], out=dst, in0=src, scalar=wcol, in1=gap, op0=ALU.mult, op1=ALU.mult)

                def evac_qpad(j, b):
                    sl = slice(j * 512, (j + 1) * 512)
                    ACT([PB[b]], [QTP], out=qTp[0:64, 0, sl], in_=psum[b][0:64, :], func=AF.Copy)
                    V("tensor_copy", [PB[b]], [QTP], out=qTp[64:128, 1, sl], in_=psum[b][64:128, :])

                def attn_head(h):
                    hb = h
                    for qt in range(4):
                        steps = [(m, jb) for jb in range(4 * (qt + 1)) for m in (0, 1)]

                        def emit_S(idx):
                            m, jb = steps[idx]
                            bk = SBK[idx % 3]
                            r = jb - 4 * qt
                            c0 = 128 * r if r > 0 else 0
                            mm(psum[bk][:, c0:512], kT[:, jb * 128:(jb + 1) * 128], qTp[:, m, qt * 512 + c0:qt * 512 + 512],
                               True, r < 0, [KT, QTP], [PB[bk]])
                            if r >= 0:
                                mm(psum[bk][:, c0:c0 + 128], identb, maskb, False, True, [CST], [PB[bk]])

                        def emit_rest(idx):
                            m, jb = steps[idx]
                            bk = SBK[idx % 3]
                            pt = idx % 3
                            r = jb - 4 * qt
                            c0 = 128 * r if r > 0 else 0
                            if h == 0:
                                for u in range(2):
                                    a, bnd = max(c0, 256 * u), 256 * u + 256
                                    if a >= bnd:
                                        continue
                                    d = jb - 4 * qt - 2 * u
                                    ACT([PB[bk], CST], [PTB[pt]], out=PT[pt][:, a:bnd], in_=psum[bk][:, a:bnd], func=AF.Exp, bias=tab(0, d), scale=0.125)
                            else:
                                d = jb - 4 * qt
                                ACT([PB[bk], CST], [PTB[pt]], out=PT[pt][:, c0:512], in_=psum[bk][:, c0:512], func=AF.Exp, bias=tab(h, d), scale=0.125)
                            for qs in range(max(r, 0), 4):
                                ob = 4 + 2 * m + qs // 2
                                oc = (qs % 2) * 256
                                mm(psum[ob][:, oc:oc + 129], PT[pt][:, qs * 128:(qs + 1) * 128], vbuf[0][:, jb, 0:129],
                                   (jb == 0 and qs % 2 == 0), (jb == 4 * qt + qs), [PTB[pt], VB[0]], [PB[ob]])
                        n = len(steps)
                        SBK = [2, 3, 0]
                        emit_S(0)
                        emit_S(1)
                        for idx in range(n):
                            if idx + 2 < n:
                                emit_S(idx + 2)
                            emit_rest(idx)
                            if idx == 7:
                                flush_pending()
                        QS = range(4)
                        oc_ = lambda qs: (qs % 2) * 256
                        b0_ = lambda qs: 4 + qs // 2
                        b1_ = lambda qs: 6 + qs // 2
                        for bq in range(4):
                            src_o = psum[4 + bq][:].rearrange("p (a n) -> p a n", a=2)[:, :, 0:129]
                            if bq % 2 == 0:
                                ACT([PB[4 + bq]], [OST[bq]], out=ost[bq], in_=src_o, func=AF.Copy)
                            else:
                                V("tensor_copy", [PB[4 + bq]], [OST[bq]], out=ost[bq], in_=src_o)
                        s0_ = lambda qs: ost[qs // 2]
                        s1_ = lambda qs: ost[2 + qs // 2]
                        S0_ = lambda qs: OST[qs // 2]
                        S1_ = lambda qs: OST[2 + qs // 2]
                        for qs in QS:
                            V("reciprocal", [S0_(qs)], [SMB[qs]], out=sm[:, 8 * qs:8 * qs + 1], in_=s0_(qs)[:, qs % 2, 128:129])
                            V("reciprocal", [S1_(qs)], [SMB[qs]], out=sm[:, 8 * qs + 1:8 * qs + 2], in_=s1_(qs)[:, qs % 2, 128:129])
                        for qs in QS:
                            V("tensor_tensor", [SMB[qs], PRM], [SMB[qs]], out=sm[:, 8 * qs + 2:8 * qs + 3], in0=sm[:, 8 * qs + 1:8 * qs + 2], in1=prm[:, pb:pb + 1], op=ALU.mult)
                        for qs in QS:
                            V("tensor_scalar", [S0_(qs), SMB[qs]], [OFB[qs]], out=o1[qs], in0=s0_(qs)[:, qs % 2, 0:128], scalar1=sm[:, 8 * qs:8 * qs + 1], scalar2=None, op0=ALU.mult)
                        for qs in QS:
                            V("scalar_tensor_tensor", [S1_(qs), SMB[qs]], [OFB[qs]], out=o1[qs], in0=s1_(qs)[:, qs % 2, 0:128], scalar=sm[:, 8 * qs + 2:8 * qs + 3], in1=o1[qs], op0=ALU.mult, op1=ALU.add)
                        for qs in QS:
                            V("scalar_tensor_tensor", [OFB[qs]], [SMB[qs]], out=sqj4[qs], in0=o1[qs], scalar=1.0, in1=o1[qs], op0=ALU.mult, op1=ALU.mult, accum_out=sm[:, 8 * qs + 3:8 * qs + 4])
                        for qs in QS:
                            V("tensor_scalar", [SMB[qs]], [SMB[qs]], out=sm[:, 8 * qs + 4:8 * qs + 5], in0=sm[:, 8 * qs + 3:8 * qs + 4], scalar1=1.0 / 128, scalar2=EPS, op0=ALU.mult, op1=ALU.add)
                        for qs in QS:
                            S.op("pool", "tensor_tensor", [SMB[qs], CST], [SMB[qs]], out=sm[:, 8 * qs + 6:8 * qs + 7], in0=sm[:, 8 * qs + 4:8 * qs + 5], in1=nhalf, op=ALU.pow)
                        for qs in QS:
                            V("tensor_scalar", [OFB[qs], SMB[qs]], [ONBB[qs]], out=onb[:, qs, :], in0=o1[qs], scalar1=sm[:, 8 * qs + 6:8 * qs + 7], scalar2=None, op0=ALU.mult)

                        def deferred(qt=qt):
                            for qs in range(4):
                                transp(psb(1)[:, qs * 128:(qs + 1) * 128], onb[:, qs, :], [ONBB[qs]], [PB[1]])
                            finish_T(brT[:, hb, qt * 512:(qt + 1) * 512], BR[hb], prm[:, pb + 1:pb + 2], None, 1)
                        PEND.append(deferred)

                for h in range(4):
                    items.append((lambda h=h: load_cols(WS, win_cols(l, OFF_QA + h * 128, 128), 128),
                                  lambda k: proj_fm(WS.aps[k], WS.bufs[k], 128, [0, 1], evac_qpad)))
                    items.append((lambda h=h: load_cols(WS, win_cols(l, OFF_KA + h * 128, 128), 128),
                                  lambda k: proj_fm(WS.aps[k], WS.bufs[k], 128, [0, 1], evac_copy_bf16(kT, KT, "dve"))))
                    items.append((lambda h=h: load_cols(WS, win_cols(l, OFF_VA + h * 128, 128), 128),
                                  lambda k: proj_tm(WS.aps[k], WS.bufs[k], 128, [0, 1], evac_v(0))))
                    items.append((None, lambda k, h=h: attn_head(h)))

                def k_stage(j, kap, KB, sc):
                    sl = slice(j * 512, (j + 1) * 512)
                    ACT([CUM], [TC], out=tmpC, in_=cum[:, sl], func=AF.Exp, scale=-sc)
                    V("tensor_tensor", [KB, TC], [KT], out=kT[:, sl], in0=kap, in1=tmpC, op=ALU.mult)
                    if len(PEND) >= 2:
                        PEND.pop(0)()
                    V("tensor_tensor", [KT, EL], [KHTB[j % 2]], out=khT2[j % 2].rearrange("p (a b) -> p a b", a=8), in0=kT[:, sl].rearrange("p (a b) -> p a b", a=8),
                      in1=elast[:, 8 * j:8 * j + 8].unsqueeze(2).to_broadcast([128, 8, 64]), op=ALU.mult)
                    def dtr(j=j):
                        pv = psb(7)
                        for q in range(4):
                            transp(pv[:, q * 128:(q + 1) * 128], khT2[j % 2][:, q * 128:(q + 1) * 128], [KHTB[j % 2]], [PB[7]])
                        ACT([PB[7]], [KHAT], out=khat[:, 4 * j:4 * j + 4, :], in_=pv[:, 0:512].rearrange("p (a n) -> p a n", a=4), func=AF.Copy)
                    PEND.append(dtr)

                def gate_tail(j, csrc, CB, sc):
                    sl = slice(j * 512, (j + 1) * 512)
                    V("tensor_tensor_scan", [CB, CST], [CUM], out=cum[:, sl], data0=rmask, data1=csrc, initial=0.0, op0=ALU.mult, op1=ALU.add)
                    ACT([CUM], [EL], out=elast[:, 8 * j:8 * j + 8], in_=cum[:, j * 512 + 63:(j + 1) * 512:64], func=AF.Exp, scale=sc)

                def q_stage(sc, qscale):
                    def f(j, b):
                        sl = slice(j * 512, (j + 1) * 512)
                        ACT([CUM], [TA], out=tmpA, in_=cum[:, sl], func=AF.Exp, scale=sc)
                        V("scalar_tensor_tensor", [PB[b], TA], [QT], out=qT[:, sl], in0=psum[b][:], scalar=qscale, in1=tmpA, op0=ALU.mult, op1=ALU.mult)
                    return f

                def evac_silu(j, b):
                    ACT([PB[b]], [SGT], out=sgT[:, j * 512:(j + 1) * 512], in_=psum[b][:], func=AF.Silu)

                def unit_state(subs, mid=None):
                    flush_pending()
                    V("memset", [], [SBF], Sbf[:, 0, :], 0.0)
                    for g in range(4):
                        ubs = [4 + 2 * (g % 2), 5 + 2 * (g % 2)]
                        for cc in range(8):
                            c = 8 * g + cc
                            t, hf = c // 2, c % 2
                            ub = ubs[hf]
                            for (rb, nr, vb) in subs:
                                mm(psum[ub][rb:rb + nr, (cc // 2) * 128:(cc // 2 + 1) * 128], khat[64 * hf:64 * hf + 64, t, rb:rb + nr], vbuf[vb][64 * hf:64 * hf + 64, t, 0:128],
                                   True, True, [KHAT, VB[vb]], [PB[ub]])
                        if g == 1 and mid is not None:
                            mid()
                        for cc in range(8):
                            c = 8 * g + cc
                            if c == 31:
                                break
                            ub = ubs[c % 2]
                            usrc = psum[ub][:, (cc // 2) * 128:(cc // 2 + 1) * 128]
                            dstS = Sf[:, (c + 1) % 4, :]
                            DB_ = SFB[((c + 1) % 4) // 2]
                            if c == 0:
                                V("tensor_copy", [PB[ub]], [DB_], out=dstS, in_=usrc)
                            else:
                                V("scalar_tensor_tensor", [PB[ub], EL, SFB[(c % 4) // 2]], [DB_], out=dstS, in0=Sf[:, c % 4, :], scalar=elast[:, c:c + 1],
                                  in1=usrc, op0=ALU.mult, op1=ALU.add)
                            ACT([DB_], [SBF], out=Sbf[:, c + 1, :], in_=dstS, func=AF.Copy)
                def unit_out(sub, bi, nwcol):
                    rb, nr, vb = sub
                    sqbuf = [tmpB, tmpC]
                    SQB = [TB, TC]

                    def tail(j):
                        par = j % 2
                        tb = j % 2
                        for q in range(4):
                            transp(psb(tb)[:, q * 128:(q + 1) * 128], onb4[par][:, q, :], [ONB4[par]], [PB[tb]])
                        finish_T(brT[:, bi, j * 512:(j + 1) * 512], BR[bi], nwcol, (sgT[:, j * 512:(j + 1) * 512], SGT), tb)
                    for j in range(4):
                        par = j % 2
                        sbk, obk = (2, 3) if par == 0 else (4, 5)
                        for q in range(4):
                            t = 4 * j + q
                            tsl = slice(t * 128, (t + 1) * 128)
                            mm(psum[sbk][:, q * 128:(q + 1) * 128], kT[rb:rb + nr, tsl], qT[rb:rb + nr, tsl], True, True, [KT, QT], [PB[sbk]])
                        V("tensor_tensor", [PB[sbk], CST], [PTGB], out=PTg4, in0=psum[sbk][:].rearrange("p (a n) -> p a n", a=4),
                          in1=cmask.unsqueeze(1).to_broadcast([128, 4, 128]), op=ALU.mult)
                        if j > 0:
                            tail(j - 1)
                        for q in range(4):
                            t = 4 * j + q
                            csl = slice(q * 128, (q + 1) * 128)
                            mm(psum[obk][:, csl], PTg4[:, q, :], vbuf[vb][:, t, 0:128], True, False, [PTGB, VB[vb]], [PB[obk]])
                            for hf in range(2):
                                c = 2 * t + hf
                                mm(psum[obk][64 * hf:64 * hf + 64, csl], qT[rb:rb + nr, t * 128 + 64 * hf:t * 128 + 64 * hf + 64], Sbf[rb:rb + nr, c, :], False, hf == 1,
                                   [QT, SBF], [PB[obk]])
                        o3 = psum[obk][:].rearrange("p (a n) -> p a n", a=4)
                        s4 = sm4[par]
                        SMp = SMB[par]
                        ACT([PB[obk]], [SQB[par]], out=sqbuf[par], in_=psum[obk][:], func=AF.Square)
                        V("reduce_sum", [SQB[par]], [SMp], out=s4[:, 0:4], in_=sqbuf[par].rearrange("p (a n) -> p a n", a=4), axis=AX.X)
                        V("tensor_scalar", [SMp], [SMp], out=s4[:, 4:8], in0=s4[:, 0:4], scalar1=1.0 / 128, scalar2=EPS, op0=ALU.mult, op1=ALU.add)
                        S.op("pool", "tensor_tensor", [SMp, CST], [SMp], out=s4[:, 12:16], in0=s4[:, 4:8], in1=nhalf.to_broadcast([128, 4]), op=ALU.pow)
                        V("tensor_tensor", [PB[obk], SMp], [ONB4[par]], out=onb4[par], in0=o3, in1=s4[:, 12:16].unsqueeze(2).to_broadcast([128, 4, 128]), op=ALU.mult)
                    tail(3)

                def sg_item(c0):
                    return (lambda: load_cols(WS, win_cols(l, c0, 128), 128),
                            lambda k: proj_fm(WS.aps[k], WS.bufs[k], 128, [0, 1], evac_silu))

                def load_small(k):
                    S.dma("pool", ch_wg, wgb[0:16, :], wg_d[l], writes=[WG])
                items.append((None, load_small))

                def evac_glr(j, b):
                    ACT([PB[b]], [GLR], out=glrT[0:16, j * 512:(j + 1) * 512], in_=psum[b][0:16, :], func=AF.Copy)
                for g in range(2):
                    items.append((lambda: load_cols(WS, win_cols(l, OFF_GLR, 16), 16),
                                  lambda k: proj_fm(WS.aps[k], WS.bufs[k], 16, [0, 1], evac_glr)))
                    def gla_gate(k, g=g):
                        for j in range(4):
                            b = j % 2
                            mm(psum[b][:], wgb[0:16, g * 128:(g + 1) * 128], glrT[0:16, j * 512:(j + 1) * 512], True, True, [WG, GLR], [PB[b]])
                            ACT([PB[b], PRM], [TA], out=tmpA, in_=psum[b][:], func=AF.Exp, bias=prm[:, pb + 4 + g:pb + 5 + g], scale=-1.0)
                            ACT([TA], [TB], out=tmpB, in_=tmpA, func=AF.Ln, bias=1.0, scale=1.0)
                            gate_tail(j, tmpB, TB, -1.0 / 16)
                    items.append((None, gla_gate))
                    items.append((lambda g=g: load_cols(WS, win_cols(l, OFF_KG + g * 128, 128), 128),
                                  lambda k: proj_fm(WS.aps[k], WS.bufs[k], 128, [0, 1], lambda j, b: k_stage(j, psum[b][:], PB[b], -1.0 / 16))))
                    items.append((lambda g=g: load_cols(WS, win_cols(l, OFF_QG + g * 128, 128), 128),
                                  lambda k: proj_fm(WS.aps[k], WS.bufs[k], 128, [0, 1], q_stage(-1.0 / 16, 0.125))))
                    for si in range(2):
                        items.append((lambda g=g, si=si: load_cols(WS, win_cols(l, OFF_VG + (2 * g + si) * 128, 128), 128),
                                      lambda k, si=si: proj_tm(WS.aps[k], WS.bufs[k], 128, [0, 1], evac_v(si))))
                    gsubs = [(0, 64, 0), (64, 64, 1)]
                    items.append((lambda g=g: load_cols(WS, win_cols(l, OFF_GOUT + (2 * g) * 128, 128), 128),
                                  lambda k: unit_state(gsubs, lambda: proj_fm(WS.aps[k], WS.bufs[k], 128, [0, 1], evac_silu))))
                    items.append((None, lambda k, g=g: unit_out(gsubs[0], 4 + 2 * g, prm[:, pb + 2:pb + 3])))
                    items.append(sg_item(OFF_GOUT + (2 * g + 1) * 128))
                    items.append((None, lambda k, g=g: unit_out(gsubs[1], 4 + 2 * g + 1, prm[:, pb + 2:pb + 3])))
                for h in range(4):
                    def hg_gate(j, b, h=h):
                        ACT([PB[b]], [TA], out=tmpA, in_=psum[b][:], func=AF.Sigmoid)
                        V("tensor_scalar", [TA, PRM], [TB], out=tmpB, in0=tmpA, scalar1=prm[:, pb + 6 + h:pb + 7 + h], scalar2=prm[:, pb + 10 + h:pb + 11 + h], op0=ALU.mult, op1=ALU.add)
                        ACT([TB], [TA], out=tmpA, in_=tmpB, func=AF.Ln)
                        V("tensor_scalar", [TB], [TB], out=tmpB, in0=tmpB, scalar1=-1.0, scalar2=1.0, op0=ALU.mult, op1=ALU.add)
                        gate_tail(j, tmpA, TA, 1.0)
                        k_stage(j, tmpB, TB, 1.0)
                    items.append((lambda h=h: load_cols(WS, win_cols(l, OFF_FH + h * 128, 128), 128),
                                  lambda k, hg_gate=hg_gate: proj_fm(WS.aps[k], WS.bufs[k], 128, [0, 1], hg_gate)))
                    items.append((lambda h=h: load_cols(WS, win_cols(l, OFF_QH + h * 128, 128), 128),
                                  lambda k: proj_fm(WS.aps[k], WS.bufs[k], 128, [0, 1], q_stage(1.0, 1.0))))
                    items.append((lambda h=h: load_cols(WS, win_cols(l, OFF_IH + h * 128, 128), 128),
                                  lambda k: proj_tm(WS.aps[k], WS.bufs[k], 128, [0, 1], evac_v(0))))
                    items.append((lambda h=h: load_cols(WS, win_cols(l, OFF_HOUT + h * 128, 128), 128),
                                  lambda k: unit_state([(0, 128, 0)], lambda: proj_fm(WS.aps[k], WS.bufs[k], 128, [0, 1], evac_silu))))
                    items.append((None, lambda k, h=h: unit_out((0, 128, 0), 8 + h, prm[:, pb + 3:pb + 4])))

                if only is not None:
                    items = [it for i, it in enumerate(items) if only(i)]
                run_items(items, 2)
                flush_pending()
                if dbg and s == 0 and l == 0:
                    dump("brT", brT, [12, T], BF16, BR)
                S.barrier()
                if stop_after == "mix":
                    stop = True
                    break

                C = Carver(big, SCR_LO, SBUF_BYTES)
                mergedT = C.alloc([8, T], BF16)
                MG = [Buf("mg%d" % i) for i in range(8)]
                WG1 = Slots(C, 2, [8, 3, 128], "wg1")
                WB1 = Slots(C, 2, [3, 4, 128], "wb1")
                sig = [C.alloc([512], F32) for _ in range(3)]
                SIG = [Buf("sig%d" % i) for i in range(3)]
                acc = C.alloc([512], F32)
                ACC = Buf("acc")

                def t1_load(c):
                    def f():
                        k = WG1.next()
                        for n in range(3):
                            S.dma("pool", WG1.chs[k], WG1.aps[k][:, :, n, :], win_cols(l, OFF_MG + n * 1024 + c * 128, 128), writes=[WG1.bufs[k]])
                        k2 = WB1.next()
                        for n in range(3):
                            S.dma("pool", WB1.chs[k2], WB1.aps[k2][:, n, :, :], wbr_d[l, n][:, c * 128:(c + 1) * 128].rearrange("(k p) n -> p k n", p=128), writes=[WB1.bufs[k2]])
                        return (k, k2)
                    return f

                def t1_comp(c):
                    def f(kk):
                        k, k2 = kk
                        for j in range(4):
                            sl = slice(j * 512, (j + 1) * 512)
                            for n in range(3):
                                for kc in range(8):
                                    mm(psum[n][:], WG1.aps[k][:, kc, n, :], xT[:, kc, sl], kc == 0, kc == 7, [WG1.bufs[k]] + XT[4 * j:4 * j + 4], [PB[n]])
                                ACT([PB[n]], [SIG[n]], out=sig[n], in_=psum[n][:], func=AF.Sigmoid)
                            for n in range(3):
                                for kc in range(4):
                                    mm(psum[3 + n][:], WB1.aps[k2][:, n, kc, :], brT[:, 4 * n + kc, sl], kc == 0, kc == 3, [WB1.bufs[k2], BR[4 * n + kc]], [PB[3 + n]])
                            V("tensor_tensor", [PB[3], SIG[0]], [ACC], out=acc, in0=psum[3][:], in1=sig[0], op=ALU.mult)
                            V("tensor_tensor", [PB[4]], [SIG[1]], out=sig[1], in0=psum[4][:], in1=sig[1], op=ALU.mult)
                            V("tensor_tensor", [PB[5]], [SIG[2]], out=sig[2], in0=psum[5][:], in1=sig[2], op=ALU.mult)
                            V("tensor_tensor", [SIG[1]], [ACC], out=acc, in0=acc, in1=sig[1], op=ALU.add)
                            V("tensor_tensor", [ACC, SIG[2]], [MG[c]], out=mergedT[:, c, sl], in0=acc, in1=sig[2], op=ALU.add)
                    return f
                run_items([(t1_load(c), t1_comp(c)) for c in range(8)], 2)
                S.barrier()
                C2 = Carver(big, SCR_LO - 12 * T * 2, SCR_LO)
                woT = C2.alloc([8, D], BF16)
                lnw = C2.alloc([D], F32)
                lnb = C2.alloc([D], F32)
                st6 = C2.alloc([16, 2, 6], F32)
                mvs = [C2.alloc([8], F32) for _ in range(2)]
                xbs = [C2.alloc([D], BF16) for _ in range(2)]
                WO, LNP = Buf("wo"), Buf("lnp")
                LNBs = [Buf("lnb0"), Buf("lnb1")]
                ch_wo = S.chan("wo_%d" % S.uid())
                for hq in range(2):
                    S.dma("pool", ch_wo, woT[:, 4 * hq:4 * hq + 4, :], wout_d[l][512 * hq:512 * hq + 512, :].rearrange("(k p) n -> p k n", p=128), writes=[WO])
                S.dma("sp", ch_p, lnw, ln1w_d[l].partition_broadcast(128), writes=[LNP])
                S.dma("sp", ch_p, lnb, ln1b_d[l].partition_broadcast(128), writes=[LNP])
                def fa(t_):
                    ln_finish_a(t_, st6[:, t_, :, :], lnw, lnb, LNP, mvs[t_ % 2], xbs[t_ % 2], LNBs[t_ % 2])

                def fb(t_):
                    ln_finish_b(t_, 2 + (t_ % 2), xbs[t_ % 2], LNBs[t_ % 2])
                for tt in range(18):
                    if tt < 16:
                        bks = [(4 * (tt % 2)) + 0, (4 * (tt % 2)) + 1]
                        for hf in range(2):
                            for kc in range(8):
                                mm(psum[bks[hf]][:], mergedT[:, kc, tt * 128:(tt + 1) * 128], woT[:, kc, hf * 512:(hf + 1) * 512], kc == 0, kc == 7, [MG[kc], WO], [PB[bks[hf]]])
                            ln_pre(tt, hf, bks[hf], st6[:, tt, :, :])
                    if 0 <= tt - 2:
                        fb(tt - 2)
                    if 0 <= tt - 1 < 16:
                        fa(tt - 1)
                if dbg and s == 0 and l == 0:
                    dump("x1", xres, [16, D], F32, XR)
                S.barrier()
                if stop_after == "t1":
                    stop = True
                    break

                C = Carver(big, SCR_LO - 12 * T * 2, SBUF_BYTES)
                hT = C.alloc([32, 1024], BF16)
                HT = [Buf("hT%d" % i) for i in range(32)]
                WU = Slots(C, 3, [8, 128], "wu")
                WD = Slots(C, 3, [512], "wd")
                lnw = C.alloc([D], F32)
                lnb = C.alloc([D], F32)
                st6h = [C.alloc([8, 2, 6], F32) for _ in range(2)]
                LNQ = []
                mvs = [C.alloc([8], F32) for _ in range(2)]
                xbs = [C.alloc([D], BF16) for _ in range(2)]
                tmpR = [C.alloc([512], F32) for _ in range(2)]
                TR = [Buf("tr0"), Buf("tr1")]
                LNP = Buf("lnp2")
                LNBs = [Buf("lnb20"), Buf("lnb21")]
                S.dma("sp", ch_p, lnw, ln2w_d[l].partition_broadcast(128), writes=[LNP])
                S.dma("sp", ch_p, lnb, ln2b_d[l].partition_broadcast(128), writes=[LNP])
                for half in range(2):
                    t0 = half * 1024
                    its = []
                    for f in range(32):
                        def ldu(f=f):
                            return load_cols(WU, wup_d[l][:, f * 128:(f + 1) * 128].rearrange("(k p) n -> p k n", p=128), 128)

                        def cpu(k, f=f):
                            for jj in range(2):
                                b = jj
                                tsl = slice(t0 + jj * 512, t0 + (jj + 1) * 512)
                                for kc in range(8):
                                    mm(psum[b][:], WU.aps[k][:, kc, :], xT[:, kc, tsl], kc == 0, kc == 7,
                                       [WU.bufs[k]] + XT[(t0 + jj * 512) // 128:(t0 + jj * 512) // 128 + 4], [PB[b]])
                                ACT([PB[b]], [TR[jj]], out=tmpR[jj], in_=psum[b][:], func=AF.Relu)
                                V("tensor_tensor", [PB[b], TR[jj]], [HT[f]], out=hT[:, f, jj * 512:(jj + 1) * 512], in0=psum[b][:], in1=tmpR[jj], op=ALU.mult)
                            if f % 2 == 1 and LNQ:
                                LNQ.pop(0)()
                        its.append((ldu, cpu))
                    run_items(its, 2)
                    for chh in range(2):
                        its = []
                        for f in range(32):
                            def ldd(f=f, chh=chh):
                                k = WD.next()
                                S.dma("pool", WD.chs[k], WD.aps[k], wdn_d[l][f * 128:(f + 1) * 128, chh * 512:(chh + 1) * 512], writes=[WD.bufs[k]])
                                return k

                            def cpd(k, f=f):
                                for q in range(8):
                                    mm(psum[q][:], hT[:, f, q * 128:(q + 1) * 128], WD.aps[k], f == 0, f == 31, [HT[f], WD.bufs[k]], [PB[q]])
                            its.append((ldd, cpd))
                        run_items(its, 2)
                        for q in range(8):
                            ln_pre(half * 8 + q, chh, q, st6h[half][:, q, :, :])
                    for q in range(8):
                        tt = half * 8 + q

                        def lnfa(tt=tt, q=q, half=half):
                            ln_finish_a(tt, st6h[half][:, q, :, :], lnw, lnb, LNP, mvs[q % 2], xbs[q % 2], LNBs[q % 2])

                        def lnfb(tt=tt, q=q):
                            ln_finish_b(tt, 2 + q % 6, xbs[q % 2], LNBs[q % 2])
                        if q == 0:
                            LNQ.append(lnfa)
                        else:
                            prevb = LNQ.pop()
                            LNQ.append(lnfa)
                            LNQ.append(prevb)
                        LNQ.append(lnfb)
                while LNQ:
                    LNQ.pop(0)()
                if dbg and s == 0 and l == 0:
                    dump("x2", xres, [16, D], F32, XR)
                S.barrier()
            if stop:
                break
            for tt in range(16):
                out_toks.append(S.dma("sp", ch_o[tt % 2], out_d[s, tt * 128:(tt + 1) * 128, :], xres[:, tt, :], reads=[XR[tt]]))
            S.barrier()
        S.final_wait("sp", out_toks)
        S.emit()
    return nc, list(dbg_d.keys())


_CACHE = {}


def kernel(**inputs):
    n = 8
    x = np.ascontiguousarray(inputs["x"], dtype=np.float32)
    if "nc" not in _CACHE:
        _CACHE["nc"] = build()[0]
    nc = _CACHE["nc"]
    consts = make_consts()
    names = ["w_in", "gla_w_gate", "gla_b_gate", "diff_lambda", "diff_norm_w", "gla_norm_w", "hgrn_norm_w", "hgrn_lb",
             "w_branch", "w_out", "ln1_w", "ln1_b", "w_up", "w_down", "ln2_w", "ln2_b"]
    shared = {k: np.ascontiguousarray(inputs[k], dtype=np.float32) for k in names}
    in_maps = []
    for c in range(n):
        m = dict(shared)
        m["x"] = x[NSEQ * c:NSEQ * (c + 1)]
        m["consts"] = consts
        in_maps.append(m)
    res = run_bass_kernel_spmd(nc, in_maps, core_ids=list(range(n)))
    return np.concatenate([r["out"] for r in res.results], axis=0).astype(np.float32)
```

```python
import contextlib
import numpy as np
import concourse.bass as bass
import concourse.mybir as mybir
from concourse.bass_utils import run_bass_kernel_spmd

F32 = mybir.dt.float32
BF16 = mybir.dt.bfloat16
AF = mybir.ActivationFunctionType
ALU = mybir.AluOpType
AX = mybir.AxisListType

ENGS = ("pe", "act", "dve", "pool", "sp")


class Buf:
    __slots__ = ("name", "w", "r")

    def __init__(self, name):
        self.name = name
        self.w = None
        self.r = {}


class Chan:
    __slots__ = ("key", "cnt")

    def __init__(self, key):
        self.key = key
        self.cnt = 0


class Sched:
    def __init__(self, nc, stack):
        self.nc = nc
        self.stack = stack
        self.stream = {e: [] for e in ENGS}
        self.cnt = {e: 0 for e in ENGS}
        self.seen = {e: {} for e in ENGS}
        self.sems = {}
        for e in ENGS:
            self.sems[e] = stack.enter_context(nc.semaphore("s_" + e))
        self.chans = []
        self.same_sync = True
        self._uid = 0

    def uid(self):
        self._uid += 1
        return self._uid

    def chan(self, name):
        c = Chan("ch_" + name)
        self.sems[c.key] = self.stack.enter_context(self.nc.semaphore(c.key))
        self.chans.append(c)
        return c

    def _deps(self, reads, writes):
        deps = {}

        def add(t):
            if t is None:
                return
            k, v = t
            if deps.get(k, 0) < v:
                deps[k] = v
        for b in reads:
            add(b.w)
        for b in writes:
            add(b.w)
            for k, v in b.r.items():
                add((k, v))
        return deps

    def _filter(self, eng, deps):
        waits = []
        seen = self.seen[eng]
        for k, v in deps.items():
            if k == eng and (eng == "pe" or eng == "sp" or not self.same_sync):
                continue
            if seen.get(k, 0) >= v:
                continue
            seen[k] = v
            waits.append((k, v))
        return waits

    def _mark(self, tok, reads, writes):
        k, v = tok
        for b in reads:
            if b.r.get(k, 0) < v:
                b.r[k] = v
        for b in writes:
            b.w = tok
            b.r = {}

    def op(self, eng, meth, reads=(), writes=(), *args, **kw):
        fn = (meth, args, kw)
        deps = self._deps(reads, writes)
        waits = self._filter(eng, deps)
        self.cnt[eng] += 1
        tok = (eng, self.cnt[eng])
        self.stream[eng].append((waits, fn, eng, 1))
        self._mark(tok, reads, writes)
        return tok

    def dma(self, q, chan, out, in_, reads=(), writes=(), **kw):
        deps = self._deps(reads, writes)
        if chan.cnt > 0:
            if deps.get(chan.key, 0) < chan.cnt:
                deps[chan.key] = chan.cnt
        waits = self._filter(q, deps)
        chan.cnt += 16
        tok = (chan.key, chan.cnt)
        self.stream[q].append((waits, ("dma_start", (), dict(out=out, in_=in_, **kw)), chan.key, 16))
        self._mark(tok, reads, writes)
        return tok

    def barrier(self):
        snap = {e: self.cnt[e] for e in ENGS if self.cnt[e] > 0}
        for c in self.chans:
            if c.cnt > 0:
                snap[c.key] = c.cnt
        for e in ENGS:
            deps = {k: v for k, v in snap.items() if k != e}
            waits = self._filter(e, deps)
            if waits:
                self.stream[e].append((waits, None, None, 0))

    def final_wait(self, eng, toks):
        deps = {}
        for k, v in toks:
            if deps.get(k, 0) < v:
                deps[k] = v
        waits = self._filter(eng, deps)
        if waits:
            self.stream[eng].append((waits, None, None, 0))

    def emit(self):
        nc = self.nc
        sems = self.sems
        streams = self.stream

        def run(e, lst):
            for waits, fn, key, n in lst:
                for k, v in waits:
                    e.wait_ge(sems[k], v)
                if fn is not None:
                    meth, args, kw = fn
                    ins = getattr(e, meth)(*args, **kw)
                    ins.then_inc(sems[key], n)

        with nc.Block() as block:
            @block.tensor
            def _(e):
                run(e, streams["pe"])

            @block.scalar
            def _(e):
                run(e, streams["act"])

            @block.vector
            def _(e):
                run(e, streams["dve"])

            @block.gpsimd
            def _(e):
                run(e, streams["pool"])

            @block.sync
            def _(e):
                run(e, streams["sp"])

D = 1024
T = 2048
DEPTH = 2
NSEQ = 2
IN_W = 8208
DFF = 4096
ALPHA_C = float((2 * DEPTH) ** 0.25)
EPS = 1e-5
OFF_QA, OFF_KA, OFF_VA = 0, 512, 1024
OFF_QG, OFF_KG, OFF_VG, OFF_GLR, OFF_GOUT = 1536, 1792, 2048, 2560, 2576
OFF_QH, OFF_FH, OFF_IH, OFF_HOUT, OFF_MG = 3088, 3600, 4112, 4624, 5136
SBUF_BYTES = 212800
NTAB = 19


def _prod(s):
    r = 1
    for v in s:
        r *= v
    return r


class Carver:
    def __init__(self, big, lo, hi):
        self.big, self.off, self.hi = big, lo, hi

    def alloc(self, shape, dt):
        sz = 4 if dt == F32 else 2
        nb = _prod(shape) * sz
        nb = (nb + 63) // 64 * 64
        assert self.off + nb <= self.hi, ("SBUF carve overflow", self.off, nb, self.hi)
        ap = self.big[:, self.off // 2:(self.off + nb) // 2]
        self.off += nb
        if dt == F32:
            ap = ap.bitcast(F32)
        ap = ap[:, 0:_prod(shape)]
        if len(shape) == 2:
            ap = ap.rearrange("p (a b) -> p a b", a=shape[0])
        elif len(shape) == 3:
            ap = ap.rearrange("p (a b c) -> p a b c", a=shape[0], b=shape[1])
        return ap


def make_consts():
    c = np.zeros((128, 1024), np.float32)
    c[:, 0:128] = np.eye(128, dtype=np.float32)
    j = np.arange(128)[:, None]
    i = np.arange(128)[None, :]
    c[:, 128:256] = np.where(i < j, -30000.0, 0.0)
    c[:, 256:384] = ((i >= j) & ((i // 64) == (j // 64))).astype(np.float32)
    slopes = [2.0 ** (-8.0 * (h + 1) / 4) for h in range(4)]
    for h in range(4):
        for d in range(NTAB):
            c[:, 384 + h * NTAB + d] = slopes[h] * (128.0 * (d - 15) + np.arange(128))
    c[:, 461] = -0.5
    c[:, 512:1024] = 1.0
    c[:, 512:1024:64] = 0.0
    return c


def build(n_seq=NSEQ, n_layers=DEPTH, dbg=False, stop_after=None, only=None):
    nc = bass.Bass("TRN2", target_bir_lowering=False)
    dram = {}

    def din(name, shape):
        dram[name] = nc.dram_tensor(name, list(shape), F32, kind="ExternalInput").ap()
        return dram[name]
    x_d = din("x", [NSEQ, T, D])
    w_in_d = din("w_in", [DEPTH, D, IN_W])
    wg_d = din("gla_w_gate", [DEPTH, 16, 256])
    bg_d = din("gla_b_gate", [DEPTH, 256])
    dl_d = din("diff_lambda", [DEPTH, 4, 64])
    dnw_d = din("diff_norm_w", [DEPTH, 128])
    gnw_d = din("gla_norm_w", [DEPTH, 128])
    hnw_d = din("hgrn_norm_w", [DEPTH, 128])
    hlb_d = din("hgrn_lb", [DEPTH, 512])
    wbr_d = din("w_branch", [DEPTH, 3, 512, D])
    wout_d = din("w_out", [DEPTH, D, D])
    ln1w_d = din("ln1_w", [DEPTH, D])
    ln1b_d = din("ln1_b", [DEPTH, D])
    wup_d = din("w_up", [DEPTH, D, DFF])
    wdn_d = din("w_down", [DEPTH, DFF, D])
    ln2w_d = din("ln2_w", [DEPTH, D])
    ln2b_d = din("ln2_b", [DEPTH, D])
    cst_d = din("consts", [128, 1024])
    out_d = nc.dram_tensor("out", [NSEQ, T, D], F32, kind="ExternalOutput").ap()
    dbg_d = {}

    with contextlib.ExitStack() as st:
        S = Sched(nc, st)
        big = st.enter_context(nc.sbuf_tensor("big", [128, SBUF_BYTES // 2], BF16))
        psum = [st.enter_context(nc.psum_tensor("ps%d" % i, [128, 512], F32)) for i in range(8)]
        PB = [Buf("psum%d" % i) for i in range(8)]

        def psb(i):
            return psum[i][:].bitcast(BF16)

        P = Carver(big, 0, SBUF_BYTES)
        xres = P.alloc([16, D], F32)
        xT = P.alloc([8, T], BF16)
        cst = P.alloc([1024], F32)
        identb = P.alloc([128], BF16)
        maskb = P.alloc([128], BF16)
        prm = P.alloc([64], F32)
        brT = P.alloc([12, T], BF16)
        SCR_LO = P.off
        XR = [Buf("xres%d" % i) for i in range(16)]
        XT = [Buf("xT%d" % i) for i in range(16)]
        BR = [Buf("brT%d" % i) for i in range(12)]
        CST = Buf("cst")
        PRM = Buf("prm")
        cmask = cst[:, 256:384]
        rmask = cst[:, 512:1024]
        nhalf = cst[:, 461:462]

        def tab(h, d):
            i = 384 + h * NTAB + d + 15
            return cst[:, i:i + 1]

        ch_x = [S.chan("x%d" % i) for i in range(2)]
        ch_o = [S.chan("o%d" % i) for i in range(2)]
        ch_c = S.chan("c")
        ch_p = S.chan("p")
        ch_dbg = S.chan("dbg")
        out_toks = []

        def dump(name, ap, shape, dt, reads):
            if not dbg:
                return
            t = nc.dram_tensor("dbg_" + name, [128] + list(shape), dt, kind="ExternalOutput").ap()
            dbg_d[name] = t
            out_toks.append(S.dma("sp", ch_dbg, t, ap, reads=reads))

        def ACT(reads, writes, **kw):
            S.op("act", "activation", reads, writes, **kw)

        def V(meth, reads, writes, *a, **kw):
            S.op("dve", meth, reads, writes, *a, **kw)

        def G(meth, reads, writes, *a, **kw):
            S.op("pool", meth, reads, writes, *a, **kw)

        def mm(out, lhsT, rhs, start, stop, reads, writes):
            S.op("pe", "matmul", reads, writes, out, lhsT=lhsT, rhs=rhs, start=start, stop=stop, skip_group_check=True)

        def transp(out, in_, reads, writes):
            S.op("pe", "transpose", list(reads) + [CST], writes, out=out, in_=in_, identity=identb)

        S.dma("sp", ch_c, cst, cst_d, writes=[CST])
        V("tensor_copy", [CST], [CST], out=identb, in_=cst[:, 0:128])
        V("tensor_copy", [CST], [CST], out=maskb, in_=cst[:, 128:256])
        SC0 = Carver(big, SCR_LO, SBUF_BYTES)
        dlt = SC0.alloc([2, 4, 64], F32)
        dlp = SC0.alloc([2, 2, 64], F32)
        lbt = SC0.alloc([2, 4], F32)
        SETUP = Buf("setup")
        S.dma("sp", ch_p, dlt.rearrange("p l a b -> p (l a b)"),
              dl_d.rearrange("l a b -> (l a b)").partition_broadcast(128), writes=[SETUP])
        S.dma("sp", ch_p, lbt, hlb_d.rearrange("l (h p) -> p l h", p=128), writes=[SETUP], allow_slow_non_contiguous=True)
        for l in range(DEPTH):
            b = 32 * l
            S.dma("sp", ch_p, prm[:, b + 1:b + 2], dnw_d[l].unsqueeze(1), writes=[PRM])
            S.dma("sp", ch_p, prm[:, b + 2:b + 3], gnw_d[l].unsqueeze(1), writes=[PRM])
            S.dma("sp", ch_p, prm[:, b + 3:b + 4], hnw_d[l].unsqueeze(1), writes=[PRM])
            S.dma("sp", ch_p, prm[:, b + 4:b + 6], bg_d[l].rearrange("(g p) -> p g", p=128), writes=[PRM], allow_slow_non_contiguous=True)
        for l in range(DEPTH):
            b = 32 * l
            lam_init = 0.8 - 0.6 * float(np.exp(-0.3 * l))
            V("tensor_tensor", [SETUP], [SETUP], out=dlp[:, l, 0, :], in0=dlt[:, l, 0, :], in1=dlt[:, l, 1, :], op=ALU.mult)
            V("tensor_tensor", [SETUP], [SETUP], out=dlp[:, l, 1, :], in0=dlt[:, l, 2, :], in1=dlt[:, l, 3, :], op=ALU.mult)
            V("reduce_sum", [SETUP, PRM], [PRM], out=prm[:, b + 16:b + 18], in_=dlp[:, l, :, :], axis=AX.X)
            ACT([PRM], [PRM], out=prm[:, b + 18:b + 20], in_=prm[:, b + 16:b + 18], func=AF.Exp)
            V("scalar_tensor_tensor", [PRM], [PRM], out=prm[:, b + 0:b + 1], in0=prm[:, b + 19:b + 20], scalar=-lam_init, in1=prm[:, b + 18:b + 19], op0=ALU.add, op1=ALU.subtract)
            V("tensor_scalar", [PRM], [PRM], out=prm[:, b + 1:b + 2], in0=prm[:, b + 1:b + 2], scalar1=1.0 - lam_init, scalar2=None, op0=ALU.mult)
            V("tensor_scalar", [PRM], [PRM], out=prm[:, b + 4:b + 6], in0=prm[:, b + 4:b + 6], scalar1=-1.0, scalar2=None, op0=ALU.mult)
        V("memset", [PRM], [PRM], prm[:, 6:10], 1.0)
        V("memset", [PRM], [PRM], prm[:, 10:14], 1e-30)
        V("tensor_tensor", [SETUP, PRM], [PRM], out=prm[:, 48:52], in0=lbt[:, 1, :], in1=lbt[:, 0, :], op=ALU.subtract)
        ACT([PRM], [PRM], out=prm[:, 52:56], in_=prm[:, 48:52], func=AF.Sigmoid)
        V("tensor_scalar", [PRM], [PRM], out=prm[:, 38:42], in0=prm[:, 52:56], scalar1=-1.0, scalar2=1.0, op0=ALU.mult, op1=ALU.add)
        V("tensor_scalar", [PRM], [PRM], out=prm[:, 42:46], in0=prm[:, 52:56], scalar1=1e-30, scalar2=None, op0=ALU.max)
        S.barrier()

        PEND = []

        FL = {"mode": "gh"}

        def flush_pending(n=None):
            while PEND and (n is None or n > 0):
                PEND.pop(0)()
                if n is not None:
                    n -= 1

        class Slots:
            def __init__(self, carver, n, shape, name):
                self.aps = [carver.alloc(shape, BF16) for _ in range(n)]
                self.bufs = [Buf("%s%d" % (name, i)) for i in range(n)]
                self.chs = [S.chan("%s%d_%d" % (name, i, S.uid())) for i in range(n)]
                self.i = 0

            def next(self):
                k = self.i % len(self.aps)
                self.i += 1
                return k

        def run_items(items, depth):
            loads = [i for i, it in enumerate(items) if it[0] is not None]
            handles = {}
            state = {"p": 0, "out": 0}

            def pump():
                while state["p"] < len(loads) and state["out"] < depth:
                    i = loads[state["p"]]
                    handles[i] = items[i][0]()
                    state["p"] += 1
                    state["out"] += 1
            for i, it in enumerate(items):
                pump()
                it[1](handles.get(i))
                if it[0] is not None:
                    state["out"] -= 1

        def load_cols(slots, src3, ncols):
            k = slots.next()
            S.dma("pool", slots.chs[k], slots.aps[k][:, :, 0:ncols], src3, writes=[slots.bufs[k]])
            return k

        def win_cols(l, c0, n):
            return w_in_d[l][:, c0:c0 + n].rearrange("(k p) n -> p k n", p=128)

        def proj_fm(wap, wbuf, M, banks, evac):
            for j in range(4):
                b = banks[j % len(banks)]
                for kc in range(8):
                    mm(psum[b][0:M, :], wap[:, kc, 0:M], xT[:, kc, j * 512:(j + 1) * 512], kc == 0, kc == 7,
                       [wbuf] + XT[4 * j:4 * j + 4], [PB[b]])
                evac(j, b)
                if FL["mode"] == "gh1":
                    flush_pending(1)
                elif j == 1:
                    flush_pending()

        def proj_tm(wap, wbuf, N, banks, evac):
            for g in range(4):
                b = banks[g % len(banks)]
                for q in range(4):
                    tt = 4 * g + q
                    for kc in range(8):
                        mm(psum[b][:, q * 128:q * 128 + N], xT[:, kc, tt * 128:(tt + 1) * 128], wap[:, kc, 0:N], kc == 0, kc == 7,
                           [wbuf, XT[tt]], [PB[b]])
                evac(g, b)
                if FL["mode"] == "gh1":
                    flush_pending(1)
                elif g == 1:
                    flush_pending()

        def to_xT(tt, tbank, xb, XB):
            ACT([XR[tt]], [XB], out=xb, in_=xres[:, tt, :], func=AF.Copy)
            pv = psb(tbank)
            for k in range(8):
                transp(pv[:, k * 128:(k + 1) * 128], xb[:, k * 128:(k + 1) * 128], [XB], [PB[tbank]])
            ACT([PB[tbank]], [XT[tt]], out=xT[:, :, tt * 128:(tt + 1) * 128], in_=pv.rearrange("p (k n) -> p k n", k=8), func=AF.Copy)

        def ln_pre(tt, hf, bank, st6t):
            sl = slice(hf * 512, (hf + 1) * 512)
            xr = xres[:, tt, :]
            V("scalar_tensor_tensor", [PB[bank]], [XR[tt]], out=xr[:, sl], in0=xr[:, sl], scalar=ALPHA_C, in1=psum[bank][:], op0=ALU.mult, op1=ALU.add)
            V("bn_stats", [XR[tt]], [XR[tt]], out=st6t[:, hf, :], in_=xr[:, sl])

        def ln_finish_a(tt, st6t, lnw, lnb, LNP, mv, xb, LNB):
            xr = xres[:, tt, :]
            V("bn_aggr", [XR[tt]], [LNB], out=mv[:, 0:2], in_=st6t.rearrange("p a b -> p (a b)"))
            V("tensor_scalar", [LNB], [LNB], out=mv[:, 2:3], in0=mv[:, 1:2], scalar1=EPS, scalar2=None, op0=ALU.add)
            ACT([LNB], [LNB], out=mv[:, 3:4], in_=mv[:, 2:3], func=AF.Sqrt)
            V("reciprocal", [LNB], [LNB], out=mv[:, 4:5], in_=mv[:, 3:4])
            V("scalar_tensor_tensor", [LNB, LNP], [XR[tt]], out=xr, in0=xr, scalar=mv[:, 0:1], in1=lnw, op0=ALU.subtract, op1=ALU.mult)
            V("scalar_tensor_tensor", [LNB, LNP], [XR[tt]], out=xr, in0=xr, scalar=mv[:, 4:5], in1=lnb, op0=ALU.mult, op1=ALU.add)
            ACT([XR[tt]], [LNB], out=xb, in_=xres[:, tt, :], func=AF.Copy)

        def ln_finish_b(tt, tbank, xb, LNB):
            pv = psb(tbank)
            for k in range(8):
                transp(pv[:, k * 128:(k + 1) * 128], xb[:, k * 128:(k + 1) * 128], [LNB], [PB[tbank]])
            ACT([PB[tbank]], [XT[tt]], out=xT[:, :, tt * 128:(tt + 1) * 128], in_=pv.rearrange("p (k n) -> p k n", k=8), func=AF.Copy)

        for s in range(n_seq):
            C = Carver(big, SCR_LO, SBUF_BYTES)
            xb0 = C.alloc([D], BF16)
            XB0 = Buf("xb0")
            for tt in range(16):
                S.dma("sp", ch_x[tt % 2], xres[:, tt, :], x_d[s, tt * 128:(tt + 1) * 128, :], writes=[XR[tt]])
            for tt in range(16):
                to_xT(tt, tt % 2, xb0, XB0)
            S.barrier()
            stop = False
            for l in range(n_layers):
                pb = 32 * l
                C = Carver(big, SCR_LO, SBUF_BYTES)
                WS = Slots(C, 3, [8, 128], "ws")
                vbuf = [C.alloc([16, 130], BF16) for _ in range(2)]
                VB = [Buf("v0"), Buf("v1")]
                qT = C.alloc([T], BF16)
                kT = C.alloc([T], BF16)
                QT, KT = Buf("qT"), Buf("kT")
                sm = C.alloc([64], F32)
                SMB = [Buf("sm%d" % i) for i in range(4)]
                C_shared = C.off
                CA = Carver(big, C_shared, SBUF_BYTES)
                onb = CA.alloc([4, 128], BF16)
                ONBB = [Buf("onb%d" % i) for i in range(4)]
                qTp = CA.alloc([2, T], BF16)
                QTP = Buf("qTp")
                PT = [CA.alloc([512], BF16) for _ in range(3)]
                PTB = [Buf("pt%d" % i) for i in range(3)]
                ost = [CA.alloc([2, 129], F32) for _ in range(4)]
                OST = [Buf("ost%d" % i) for i in range(4)]
                o1 = [CA.alloc([128], F32) for _ in range(4)]
                sqj4 = [CA.alloc([128], F32) for _ in range(4)]
                OFB = [Buf("ofin%d" % i) for i in range(4)]
                CG = Carver(big, C_shared, SBUF_BYTES)
                cum = CG.alloc([T], F32)
                tmpA = CG.alloc([512], F32)
                tmpB = CG.alloc([512], F32)
                tmpC = CG.alloc([512], F32)
                khT0 = CG.alloc([512], BF16)
                khat = CG.alloc([16, 128], BF16)
                Sbf = CG.alloc([32, 128], BF16)
                Sf = CG.alloc([4, 128], F32)
                sgT = CG.alloc([T], BF16)
                elast = CG.alloc([32], F32)
                wgb = CG.alloc([256], BF16)
                PTg4 = CG.alloc([4, 128], BF16)
                onb4 = [CG.alloc([4, 128], BF16) for _ in range(2)]
                ONB4 = [Buf("onb4_0"), Buf("onb4_1")]
                sm4 = [sm[:, 0:16], sm[:, 16:32]]
                CUM, TA, TB, TC, KHT, KHAT, SBF, SGT, EL, GLR, WG, SQG = [Buf(n) for n in
                    ["cum", "tA", "tB", "tC", "khT", "khat", "sbf", "sgT", "elast", "glrT", "wgb", "sqg"]]
                glrT = sgT
                GLR = SGT
                SFB = [Buf("sf0"), Buf("sf1")]
                PTGB = Buf("ptg4")
                khT2 = [khT0, PTg4.rearrange("p a n -> p (a n)")]
                KHTB = [Buf("khT0"), PTGB]
                ch_wg = S.chan("wg_%d" % S.uid())

                for vb in range(2):
                    V("memset", [], [VB[vb]], vbuf[vb][:, :, 128:130], 1.0)
                V("memset", [], [QTP], qTp[64:128, 0, :], 0.0)
                V("memset", [], [QTP], qTp[0:64, 1, :], 0.0)

                items = []
                FL["mode"] = "gh"

                def evac_copy_bf16(dst, DB, eng):
                    def f(j, b):
                        if eng == "act":
                            ACT([PB[b]], [DB], out=dst[:, j * 512:(j + 1) * 512], in_=psum[b][:], func=AF.Copy)
                        else:
                            V("tensor_copy", [PB[b]], [DB], out=dst[:, j * 512:(j + 1) * 512], in_=psum[b][:])
                    return f

                def evac_v(vb):
                    def f(g, b):
                        ACT([PB[b]], [VB[vb]], out=vbuf[vb][:, 4 * g:4 * g + 4, 0:128], in_=psum[b][:].rearrange("p (a n) -> p a n", a=4), func=AF.Copy)
                    return f

                def rms_chain(src, SRC, qs, k0, junk):
                    SM = SMB[qs]
                    ACT([SRC], [SM], out=junk, in_=src, func=AF.Square, accum_out=sm[:, k0:k0 + 1])
                    V("tensor_scalar", [SM], [SM], out=sm[:, k0 + 1:k0 + 2], in0=sm[:, k0:k0 + 1], scalar1=1.0 / 128, scalar2=EPS, op0=ALU.mult, op1=ALU.add)
                    ACT([SM], [SM], out=sm[:, k0 + 2:k0 + 3], in_=sm[:, k0 + 1:k0 + 2], func=AF.Sqrt)
                    V("reciprocal", [SM], [SM], out=sm[:, k0 + 3:k0 + 4], in_=sm[:, k0 + 2:k0 + 3])
                    V("tensor_scalar", [SRC, SM], [ONBB[qs]], out=onb[:, qs, :], in0=src, scalar1=sm[:, k0 + 3:k0 + 4], scalar2=None, op0=ALU.mult)

                def onb_transpose(qs, tb):
                    transp(psb(tb)[:, qs * 128:(qs + 1) * 128], onb[:, qs, :], [ONBB[qs]], [PB[tb]])

                def finish_T(dst, DB, wcol, gate, tb=0):
                    src = psb(tb)[:, 0:512]
                    if gate is None:
                        V("tensor_scalar", [PB[tb], PRM], [DB], out=dst, in0=src, scalar1=wcol, scalar2=None, op0=ALU.mult)
                    else:
                        gap, GB = gate
                        V("scalar_tensor_tensor", [PB[tb], PRM, GB], [DB], out=dst, in0=src, scalar=wcol, in1=gap, op0=ALU.mult, op1=ALU.mult)

                def evac_qpad(j, b):
                    sl = slice(j * 512, (j + 1) * 512)
                    ACT([PB[b]], [QTP], out=qTp[0:64, 0, sl], in_=psum[b][0:64, :], func=AF.Copy)
                    V("tensor_copy", [PB[b]], [QTP], out=qTp[64:128, 1, sl], in_=psum[b][64:128, :])

                def attn_head(h):
                    hb = h
                    for qt in range(4):
                        steps = [(m, jb) for jb in range(4 * (qt + 1)) for m in (0, 1)]

                        def emit_S(idx):
                            m, jb = steps[idx]
                            bk = SBK[idx % 3]
                            r = jb - 4 * qt
                            c0 = 128 * r if r > 0 else 0
                            mm(psum[bk][:, c0:512], kT[:, jb * 128:(jb + 1) * 128], qTp[:, m, qt * 512 + c0:qt * 512 + 512],
                               True, r < 0, [KT, QTP], [PB[bk]])
                            if r >= 0:
                                mm(psum[bk][:, c0:c0 + 128], identb, maskb, False, True, [CST], [PB[bk]])

                        def emit_rest(idx):
                            m, jb = steps[idx]
                            bk = SBK[idx % 3]
                            pt = idx % 3
                            r = jb - 4 * qt
                            c0 = 128 * r if r > 0 else 0
                            if h == 0:
                                for u in range(2):
                                    a, bnd = max(c0, 256 * u), 256 * u + 256
                                    if a >= bnd:
                                        continue
                                    d = jb - 4 * qt - 2 * u
                                    ACT([PB[bk], CST], [PTB[pt]], out=PT[pt][:, a:bnd], in_=psum[bk][:, a:bnd], func=AF.Exp, bias=tab(0, d), scale=0.125)
                            else:
                                d = jb - 4 * qt
                                ACT([PB[bk], CST], [PTB[pt]], out=PT[pt][:, c0:512], in_=psum[bk][:, c0:512], func=AF.Exp, bias=tab(h, d), scale=0.125)
                            for qs in range(max(r, 0), 4):
                                ob = 4 + 2 * m + qs // 2
                                oc = (qs % 2) * 256
                                mm(psum[ob][:, oc:oc + 129], PT[pt][:, qs * 128:(qs + 1) * 128], vbuf[0][:, jb, 0:129],
                                   (jb == 0 and qs % 2 == 0), (jb == 4 * qt + qs), [PTB[pt], VB[0]], [PB[ob]])
                        n = len(steps)
                        SBK = [2, 3, 0]
                        emit_S(0)
                        emit_S(1)
                        for idx in range(n):
                            if idx + 2 < n:
                                emit_S(idx + 2)
                            emit_rest(idx)
                            if idx == 7:
                                flush_pending()
                        QS = range(4)
                        oc_ = lambda qs: (qs % 2) * 256
                        b0_ = lambda qs: 4 + qs // 2
                        b1_ = lambda qs: 6 + qs // 2
                        for bq in range(4):
                            src_o = psum[4 + bq][:].rearrange("p (a n) -> p a n", a=2)[:, :, 0:129]
                            if bq % 2 == 0:
                                ACT([PB[4 + bq]], [OST[bq]], out=ost[bq], in_=src_o, func=AF.Copy)
                            else:
                                V("tensor_copy", [PB[4 + bq]], [OST[bq]], out=ost[bq], in_=src_o)
                        s0_ = lambda qs: ost[qs // 2]
                        s1_ = lambda qs: ost[2 + qs // 2]
                        S0_ = lambda qs: OST[qs // 2]
                        S1_ = lambda qs: OST[2 + qs // 2]
                        for qs in QS:
                            V("reciprocal", [S0_(qs)], [SMB[qs]], out=sm[:, 8 * qs:8 * qs + 1], in_=s0_(qs)[:, qs % 2, 128:129])
                            V("reciprocal", [S1_(qs)], [SMB[qs]], out=sm[:, 8 * qs + 1:8 * qs + 2], in_=s1_(qs)[:, qs % 2, 128:129])
                        for qs in QS:
                            V("tensor_tensor", [SMB[qs], PRM], [SMB[qs]], out=sm[:, 8 * qs + 2:8 * qs + 3], in0=sm[:, 8 * qs + 1:8 * qs + 2], in1=prm[:, pb:pb + 1], op=ALU.mult)
                        for qs in QS:
                            V("tensor_scalar", [S0_(qs), SMB[qs]], [OFB[qs]], out=o1[qs], in0=s0_(qs)[:, qs % 2, 0:128], scalar1=sm[:, 8 * qs:8 * qs + 1], scalar2=None, op0=ALU.mult)
                        for qs in QS:
                            V("scalar_tensor_tensor", [S1_(qs), SMB[qs]], [OFB[qs]], out=o1[qs], in0=s1_(qs)[:, qs % 2, 0:128], scalar=sm[:, 8 * qs + 2:8 * qs + 3], in1=o1[qs], op0=ALU.mult, op1=ALU.add)
                        for qs in QS:
                            V("scalar_tensor_tensor", [OFB[qs]], [SMB[qs]], out=sqj4[qs], in0=o1[qs], scalar=1.0, in1=o1[qs], op0=ALU.mult, op1=ALU.mult, accum_out=sm[:, 8 * qs + 3:8 * qs + 4])
                        for qs in QS:
                            V("tensor_scalar", [SMB[qs]], [SMB[qs]], out=sm[:, 8 * qs + 4:8 * qs + 5], in0=sm[:, 8 * qs + 3:8 * qs + 4], scalar1=1.0 / 128, scalar2=EPS, op0=ALU.mult, op1=ALU.add)
                        for qs in QS:
                            S.op("pool", "tensor_tensor", [SMB[qs], CST], [SMB[qs]], out=sm[:, 8 * qs + 6:8 * qs + 7], in0=sm[:, 8 * qs + 4:8 * qs + 5], in1=nhalf, op=ALU.pow)
                        for qs in QS:
                            V("tensor_scalar", [OFB[qs], SMB[qs]], [ONBB[qs]], out=onb[:, qs, :], in0=o1[qs], scalar1=sm[:, 8 * qs + 6:8 * qs + 7], scalar2=None, op0=ALU.mult)

                        def deferred(qt=qt):
                            for qs in range(4):
                                transp(psb(1)[:, qs * 128:(qs + 1) * 128], onb[:, qs, :], [ONBB[qs]], [PB[1]])
                            finish_T(brT[:, hb, qt * 512:(qt + 1) * 512], BR[hb], prm[:, pb + 1:pb + 2], None, 1)
                        PEND.append(deferred)

                for h in range(4):
                    items.append((lambda h=h: load_cols(WS, win_cols(l, OFF_QA + h * 128, 128), 128),
                                  lambda k: proj_fm(WS.aps[k], WS.bufs[k], 128, [0, 1], evac_qpad)))
                    items.append((lambda h=h: load_cols(WS, win_cols(l, OFF_KA + h * 128, 128), 128),
                                  lambda k: proj_fm(WS.aps[k], WS.bufs[k], 128, [0, 1], evac_copy_bf16(kT, KT, "dve"))))
                    items.append((lambda h=h: load_cols(WS, win_cols(l, OFF_VA + h * 128, 128), 128),
                                  lambda k: proj_tm(WS.aps[k], WS.bufs[k], 128, [0, 1], evac_v(0))))
                    items.append((None, lambda k, h=h: attn_head(h)))

                def k_stage(j, kap, KB, sc):
                    sl = slice(j * 512, (j + 1) * 512)
                    ACT([CUM], [TC], out=tmpC, in_=cum[:, sl], func=AF.Exp, scale=-sc)
                    V("tensor_tensor", [KB, TC], [KT], out=kT[:, sl], in0=kap, in1=tmpC, op=ALU.mult)
                    if len(PEND) >= 2:
                        PEND.pop(0)()
                    V("tensor_tensor", [KT, EL], [KHTB[j % 2]], out=khT2[j % 2].rearrange("p (a b) -> p a b", a=8), in0=kT[:, sl].rearrange("p (a b) -> p a b", a=8),
                      in1=elast[:, 8 * j:8 * j + 8].unsqueeze(2).to_broadcast([128, 8, 64]), op=ALU.mult)
                    def dtr(j=j):
                        pv = psb(7)
                        for q in range(4):
                            transp(pv[:, q * 128:(q + 1) * 128], khT2[j % 2][:, q * 128:(q + 1) * 128], [KHTB[j % 2]], [PB[7]])
                        ACT([PB[7]], [KHAT], out=khat[:, 4 * j:4 * j + 4, :], in_=pv[:, 0:512].rearrange("p (a n) -> p a n", a=4), func=AF.Copy)
                    PEND.append(dtr)

                def gate_tail(j, csrc, CB, sc):
                    sl = slice(j * 512, (j + 1) * 512)
                    V("tensor_tensor_scan", [CB, CST], [CUM], out=cum[:, sl], data0=rmask, data1=csrc, initial=0.0, op0=ALU.mult, op1=ALU.add)
                    ACT([CUM], [EL], out=elast[:, 8 * j:8 * j + 8], in_=cum[:, j * 512 + 63:(j + 1) * 512:64], func=AF.Exp, scale=sc)

                def q_stage(sc, qscale):
                    def f(j, b):
                        sl = slice(j * 512, (j + 1) * 512)
                        ACT([CUM], [TA], out=tmpA, in_=cum[:, sl], func=AF.Exp, scale=sc)
                        V("scalar_tensor_tensor", [PB[b], TA], [QT], out=qT[:, sl], in0=psum[b][:], scalar=qscale, in1=tmpA, op0=ALU.mult, op1=ALU.mult)
                    return f

                def evac_silu(j, b):
                    ACT([PB[b]], [SGT], out=sgT[:, j * 512:(j + 1) * 512], in_=psum[b][:], func=AF.Silu)

                def unit_state(subs, mid=None):
                    flush_pending()
                    V("memset", [], [SBF], Sbf[:, 0, :], 0.0)
                    for g in range(4):
                        ubs = [4 + 2 * (g % 2), 5 + 2 * (g % 2)]
                        for cc in range(8):
                            c = 8 * g + cc
                            t, hf = c // 2, c % 2
                            ub = ubs[hf]
                            for (rb, nr, vb) in subs:
                                mm(psum[ub][rb:rb + nr, (cc // 2) * 128:(cc // 2 + 1) * 128], khat[64 * hf:64 * hf + 64, t, rb:rb + nr], vbuf[vb][64 * hf:64 * hf + 64, t, 0:128],
                                   True, True, [KHAT, VB[vb]], [PB[ub]])
                        if g == 1 and mid is not None:
                            mid()
                        for cc in range(8):
                            c = 8 * g + cc
                            if c == 31:
                                break
                            ub = ubs[c % 2]
                            usrc = psum[ub][:, (cc // 2) * 128:(cc // 2 + 1) * 128]
                            dstS = Sf[:, (c + 1) % 4, :]
                            DB_ = SFB[((c + 1) % 4) // 2]
                            if c == 0:
                                V("tensor_copy", [PB[ub]], [DB_], out=dstS, in_=usrc)
                            else:
                                V("scalar_tensor_tensor", [PB[ub], EL, SFB[(c % 4) // 2]], [DB_], out=dstS, in0=Sf[:, c % 4, :], scalar=elast[:, c:c + 1],
                                  in1=usrc, op0=ALU.mult, op1=ALU.add)
                            ACT([DB_], [SBF], out=Sbf[:, c + 1, :], in_=dstS, func=AF.Copy)
                def unit_out(sub, bi, nwcol):
                    rb, nr, vb = sub
                    sqbuf = [tmpB, tmpC]
                    SQB = [TB, TC]

                    def tail(j):
                        par = j % 2
                        tb = j % 2
                        for q in range(4):
                            transp(psb(tb)[:, q * 128:(q + 1) * 128], onb4[par][:, q, :], [ONB4[par]], [PB[tb]])
                        finish_T(brT[:, bi, j * 512:(j + 1) * 512], BR[bi], nwcol, (sgT[:, j * 512:(j + 1) * 512], SGT), tb)
                    for j in range(4):
                        par = j % 2
                        sbk, obk = (2, 3) if par == 0 else (4, 5)
                        for q in range(4):
                            t = 4 * j + q
                            tsl = slice(t * 128, (t + 1) * 128)
                            mm(psum[sbk][:, q * 128:(q + 1) * 128], kT[rb:rb + nr, tsl], qT[rb:rb + nr, tsl], True, True, [KT, QT], [PB[sbk]])
                        V("tensor_tensor", [PB[sbk], CST], [PTGB], out=PTg4, in0=psum[sbk][:].rearrange("p (a n) -> p a n", a=4),
                          in1=cmask.unsqueeze(1).to_broadcast([128, 4, 128]), op=ALU.mult)
                        if j > 0:
                            tail(j - 1)
                        for q in range(4):
                            t = 4 * j + q
                            csl = slice(q * 128, (q + 1) * 128)
                            mm(psum[obk][:, csl], PTg4[:, q, :], vbuf[vb][:, t, 0:128], True, False, [PTGB, VB[vb]], [PB[obk]])
                            for hf in range(2):
                                c = 2 * t + hf
                                mm(psum[obk][64 * hf:64 * hf + 64, csl], qT[rb:rb + nr, t * 128 + 64 * hf:t * 128 + 64 * hf + 64], Sbf[rb:rb + nr, c, :], False, hf == 1,
                                   [QT, SBF], [PB[obk]])
                        o3 = psum[obk][:].rearrange("p (a n) -> p a n", a=4)
                        s4 = sm4[par]
                        SMp = SMB[par]
                        ACT([PB[obk]], [SQB[par]], out=sqbuf[par], in_=psum[obk][:], func=AF.Square)
                        V("reduce_sum", [SQB[par]], [SMp], out=s4[:, 0:4], in_=sqbuf[par].rearrange("p (a n) -> p a n", a=4), axis=AX.X)
                        V("tensor_scalar", [SMp], [SMp], out=s4[:, 4:8], in0=s4[:, 0:4], scalar1=1.0 / 128, scalar2=EPS, op0=ALU.mult, op1=ALU.add)
                        S.op("pool", "tensor_tensor", [SMp, CST], [SMp], out=s4[:, 12:16], in0=s4[:, 4:8], in1=nhalf.to_broadcast([128, 4]), op=ALU.pow)
                        V("tensor_tensor", [PB[obk], SMp], [ONB4[par]], out=onb4[par], in0=o3, in1=s4[:, 12:16].unsqueeze(2).to_broadcast([128, 4, 128]), op=ALU.mult)
                    tail(3)

                def sg_item(c0):
                    return (lambda: load_cols(WS, win_cols(l, c0, 128), 128),
                            lambda k: proj_fm(WS.aps[k], WS.bufs[k], 128, [0, 1], evac_silu))

                def load_small(k):
                    S.dma("pool", ch_wg, wgb[0:16, :], wg_d[l], writes=[WG])
                items.append((None, load_small))

                def evac_glr(j, b):
                    ACT([PB[b]], [GLR], out=glrT[0:16, j * 512:(j + 1) * 512], in_=psum[b][0:16, :], func=AF.Copy)
                for g in range(2):
                    items.append((lambda: load_cols(WS, win_cols(l, OFF_GLR, 16), 16),
                                  lambda k: proj_fm(WS.aps[k], WS.bufs[k], 16, [0, 1], evac_glr)))
                    def gla_gate(k, g=g):
                        for j in range(4):
                            b = j % 2
                            mm(psum[b][:], wgb[0:16, g * 128:(g + 1) * 128], glrT[0:16, j * 512:(j + 1) * 512], True, True, [WG, GLR], [PB[b]])
                            ACT([PB[b], PRM], [TA], out=tmpA, in_=psum[b][:], func=AF.Exp, bias=prm[:, pb + 4 + g:pb + 5 + g], scale=-1.0)
                            ACT([TA], [TB], out=tmpB, in_=tmpA, func=AF.Ln, bias=1.0, scale=1.0)
                            gate_tail(j, tmpB, TB, -1.0 / 16)
                    items.append((None, gla_gate))
                    items.append((lambda g=g: load_cols(WS, win_cols(l, OFF_KG + g * 128, 128), 128),
                                  lambda k: proj_fm(WS.aps[k], WS.bufs[k], 128, [0, 1], lambda j, b: k_stage(j, psum[b][:], PB[b], -1.0 / 16))))
                    items.append((lambda g=g: load_cols(WS, win_cols(l, OFF_QG + g * 128, 128), 128),
                                  lambda k: proj_fm(WS.aps[k], WS.bufs[k], 128, [0, 1], q_stage(-1.0 / 16, 0.125))))
                    for si in range(2):
                        items.append((lambda g=g, si=si: load_cols(WS, win_cols(l, OFF_VG + (2 * g + si) * 128, 128), 128),
                                      lambda k, si=si: proj_tm(WS.aps[k], WS.bufs[k], 128, [0, 1], evac_v(si))))
                    gsubs = [(0, 64, 0), (64, 64, 1)]
                    items.append((lambda g=g: load_cols(WS, win_cols(l, OFF_GOUT + (2 * g) * 128, 128), 128),
                                  lambda k: unit_state(gsubs, lambda: proj_fm(WS.aps[k], WS.bufs[k], 128, [0, 1], evac_silu))))
                    items.append((None, lambda k, g=g: unit_out(gsubs[0], 4 + 2 * g, prm[:, pb + 2:pb + 3])))
                    items.append(sg_item(OFF_GOUT + (2 * g + 1) * 128))
                    items.append((None, lambda k, g=g: unit_out(gsubs[1], 4 + 2 * g + 1, prm[:, pb + 2:pb + 3])))
                for h in range(4):
                    def hg_gate(j, b, h=h):
                        sl = slice(j * 512, (j + 1) * 512)
                        oml = prm[:, pb + 6 + h:pb + 7 + h]
                        ACT([PB[b]], [TA], out=tmpA, in_=psum[b][:], func=AF.Sigmoid)
                        V("tensor_scalar", [TA, PRM], [TB], out=tmpB, in0=tmpA, scalar1=oml, scalar2=prm[:, pb + 10 + h:pb + 11 + h], op0=ALU.mult, op1=ALU.add)
                        ACT([TB], [CUM], out=cum[:, sl], in_=tmpB, func=AF.Ln)
                        ACT([PB[b]], [TC], out=tmpC, in_=psum[b][:], func=AF.Sigmoid, scale=-1.0)
                        V("tensor_scalar", [TC, PRM], [KT], out=kT[:, sl], in0=tmpC, scalar1=oml, scalar2=None, op0=ALU.mult)

                    def hg_item(k, hg_gate=hg_gate):
                        FL["mode"] = "gh1"
                        proj_fm(WS.aps[k], WS.bufs[k], 128, [0, 1], hg_gate)
                        for j in range(4):
                            sl = slice(j * 512, (j + 1) * 512)
                            V("tensor_tensor_scan", [CST], [CUM], out=cum[:, sl], data0=rmask, data1=cum[:, sl], initial=0.0, op0=ALU.mult, op1=ALU.add)
                        ACT([CUM], [EL], out=elast[:, 0:32], in_=cum[:, 63:T:64], func=AF.Exp, scale=1.0)

                        def kchain(j):
                            sl = slice(j * 512, (j + 1) * 512)
                            ACT([CUM], [TC], out=tmpC, in_=cum[:, sl], func=AF.Exp, scale=-1.0)
                            V("tensor_tensor", [TC], [KT], out=kT[:, sl], in0=kT[:, sl], in1=tmpC, op=ALU.mult)
                            V("tensor_tensor", [KT, EL], [KHTB[j % 2]], out=khT2[j % 2].rearrange("p (a b) -> p a b", a=8), in0=kT[:, sl].rearrange("p (a b) -> p a b", a=8),
                              in1=elast[:, 8 * j:8 * j + 8].unsqueeze(2).to_broadcast([128, 8, 64]), op=ALU.mult)

                        def ktr(j):
                            pv = psb(7)
                            for q in range(4):
                                transp(pv[:, q * 128:(q + 1) * 128], khT2[j % 2][:, q * 128:(q + 1) * 128], [KHTB[j % 2]], [PB[7]])
                            ACT([PB[7]], [KHAT], out=khat[:, 4 * j:4 * j + 4, :], in_=pv[:, 0:512].rearrange("p (a n) -> p a n", a=4), func=AF.Copy)
                        PEND.append(lambda: kchain(0))
                        PEND.append(lambda: (kchain(1), ktr(0)))
                        PEND.append(lambda: (kchain(2), ktr(1)))
                        PEND.append(lambda: (kchain(3), ktr(2)))
                        PEND.append(lambda: ktr(3))
                    items.append((lambda h=h: load_cols(WS, win_cols(l, OFF_FH + h * 128, 128), 128), hg_item))
                    items.append((lambda h=h: load_cols(WS, win_cols(l, OFF_QH + h * 128, 128), 128),
                                  lambda k: proj_fm(WS.aps[k], WS.bufs[k], 128, [0, 1], q_stage(1.0, 1.0))))
                    items.append((lambda h=h: load_cols(WS, win_cols(l, OFF_IH + h * 128, 128), 128),
                                  lambda k: proj_tm(WS.aps[k], WS.bufs[k], 128, [0, 1], evac_v(0))))
                    items.append((lambda h=h: load_cols(WS, win_cols(l, OFF_HOUT + h * 128, 128), 128),
                                  lambda k: unit_state([(0, 128, 0)], lambda: proj_fm(WS.aps[k], WS.bufs[k], 128, [0, 1], evac_silu))))
                    items.append((None, lambda k, h=h: unit_out((0, 128, 0), 8 + h, prm[:, pb + 3:pb + 4])))

                if only is not None:
                    items = [it for i, it in enumerate(items) if only(i)]
                run_items(items, 2)
                flush_pending()
                if dbg and s == 0 and l == 0:
                    dump("brT", brT, [12, T], BF16, BR)
                S.barrier()
                if stop_after == "mix":
                    stop = True
                    break

                C = Carver(big, SCR_LO, SBUF_BYTES)
                mergedT = C.alloc([8, T], BF16)
                MG = [Buf("mg%d" % i) for i in range(8)]
                WG1 = Slots(C, 2, [8, 3, 128], "wg1")
                WB1 = Slots(C, 2, [3, 4, 128], "wb1")
                sig = [C.alloc([512], F32) for _ in range(3)]
                SIG = [Buf("sig%d" % i) for i in range(3)]
                acc = C.alloc([512], F32)
                ACC = Buf("acc")

                def t1_load(c):
                    def f():
                        k = WG1.next()
                        for n in range(3):
                            S.dma("pool", WG1.chs[k], WG1.aps[k][:, :, n, :], win_cols(l, OFF_MG + n * 1024 + c * 128, 128), writes=[WG1.bufs[k]])
                        k2 = WB1.next()
                        for n in range(3):
                            S.dma("pool", WB1.chs[k2], WB1.aps[k2][:, n, :, :], wbr_d[l, n][:, c * 128:(c + 1) * 128].rearrange("(k p) n -> p k n", p=128), writes=[WB1.bufs[k2]])
                        return (k, k2)
                    return f

                def t1_comp(c):
                    def f(kk):
                        k, k2 = kk
                        for j in range(4):
                            sl = slice(j * 512, (j + 1) * 512)
                            for n in range(3):
                                for kc in range(8):
                                    mm(psum[n][:], WG1.aps[k][:, kc, n, :], xT[:, kc, sl], kc == 0, kc == 7, [WG1.bufs[k]] + XT[4 * j:4 * j + 4], [PB[n]])
                                ACT([PB[n]], [SIG[n]], out=sig[n], in_=psum[n][:], func=AF.Sigmoid)
                            for n in range(3):
                                for kc in range(4):
                                    mm(psum[3 + n][:], WB1.aps[k2][:, n, kc, :], brT[:, 4 * n + kc, sl], kc == 0, kc == 3, [WB1.bufs[k2], BR[4 * n + kc]], [PB[3 + n]])
                            V("tensor_tensor", [PB[3], SIG[0]], [ACC], out=acc, in0=psum[3][:], in1=sig[0], op=ALU.mult)
                            V("tensor_tensor", [PB[4]], [SIG[1]], out=sig[1], in0=psum[4][:], in1=sig[1], op=ALU.mult)
                            V("tensor_tensor", [PB[5]], [SIG[2]], out=sig[2], in0=psum[5][:], in1=sig[2], op=ALU.mult)
                            V("tensor_tensor", [SIG[1]], [ACC], out=acc, in0=acc, in1=sig[1], op=ALU.add)
                            V("tensor_tensor", [ACC, SIG[2]], [MG[c]], out=mergedT[:, c, sl], in0=acc, in1=sig[2], op=ALU.add)
                    return f
                run_items([(t1_load(c), t1_comp(c)) for c in range(8)], 2)
                S.barrier()
                C2 = Carver(big, SCR_LO - 12 * T * 2, SCR_LO)
                woT = C2.alloc([8, D], BF16)
                lnw = C2.alloc([D], F32)
                lnb = C2.alloc([D], F32)
                st6 = C2.alloc([16, 2, 6], F32)
                mvs = [C2.alloc([8], F32) for _ in range(2)]
                xbs = [C2.alloc([D], BF16) for _ in range(2)]
                WO, LNP = Buf("wo"), Buf("lnp")
                LNBs = [Buf("lnb0"), Buf("lnb1")]
                ch_wo = S.chan("wo_%d" % S.uid())
                for hq in range(2):
                    S.dma("pool", ch_wo, woT[:, 4 * hq:4 * hq + 4, :], wout_d[l][512 * hq:512 * hq + 512, :].rearrange("(k p) n -> p k n", p=128), writes=[WO])
                S.dma("sp", ch_p, lnw, ln1w_d[l].partition_broadcast(128), writes=[LNP])
                S.dma("sp", ch_p, lnb, ln1b_d[l].partition_broadcast(128), writes=[LNP])
                def fa(t_):
                    ln_finish_a(t_, st6[:, t_, :, :], lnw, lnb, LNP, mvs[t_ % 2], xbs[t_ % 2], LNBs[t_ % 2])

                def fb(t_):
                    ln_finish_b(t_, 2 + (t_ % 2), xbs[t_ % 2], LNBs[t_ % 2])
                for tt in range(18):
                    if tt < 16:
                        bks = [(4 * (tt % 2)) + 0, (4 * (tt % 2)) + 1]
                        for hf in range(2):
                            for kc in range(8):
                                mm(psum[bks[hf]][:], mergedT[:, kc, tt * 128:(tt + 1) * 128], woT[:, kc, hf * 512:(hf + 1) * 512], kc == 0, kc == 7, [MG[kc], WO], [PB[bks[hf]]])
                            ln_pre(tt, hf, bks[hf], st6[:, tt, :, :])
                    if 0 <= tt - 2:
                        fb(tt - 2)
                    if 0 <= tt - 1 < 16:
                        fa(tt - 1)
                if dbg and s == 0 and l == 0:
                    dump("x1", xres, [16, D], F32, XR)
                S.barrier()
                if stop_after == "t1":
                    stop = True
                    break

                C = Carver(big, SCR_LO - 12 * T * 2, SBUF_BYTES)
                hT = C.alloc([32, 1024], BF16)
                HT = [Buf("hT%d" % i) for i in range(32)]
                WU = Slots(C, 3, [8, 128], "wu")
                WD = Slots(C, 3, [512], "wd")
                lnw = C.alloc([D], F32)
                lnb = C.alloc([D], F32)
                st6h = [C.alloc([8, 2, 6], F32) for _ in range(2)]
                LNQ = []
                mvs = [C.alloc([8], F32) for _ in range(2)]
                xbs = [C.alloc([D], BF16) for _ in range(2)]
                tmpR = [C.alloc([512], F32) for _ in range(2)]
                TR = [Buf("tr0"), Buf("tr1")]
                LNP = Buf("lnp2")
                LNBs = [Buf("lnb20"), Buf("lnb21")]
                S.dma("sp", ch_p, lnw, ln2w_d[l].partition_broadcast(128), writes=[LNP])
                S.dma("sp", ch_p, lnb, ln2b_d[l].partition_broadcast(128), writes=[LNP])
                for half in range(2):
                    t0 = half * 1024
                    its = []
                    for f in range(32):
                        def ldu(f=f):
                            return load_cols(WU, wup_d[l][:, f * 128:(f + 1) * 128].rearrange("(k p) n -> p k n", p=128), 128)

                        def cpu(k, f=f):
                            for jj in range(2):
                                b = jj
                                tsl = slice(t0 + jj * 512, t0 + (jj + 1) * 512)
                                for kc in range(8):
                                    mm(psum[b][:], WU.aps[k][:, kc, :], xT[:, kc, tsl], kc == 0, kc == 7,
                                       [WU.bufs[k]] + XT[(t0 + jj * 512) // 128:(t0 + jj * 512) // 128 + 4], [PB[b]])
                                ACT([PB[b]], [TR[jj]], out=tmpR[jj], in_=psum[b][:], func=AF.Relu)
                                V("tensor_tensor", [PB[b], TR[jj]], [HT[f]], out=hT[:, f, jj * 512:(jj + 1) * 512], in0=psum[b][:], in1=tmpR[jj], op=ALU.mult)
                            if f % 2 == 1 and LNQ:
                                LNQ.pop(0)()
                        its.append((ldu, cpu))
                    run_items(its, 2)
                    for chh in range(2):
                        its = []
                        for f in range(32):
                            def ldd(f=f, chh=chh):
                                k = WD.next()
                                S.dma("pool", WD.chs[k], WD.aps[k], wdn_d[l][f * 128:(f + 1) * 128, chh * 512:(chh + 1) * 512], writes=[WD.bufs[k]])
                                return k

                            def cpd(k, f=f):
                                for q in range(8):
                                    mm(psum[q][:], hT[:, f, q * 128:(q + 1) * 128], WD.aps[k], f == 0, f == 31, [HT[f], WD.bufs[k]], [PB[q]])
                            its.append((ldd, cpd))
                        run_items(its, 2)
                        for q in range(8):
                            ln_pre(half * 8 + q, chh, q, st6h[half][:, q, :, :])
                    for q in range(8):
                        tt = half * 8 + q

                        def lnfa(tt=tt, q=q, half=half):
                            ln_finish_a(tt, st6h[half][:, q, :, :], lnw, lnb, LNP, mvs[q % 2], xbs[q % 2], LNBs[q % 2])
                            if l == n_layers - 1 and stop_after is None:
                                out_toks.append(S.dma("sp", ch_o[tt % 2], out_d[s, tt * 128:(tt + 1) * 128, :], xres[:, tt, :], reads=[XR[tt]]))

                        def lnfb(tt=tt, q=q):
                            ln_finish_b(tt, 2 + q % 6, xbs[q % 2], LNBs[q % 2])
                        if q == 0:
                            LNQ.append(lnfa)
                        else:
                            prevb = LNQ.pop()
                            LNQ.append(lnfa)
                            LNQ.append(prevb)
                        LNQ.append(lnfb)
                while LNQ:
                    LNQ.pop(0)()
                if dbg and s == 0 and l == 0:
                    dump("x2", xres, [16, D], F32, XR)
                S.barrier()
            if stop:
                break
            S.barrier()
        S.final_wait("sp", out_toks)
        S.emit()
    return nc, list(dbg_d.keys())


_CACHE = {}


def kernel(**inputs):
    n = 8
    x = np.ascontiguousarray(inputs["x"], dtype=np.float32)
    if "nc" not in _CACHE:
        _CACHE["nc"] = build()[0]
    nc = _CACHE["nc"]
    consts = make_consts()
    names = ["w_in", "gla_w_gate", "gla_b_gate", "diff_lambda", "diff_norm_w", "gla_norm_w", "hgrn_norm_w", "hgrn_lb",
             "w_branch", "w_out", "ln1_w", "ln1_b", "w_up", "w_down", "ln2_w", "ln2_b"]
    shared = {k: np.ascontiguousarray(inputs[k], dtype=np.float32) for k in names}
    in_maps = []
    for c in range(n):
        m = dict(shared)
        m["x"] = x[NSEQ * c:NSEQ * (c + 1)]
        m["consts"] = consts
        in_maps.append(m)
    res = run_bass_kernel_spmd(nc, in_maps, core_ids=list(range(n)))
    return np.concatenate([r["out"] for r in res.results], axis=0).astype(np.float32)
```

```python
import contextlib
import numpy as np
import concourse.bass as bass
import concourse.mybir as mybir
from concourse.bass_utils import run_bass_kernel_spmd

F32 = mybir.dt.float32
BF16 = mybir.dt.bfloat16
AF = mybir.ActivationFunctionType
ALU = mybir.AluOpType
AX = mybir.AxisListType

ENGS = ("pe", "act", "dve", "pool", "sp")


class Buf:
    __slots__ = ("name", "w", "r")

    def __init__(self, name):
        self.name = name
        self.w = None
        self.r = {}


class Chan:
    __slots__ = ("key", "cnt")

    def __init__(self, key):
        self.key = key
        self.cnt = 0


class Sched:
    def __init__(self, nc, stack):
        self.nc = nc
        self.stack = stack
        self.stream = {e: [] for e in ENGS}
        self.cnt = {e: 0 for e in ENGS}
        self.seen = {e: {} for e in ENGS}
        self.sems = {}
        for e in ENGS:
            self.sems[e] = stack.enter_context(nc.semaphore("s_" + e))
        self.chans = []
        self.same_sync = True
        self._uid = 0

    def uid(self):
        self._uid += 1
        return self._uid

    def chan(self, name):
        c = Chan("ch_" + name)
        self.sems[c.key] = self.stack.enter_context(self.nc.semaphore(c.key))
        self.chans.append(c)
        return c

    def _deps(self, reads, writes):
        deps = {}

        def add(t):
            if t is None:
                return
            k, v = t
            if deps.get(k, 0) < v:
                deps[k] = v
        for b in reads:
            add(b.w)
        for b in writes:
            add(b.w)
            for k, v in b.r.items():
                add((k, v))
        return deps

    def _filter(self, eng, deps):
        waits = []
        seen = self.seen[eng]
        for k, v in deps.items():
            if k == eng and (eng == "pe" or eng == "sp" or not self.same_sync):
                continue
            if seen.get(k, 0) >= v:
                continue
            seen[k] = v
            waits.append((k, v))
        return waits

    def _mark(self, tok, reads, writes):
        k, v = tok
        for b in reads:
            if b.r.get(k, 0) < v:
                b.r[k] = v
        for b in writes:
            b.w = tok
            b.r = {}

    def op(self, eng, meth, reads=(), writes=(), *args, **kw):
        fn = (meth, args, kw)
        deps = self._deps(reads, writes)
        waits = self._filter(eng, deps)
        self.cnt[eng] += 1
        tok = (eng, self.cnt[eng])
        self.stream[eng].append((waits, fn, eng, 1))
        self._mark(tok, reads, writes)
        return tok

    def dma(self, q, chan, out, in_, reads=(), writes=(), **kw):
        deps = self._deps(reads, writes)
        if chan.cnt > 0:
            if deps.get(chan.key, 0) < chan.cnt:
                deps[chan.key] = chan.cnt
        waits = self._filter(q, deps)
        chan.cnt += 16
        tok = (chan.key, chan.cnt)
        self.stream[q].append((waits, ("dma_start", (), dict(out=out, in_=in_, **kw)), chan.key, 16))
        self._mark(tok, reads, writes)
        return tok

    def barrier(self):
        snap = {e: self.cnt[e] for e in ENGS if self.cnt[e] > 0}
        for c in self.chans:
            if c.cnt > 0:
                snap[c.key] = c.cnt
        for e in ENGS:
            deps = {k: v for k, v in snap.items() if k != e}
            waits = self._filter(e, deps)
            if waits:
                self.stream[e].append((waits, None, None, 0))

    def final_wait(self, eng, toks):
        deps = {}
        for k, v in toks:
            if deps.get(k, 0) < v:
                deps[k] = v
        waits = self._filter(eng, deps)
        if waits:
            self.stream[eng].append((waits, None, None, 0))

    def emit(self):
        nc = self.nc
        sems = self.sems
        streams = self.stream

        def run(e, lst):
            for waits, fn, key, n in lst:
                for k, v in waits:
                    e.wait_ge(sems[k], v)
                if fn is not None:
                    meth, args, kw = fn
                    ins = getattr(e, meth)(*args, **kw)
                    ins.then_inc(sems[key], n)

        with nc.Block() as block:
            @block.tensor
            def _(e):
                run(e, streams["pe"])

            @block.scalar
            def _(e):
                run(e, streams["act"])

            @block.vector
            def _(e):
                run(e, streams["dve"])

            @block.gpsimd
            def _(e):
                run(e, streams["pool"])

            @block.sync
            def _(e):
                run(e, streams["sp"])

D = 1024
T = 2048
DEPTH = 2
NSEQ = 2
IN_W = 8208
DFF = 4096
ALPHA_C = float((2 * DEPTH) ** 0.25)
EPS = 1e-5
OFF_QA, OFF_KA, OFF_VA = 0, 512, 1024
OFF_QG, OFF_KG, OFF_VG, OFF_GLR, OFF_GOUT = 1536, 1792, 2048, 2560, 2576
OFF_QH, OFF_FH, OFF_IH, OFF_HOUT, OFF_MG = 3088, 3600, 4112, 4624, 5136
SBUF_BYTES = 212800
NTAB = 19


def _prod(s):
    r = 1
    for v in s:
        r *= v
    return r


class Carver:
    def __init__(self, big, lo, hi):
        self.big, self.off, self.hi = big, lo, hi

    def alloc(self, shape, dt):
        sz = 4 if dt == F32 else 2
        nb = _prod(shape) * sz
        nb = (nb + 63) // 64 * 64
        assert self.off + nb <= self.hi, ("SBUF carve overflow", self.off, nb, self.hi)
        ap = self.big[:, self.off // 2:(self.off + nb) // 2]
        self.off += nb
        if dt == F32:
            ap = ap.bitcast(F32)
        ap = ap[:, 0:_prod(shape)]
        if len(shape) == 2:
            ap = ap.rearrange("p (a b) -> p a b", a=shape[0])
        elif len(shape) == 3:
            ap = ap.rearrange("p (a b c) -> p a b c", a=shape[0], b=shape[1])
        return ap


def make_consts():
    c = np.zeros((128, 1024), np.float32)
    c[:, 0:128] = np.eye(128, dtype=np.float32)
    j = np.arange(128)[:, None]
    i = np.arange(128)[None, :]
    c[:, 128:256] = np.where(i < j, -30000.0, 0.0)
    c[:, 256:384] = ((i >= j) & ((i // 64) == (j // 64))).astype(np.float32)
    slopes = [2.0 ** (-8.0 * (h + 1) / 4) for h in range(4)]
    for h in range(4):
        for d in range(NTAB):
            c[:, 384 + h * NTAB + d] = slopes[h] * (128.0 * (d - 15) + np.arange(128))
    c[:, 461] = -0.5
    c[:, 512:1024] = 1.0
    c[:, 512:1024:64] = 0.0
    return c


def build(n_seq=NSEQ, n_layers=DEPTH, dbg=False, stop_after=None, only=None):
    nc = bass.Bass("TRN2", target_bir_lowering=False)
    dram = {}

    def din(name, shape):
        dram[name] = nc.dram_tensor(name, list(shape), F32, kind="ExternalInput").ap()
        return dram[name]
    x_d = din("x", [NSEQ, T, D])
    w_in_d = din("w_in", [DEPTH, D, IN_W])
    wg_d = din("gla_w_gate", [DEPTH, 16, 256])
    bg_d = din("gla_b_gate", [DEPTH, 256])
    dl_d = din("diff_lambda", [DEPTH, 4, 64])
    dnw_d = din("diff_norm_w", [DEPTH, 128])
    gnw_d = din("gla_norm_w", [DEPTH, 128])
    hnw_d = din("hgrn_norm_w", [DEPTH, 128])
    hlb_d = din("hgrn_lb", [DEPTH, 512])
    wbr_d = din("w_branch", [DEPTH, 3, 512, D])
    wout_d = din("w_out", [DEPTH, D, D])
    ln1w_d = din("ln1_w", [DEPTH, D])
    ln1b_d = din("ln1_b", [DEPTH, D])
    wup_d = din("w_up", [DEPTH, D, DFF])
    wdn_d = din("w_down", [DEPTH, DFF, D])
    ln2w_d = din("ln2_w", [DEPTH, D])
    ln2b_d = din("ln2_b", [DEPTH, D])
    cst_d = din("consts", [128, 1024])
    out_d = nc.dram_tensor("out", [NSEQ, T, D], F32, kind="ExternalOutput").ap()
    dbg_d = {}

    with contextlib.ExitStack() as st:
        S = Sched(nc, st)
        big = st.enter_context(nc.sbuf_tensor("big", [128, SBUF_BYTES // 2], BF16))
        psum = [st.enter_context(nc.psum_tensor("ps%d" % i, [128, 512], F32)) for i in range(8)]
        PB = [Buf("psum%d" % i) for i in range(8)]

        def psb(i):
            return psum[i][:].bitcast(BF16)

        P = Carver(big, 0, SBUF_BYTES)
        xres = P.alloc([16, D], F32)
        xT = P.alloc([8, T], BF16)
        cst = P.alloc([1024], F32)
        identb = P.alloc([128], BF16)
        maskb = P.alloc([128], BF16)
        prm = P.alloc([64], F32)
        brT = P.alloc([12, T], BF16)
        SCR_LO = P.off
        XR = [Buf("xres%d" % i) for i in range(16)]
        XT = [Buf("xT%d" % i) for i in range(16)]
        BR = [Buf("brT%d" % i) for i in range(12)]
        CST = Buf("cst")
        PRM = Buf("prm")
        cmask = cst[:, 256:384]
        rmask = cst[:, 512:1024]
        nhalf = cst[:, 461:462]

        def tab(h, d):
            i = 384 + h * NTAB + d + 15
            return cst[:, i:i + 1]

        ch_x = [S.chan("x%d" % i) for i in range(2)]
        ch_o = [S.chan("o%d" % i) for i in range(2)]
        ch_c = S.chan("c")
        ch_p = S.chan("p")
        ch_dbg = S.chan("dbg")
        out_toks = []

        def dump(name, ap, shape, dt, reads):
            if not dbg:
                return
            t = nc.dram_tensor("dbg_" + name, [128] + list(shape), dt, kind="ExternalOutput").ap()
            dbg_d[name] = t
            out_toks.append(S.dma("sp", ch_dbg, t, ap, reads=reads))

        def ACT(reads, writes, **kw):
            S.op("act", "activation", reads, writes, **kw)

        def V(meth, reads, writes, *a, **kw):
            S.op("dve", meth, reads, writes, *a, **kw)

        def G(meth, reads, writes, *a, **kw):
            S.op("pool", meth, reads, writes, *a, **kw)

        def mm(out, lhsT, rhs, start, stop, reads, writes):
            S.op("pe", "matmul", reads, writes, out, lhsT=lhsT, rhs=rhs, start=start, stop=stop, skip_group_check=True)

        def transp(out, in_, reads, writes):
            S.op("pe", "transpose", list(reads) + [CST], writes, out=out, in_=in_, identity=identb)

        S.dma("sp", ch_c, cst, cst_d, writes=[CST])
        V("tensor_copy", [CST], [CST], out=identb, in_=cst[:, 0:128])
        V("tensor_copy", [CST], [CST], out=maskb, in_=cst[:, 128:256])
        SC0 = Carver(big, SCR_LO, SBUF_BYTES)
        dlt = SC0.alloc([2, 4, 64], F32)
        dlp = SC0.alloc([2, 2, 64], F32)
        lbt = SC0.alloc([2, 4], F32)
        SETUP = Buf("setup")
        S.dma("sp", ch_p, dlt.rearrange("p l a b -> p (l a b)"),
              dl_d.rearrange("l a b -> (l a b)").partition_broadcast(128), writes=[SETUP])
        S.dma("sp", ch_p, lbt, hlb_d.rearrange("l (h p) -> p l h", p=128), writes=[SETUP], allow_slow_non_contiguous=True)
        for l in range(DEPTH):
            b = 32 * l
            S.dma("sp", ch_p, prm[:, b + 1:b + 2], dnw_d[l].unsqueeze(1), writes=[PRM])
            S.dma("sp", ch_p, prm[:, b + 2:b + 3], gnw_d[l].unsqueeze(1), writes=[PRM])
            S.dma("sp", ch_p, prm[:, b + 3:b + 4], hnw_d[l].unsqueeze(1), writes=[PRM])
            S.dma("sp", ch_p, prm[:, b + 4:b + 6], bg_d[l].rearrange("(g p) -> p g", p=128), writes=[PRM], allow_slow_non_contiguous=True)
        for l in range(DEPTH):
            b = 32 * l
            lam_init = 0.8 - 0.6 * float(np.exp(-0.3 * l))
            V("tensor_tensor", [SETUP], [SETUP], out=dlp[:, l, 0, :], in0=dlt[:, l, 0, :], in1=dlt[:, l, 1, :], op=ALU.mult)
            V("tensor_tensor", [SETUP], [SETUP], out=dlp[:, l, 1, :], in0=dlt[:, l, 2, :], in1=dlt[:, l, 3, :], op=ALU.mult)
            V("reduce_sum", [SETUP, PRM], [PRM], out=prm[:, b + 16:b + 18], in_=dlp[:, l, :, :], axis=AX.X)
            ACT([PRM], [PRM], out=prm[:, b + 18:b + 20], in_=prm[:, b + 16:b + 18], func=AF.Exp)
            V("scalar_tensor_tensor", [PRM], [PRM], out=prm[:, b + 0:b + 1], in0=prm[:, b + 19:b + 20], scalar=-lam_init, in1=prm[:, b + 18:b + 19], op0=ALU.add, op1=ALU.subtract)
            V("tensor_scalar", [PRM], [PRM], out=prm[:, b + 1:b + 2], in0=prm[:, b + 1:b + 2], scalar1=1.0 - lam_init, scalar2=None, op0=ALU.mult)
            V("tensor_scalar", [PRM], [PRM], out=prm[:, b + 4:b + 6], in0=prm[:, b + 4:b + 6], scalar1=-1.0, scalar2=None, op0=ALU.mult)
        V("memset", [PRM], [PRM], prm[:, 6:10], 1.0)
        V("memset", [PRM], [PRM], prm[:, 10:14], 1e-30)
        V("tensor_tensor", [SETUP, PRM], [PRM], out=prm[:, 48:52], in0=lbt[:, 1, :], in1=lbt[:, 0, :], op=ALU.subtract)
        ACT([PRM], [PRM], out=prm[:, 52:56], in_=prm[:, 48:52], func=AF.Sigmoid)
        V("tensor_scalar", [PRM], [PRM], out=prm[:, 38:42], in0=prm[:, 52:56], scalar1=-1.0, scalar2=1.0, op0=ALU.mult, op1=ALU.add)
        V("tensor_scalar", [PRM], [PRM], out=prm[:, 42:46], in0=prm[:, 52:56], scalar1=1e-30, scalar2=None, op0=ALU.max)
        S.barrier()

        PEND = []

        FL = {"mode": "gh"}

        def flush_pending(n=None):
            while PEND and (n is None or n > 0):
                PEND.pop(0)()
                if n is not None:
                    n -= 1

        class Slots:
            def __init__(self, carver, n, shape, name):
                self.aps = [carver.alloc(shape, BF16) for _ in range(n)]
                self.bufs = [Buf("%s%d" % (name, i)) for i in range(n)]
                self.chs = [S.chan("%s%d_%d" % (name, i, S.uid())) for i in range(n)]
                self.i = 0

            def next(self):
                k = self.i % len(self.aps)
                self.i += 1
                return k

        def run_items(items, depth):
            loads = [i for i, it in enumerate(items) if it[0] is not None]
            handles = {}
            state = {"p": 0, "out": 0}

            def pump():
                while state["p"] < len(loads) and state["out"] < depth:
                    i = loads[state["p"]]
                    handles[i] = items[i][0]()
                    state["p"] += 1
                    state["out"] += 1
            for i, it in enumerate(items):
                pump()
                it[1](handles.get(i))
                if it[0] is not None:
                    state["out"] -= 1

        def load_cols(slots, src3, ncols):
            k = slots.next()
            S.dma("pool", slots.chs[k], slots.aps[k][:, :, 0:ncols], src3, writes=[slots.bufs[k]])
            return k

        def win_cols(l, c0, n):
            return w_in_d[l][:, c0:c0 + n].rearrange("(k p) n -> p k n", p=128)

        def proj_fm(wap, wbuf, M, banks, evac):
            for j in range(4):
                b = banks[j % len(banks)]
                for kc in range(8):
                    mm(psum[b][0:M, :], wap[:, kc, 0:M], xT[:, kc, j * 512:(j + 1) * 512], kc == 0, kc == 7,
                       [wbuf] + XT[4 * j:4 * j + 4], [PB[b]])
                evac(j, b)
                if FL["mode"] == "gh1":
                    flush_pending(1)
                elif j == 1:
                    flush_pending()

        def proj_tm(wap, wbuf, N, banks, evac):
            for g in range(4):
                b = banks[g % len(banks)]
                for q in range(4):
                    tt = 4 * g + q
                    for kc in range(8):
                        mm(psum[b][:, q * 128:q * 128 + N], xT[:, kc, tt * 128:(tt + 1) * 128], wap[:, kc, 0:N], kc == 0, kc == 7,
                           [wbuf, XT[tt]], [PB[b]])
                evac(g, b)
                if FL["mode"] == "gh1":
                    flush_pending(1)
                elif g == 1:
                    flush_pending()

        def to_xT(tt, tbank, xb, XB):
            ACT([XR[tt]], [XB], out=xb, in_=xres[:, tt, :], func=AF.Copy)
            pv = psb(tbank)
            for k in range(8):
                transp(pv[:, k * 128:(k + 1) * 128], xb[:, k * 128:(k + 1) * 128], [XB], [PB[tbank]])
            ACT([PB[tbank]], [XT[tt]], out=xT[:, :, tt * 128:(tt + 1) * 128], in_=pv.rearrange("p (k n) -> p k n", k=8), func=AF.Copy)

        def ln_pre(tt, hf, bank, st6t, do_add=True, do_stats=True):
            sl = slice(hf * 512, (hf + 1) * 512)
            xr = xres[:, tt, :]
            if do_add:
                V("scalar_tensor_tensor", [PB[bank]], [XR[tt]], out=xr[:, sl], in0=xr[:, sl], scalar=ALPHA_C, in1=psum[bank][:], op0=ALU.mult, op1=ALU.add)
            if do_stats:
                V("bn_stats", [XR[tt]], [XR[tt]], out=st6t[:, hf, :], in_=xr[:, sl])

        def ln_finish_a(tt, st6t, lnw, lnb, LNP, mv, xb, LNB):
            xr = xres[:, tt, :]
            V("bn_aggr", [XR[tt]], [LNB], out=mv[:, 0:2], in_=st6t.rearrange("p a b -> p (a b)"))
            V("tensor_scalar", [LNB], [LNB], out=mv[:, 2:3], in0=mv[:, 1:2], scalar1=EPS, scalar2=None, op0=ALU.add)
            ACT([LNB], [LNB], out=mv[:, 3:4], in_=mv[:, 2:3], func=AF.Sqrt)
            V("reciprocal", [LNB], [LNB], out=mv[:, 4:5], in_=mv[:, 3:4])
            V("scalar_tensor_tensor", [LNB, LNP], [XR[tt]], out=xr, in0=xr, scalar=mv[:, 0:1], in1=lnw, op0=ALU.subtract, op1=ALU.mult)
            V("scalar_tensor_tensor", [LNB, LNP], [XR[tt]], out=xr, in0=xr, scalar=mv[:, 4:5], in1=lnb, op0=ALU.mult, op1=ALU.add)
            ACT([XR[tt]], [LNB], out=xb, in_=xres[:, tt, :], func=AF.Copy)

        def ln_finish_b(tt, tbank, xb, LNB):
            pv = psb(tbank)
            for k in range(8):
                transp(pv[:, k * 128:(k + 1) * 128], xb[:, k * 128:(k + 1) * 128], [LNB], [PB[tbank]])
            ACT([PB[tbank]], [XT[tt]], out=xT[:, :, tt * 128:(tt + 1) * 128], in_=pv.rearrange("p (k n) -> p k n", k=8), func=AF.Copy)

        for s in range(n_seq):
            C = Carver(big, SCR_LO, SBUF_BYTES)
            xb0 = C.alloc([D], BF16)
            XB0 = Buf("xb0")
            for tt in range(16):
                S.dma("sp", ch_x[tt % 2], xres[:, tt, :], x_d[s, tt * 128:(tt + 1) * 128, :], writes=[XR[tt]])
            for tt in range(16):
                to_xT(tt, tt % 2, xb0, XB0)
            S.barrier()
            stop = False
            for l in range(n_layers):
                pb = 32 * l
                C = Carver(big, SCR_LO, SBUF_BYTES)
                WS = Slots(C, 3, [8, 128], "ws")
                vbuf = [C.alloc([16, 130], BF16) for _ in range(2)]
                VB = [Buf("v0"), Buf("v1")]
                qT = C.alloc([T], BF16)
                kT = C.alloc([T], BF16)
                QT, KT = Buf("qT"), Buf("kT")
                sm = C.alloc([64], F32)
                SMB = [Buf("sm%d" % i) for i in range(4)]
                C_shared = C.off
                CA = Carver(big, C_shared, SBUF_BYTES)
                onb = CA.alloc([4, 128], BF16)
                ONBB = [Buf("onb%d" % i) for i in range(4)]
                qTp = CA.alloc([2, T], BF16)
                QTP = Buf("qTp")
                PT = [CA.alloc([512], BF16) for _ in range(3)]
                PTB = [Buf("pt%d" % i) for i in range(3)]
                ost = [CA.alloc([2, 129], F32) for _ in range(4)]
                OST = [Buf("ost%d" % i) for i in range(4)]
                o1 = [CA.alloc([128], F32) for _ in range(4)]
                sqj4 = [CA.alloc([128], F32) for _ in range(4)]
                OFB = [Buf("ofin%d" % i) for i in range(4)]
                CG = Carver(big, C_shared, SBUF_BYTES)
                cum = CG.alloc([T], F32)
                tmpA = CG.alloc([512], F32)
                tmpB = CG.alloc([512], F32)
                tmpC = CG.alloc([512], F32)
                khT0 = CG.alloc([512], BF16)
                khat = CG.alloc([16, 128], BF16)
                Sbf = CG.alloc([32, 128], BF16)
                Sf = CG.alloc([4, 128], F32)
                sgT = CG.alloc([T], BF16)
                elast = CG.alloc([32], F32)
                wgb = CG.alloc([256], BF16)
                PTg4 = CG.alloc([4, 128], BF16)
                onb4 = [CG.alloc([4, 128], BF16) for _ in range(2)]
                ONB4 = [Buf("onb4_0"), Buf("onb4_1")]
                sm4 = [sm[:, 0:16], sm[:, 16:32]]
                CUM, TA, TB, TC, KHT, KHAT, SBF, SGT, EL, GLR, WG, SQG = [Buf(n) for n in
                    ["cum", "tA", "tB", "tC", "khT", "khat", "sbf", "sgT", "elast", "glrT", "wgb", "sqg"]]
                glrT = sgT
                GLR = SGT
                SFB = [Buf("sf0"), Buf("sf1")]
                PTGB = Buf("ptg4")
                khT2 = [khT0, PTg4.rearrange("p a n -> p (a n)")]
                KHTB = [Buf("khT0"), PTGB]
                ch_wg = S.chan("wg_%d" % S.uid())

                for vb in range(2):
                    V("memset", [], [VB[vb]], vbuf[vb][:, :, 128:130], 1.0)
                V("memset", [], [QTP], qTp[64:128, 0, :], 0.0)
                V("memset", [], [QTP], qTp[0:64, 1, :], 0.0)

                items = []
                FL["mode"] = "gh"

                def evac_copy_bf16(dst, DB, eng):
                    def f(j, b):
                        if eng == "act":
                            ACT([PB[b]], [DB], out=dst[:, j * 512:(j + 1) * 512], in_=psum[b][:], func=AF.Copy)
                        else:
                            V("tensor_copy", [PB[b]], [DB], out=dst[:, j * 512:(j + 1) * 512], in_=psum[b][:])
                    return f

                def evac_v(vb):
                    def f(g, b):
                        ACT([PB[b]], [VB[vb]], out=vbuf[vb][:, 4 * g:4 * g + 4, 0:128], in_=psum[b][:].rearrange("p (a n) -> p a n", a=4), func=AF.Copy)
                    return f

                def rms_chain(src, SRC, qs, k0, junk):
                    SM = SMB[qs]
                    ACT([SRC], [SM], out=junk, in_=src, func=AF.Square, accum_out=sm[:, k0:k0 + 1])
                    V("tensor_scalar", [SM], [SM], out=sm[:, k0 + 1:k0 + 2], in0=sm[:, k0:k0 + 1], scalar1=1.0 / 128, scalar2=EPS, op0=ALU.mult, op1=ALU.add)
                    ACT([SM], [SM], out=sm[:, k0 + 2:k0 + 3], in_=sm[:, k0 + 1:k0 + 2], func=AF.Sqrt)
                    V("reciprocal", [SM], [SM], out=sm[:, k0 + 3:k0 + 4], in_=sm[:, k0 + 2:k0 + 3])
                    V("tensor_scalar", [SRC, SM], [ONBB[qs]], out=onb[:, qs, :], in0=src, scalar1=sm[:, k0 + 3:k0 + 4], scalar2=None, op0=ALU.mult)

                def onb_transpose(qs, tb):
                    transp(psb(tb)[:, qs * 128:(qs + 1) * 128], onb[:, qs, :], [ONBB[qs]], [PB[tb]])

                def finish_T(dst, DB, wcol, gate, tb=0):
                    src = psb(tb)[:, 0:512]
                    if gate is None:
                        V("tensor_scalar", [PB[tb], PRM], [DB], out=dst, in0=src, scalar1=wcol, scalar2=None, op0=ALU.mult)
                    else:
                        gap, GB = gate
                        V("scalar_tensor_tensor", [PB[tb], PRM, GB], [DB], out=dst, in0=src, scalar=wcol, in1=gap, op0=ALU.mult, op1=ALU.mult)

                def evac_qpad(j, b):
                    sl = slice(j * 512, (j + 1) * 512)
                    ACT([PB[b]], [QTP], out=qTp[0:64, 0, sl], in_=psum[b][0:64, :], func=AF.Copy)
                    V("tensor_copy", [PB[b]], [QTP], out=qTp[64:128, 1, sl], in_=psum[b][64:128, :])

                def attn_head(h):
                    hb = h
                    for qt in range(4):
                        steps = [(m, jb) for jb in range(4 * (qt + 1)) for m in (0, 1)]

                        def emit_S(idx):
                            m, jb = steps[idx]
                            bk = SBK[idx % 3]
                            r = jb - 4 * qt
                            c0 = 128 * r if r > 0 else 0
                            mm(psum[bk][:, c0:512], kT[:, jb * 128:(jb + 1) * 128], qTp[:, m, qt * 512 + c0:qt * 512 + 512],
                               True, r < 0, [KT, QTP], [PB[bk]])
                            if r >= 0:
                                mm(psum[bk][:, c0:c0 + 128], identb, maskb, False, True, [CST], [PB[bk]])

                        def emit_rest(idx):
                            m, jb = steps[idx]
                            bk = SBK[idx % 3]
                            pt = idx % 3
                            r = jb - 4 * qt
                            c0 = 128 * r if r > 0 else 0
                            if h == 0:
                                for u in range(2):
                                    a, bnd = max(c0, 256 * u), 256 * u + 256
                                    if a >= bnd:
                                        continue
                                    d = jb - 4 * qt - 2 * u
                                    ACT([PB[bk], CST], [PTB[pt]], out=PT[pt][:, a:bnd], in_=psum[bk][:, a:bnd], func=AF.Exp, bias=tab(0, d), scale=0.125)
                            else:
                                d = jb - 4 * qt
                                ACT([PB[bk], CST], [PTB[pt]], out=PT[pt][:, c0:512], in_=psum[bk][:, c0:512], func=AF.Exp, bias=tab(h, d), scale=0.125)
                            for qs in range(max(r, 0), 4):
                                ob = 4 + 2 * m + qs // 2
                                oc = (qs % 2) * 256
                                mm(psum[ob][:, oc:oc + 129], PT[pt][:, qs * 128:(qs + 1) * 128], vbuf[0][:, jb, 0:129],
                                   (jb == 0 and qs % 2 == 0), (jb == 4 * qt + qs), [PTB[pt], VB[0]], [PB[ob]])
                        n = len(steps)
                        SBK = [2, 3, 0]
                        emit_S(0)
                        emit_S(1)
                        for idx in range(n):
                            if idx + 2 < n:
                                emit_S(idx + 2)
                            emit_rest(idx)
                            if idx == 7:
                                flush_pending()
                        QS = range(4)
                        oc_ = lambda qs: (qs % 2) * 256
                        b0_ = lambda qs: 4 + qs // 2
                        b1_ = lambda qs: 6 + qs // 2
                        for bq in range(4):
                            src_o = psum[4 + bq][:].rearrange("p (a n) -> p a n", a=2)[:, :, 0:129]
                            if bq % 2 == 0:
                                ACT([PB[4 + bq]], [OST[bq]], out=ost[bq], in_=src_o, func=AF.Copy)
                            else:
                                V("tensor_copy", [PB[4 + bq]], [OST[bq]], out=ost[bq], in_=src_o)
                        s0_ = lambda qs: ost[qs // 2]
                        s1_ = lambda qs: ost[2 + qs // 2]
                        S0_ = lambda qs: OST[qs // 2]
                        S1_ = lambda qs: OST[2 + qs // 2]
                        for qs in QS:
                            V("reciprocal", [S0_(qs)], [SMB[qs]], out=sm[:, 8 * qs:8 * qs + 1], in_=s0_(qs)[:, qs % 2, 128:129])
                            V("reciprocal", [S1_(qs)], [SMB[qs]], out=sm[:, 8 * qs + 1:8 * qs + 2], in_=s1_(qs)[:, qs % 2, 128:129])
                        for qs in QS:
                            V("tensor_tensor", [SMB[qs], PRM], [SMB[qs]], out=sm[:, 8 * qs + 2:8 * qs + 3], in0=sm[:, 8 * qs + 1:8 * qs + 2], in1=prm[:, pb:pb + 1], op=ALU.mult)
                        for qs in QS:
                            V("tensor_scalar", [S0_(qs), SMB[qs]], [OFB[qs]], out=o1[qs], in0=s0_(qs)[:, qs % 2, 0:128], scalar1=sm[:, 8 * qs:8 * qs + 1], scalar2=None, op0=ALU.mult)
                        for qs in QS:
                            V("scalar_tensor_tensor", [S1_(qs), SMB[qs]], [OFB[qs]], out=o1[qs], in0=s1_(qs)[:, qs % 2, 0:128], scalar=sm[:, 8 * qs + 2:8 * qs + 3], in1=o1[qs], op0=ALU.mult, op1=ALU.add)
                        for qs in QS:
                            V("scalar_tensor_tensor", [OFB[qs]], [SMB[qs]], out=sqj4[qs], in0=o1[qs], scalar=1.0, in1=o1[qs], op0=ALU.mult, op1=ALU.mult, accum_out=sm[:, 8 * qs + 3:8 * qs + 4])
                        for qs in QS:
                            V("tensor_scalar", [SMB[qs]], [SMB[qs]], out=sm[:, 8 * qs + 4:8 * qs + 5], in0=sm[:, 8 * qs + 3:8 * qs + 4], scalar1=1.0 / 128, scalar2=EPS, op0=ALU.mult, op1=ALU.add)
                        for qs in QS:
                            S.op("pool", "tensor_tensor", [SMB[qs], CST], [SMB[qs]], out=sm[:, 8 * qs + 6:8 * qs + 7], in0=sm[:, 8 * qs + 4:8 * qs + 5], in1=nhalf, op=ALU.pow)
                        for qs in QS:
                            V("tensor_scalar", [OFB[qs], SMB[qs]], [ONBB[qs]], out=onb[:, qs, :], in0=o1[qs], scalar1=sm[:, 8 * qs + 6:8 * qs + 7], scalar2=None, op0=ALU.mult)

                        def deferred(qt=qt):
                            for qs in range(4):
                                transp(psb(1)[:, qs * 128:(qs + 1) * 128], onb[:, qs, :], [ONBB[qs]], [PB[1]])
                            finish_T(brT[:, hb, qt * 512:(qt + 1) * 512], BR[hb], prm[:, pb + 1:pb + 2], None, 1)
                        PEND.append(deferred)

                for h in range(4):
                    items.append((lambda h=h: load_cols(WS, win_cols(l, OFF_QA + h * 128, 128), 128),
                                  lambda k: proj_fm(WS.aps[k], WS.bufs[k], 128, [0, 1], evac_qpad)))
                    items.append((lambda h=h: load_cols(WS, win_cols(l, OFF_KA + h * 128, 128), 128),
                                  lambda k: proj_fm(WS.aps[k], WS.bufs[k], 128, [0, 1], evac_copy_bf16(kT, KT, "dve"))))
                    items.append((lambda h=h: load_cols(WS, win_cols(l, OFF_VA + h * 128, 128), 128),
                                  lambda k: proj_tm(WS.aps[k], WS.bufs[k], 128, [0, 1], evac_v(0))))
                    items.append((None, lambda k, h=h: attn_head(h)))

                def k_stage(j, kap, KB, sc):
                    sl = slice(j * 512, (j + 1) * 512)
                    ACT([CUM], [TC], out=tmpC, in_=cum[:, sl], func=AF.Exp, scale=-sc)
                    V("tensor_tensor", [KB, TC], [KT], out=kT[:, sl], in0=kap, in1=tmpC, op=ALU.mult)
                    if len(PEND) >= 2:
                        PEND.pop(0)()
                    V("tensor_tensor", [KT, EL], [KHTB[j % 2]], out=khT2[j % 2].rearrange("p (a b) -> p a b", a=8), in0=kT[:, sl].rearrange("p (a b) -> p a b", a=8),
                      in1=elast[:, 8 * j:8 * j + 8].unsqueeze(2).to_broadcast([128, 8, 64]), op=ALU.mult)
                    def dtr(j=j):
                        pv = psb(7)
                        for q in range(4):
                            transp(pv[:, q * 128:(q + 1) * 128], khT2[j % 2][:, q * 128:(q + 1) * 128], [KHTB[j % 2]], [PB[7]])
                        ACT([PB[7]], [KHAT], out=khat[:, 4 * j:4 * j + 4, :], in_=pv[:, 0:512].rearrange("p (a n) -> p a n", a=4), func=AF.Copy)
                    PEND.append(dtr)

                def gate_tail(j, csrc, CB, sc):
                    sl = slice(j * 512, (j + 1) * 512)
                    V("tensor_tensor_scan", [CB, CST], [CUM], out=cum[:, sl], data0=rmask, data1=csrc, initial=0.0, op0=ALU.mult, op1=ALU.add)
                    ACT([CUM], [EL], out=elast[:, 8 * j:8 * j + 8], in_=cum[:, j * 512 + 63:(j + 1) * 512:64], func=AF.Exp, scale=sc)

                def q_stage(sc, qscale):
                    def f(j, b):
                        sl = slice(j * 512, (j + 1) * 512)
                        ACT([CUM], [TA], out=tmpA, in_=cum[:, sl], func=AF.Exp, scale=sc)
                        V("scalar_tensor_tensor", [PB[b], TA], [QT], out=qT[:, sl], in0=psum[b][:], scalar=qscale, in1=tmpA, op0=ALU.mult, op1=ALU.mult)
                    return f

                def evac_silu(j, b):
                    ACT([PB[b]], [SGT], out=sgT[:, j * 512:(j + 1) * 512], in_=psum[b][:], func=AF.Silu)

                def unit_state(subs, mid=None):
                    flush_pending()
                    V("memset", [], [SBF], Sbf[:, 0, :], 0.0)
                    for g in range(4):
                        ubs = [4 + 2 * (g % 2), 5 + 2 * (g % 2)]
                        for cc in range(8):
                            c = 8 * g + cc
                            t, hf = c // 2, c % 2
                            ub = ubs[hf]
                            for (rb, nr, vb) in subs:
                                mm(psum[ub][rb:rb + nr, (cc // 2) * 128:(cc // 2 + 1) * 128], khat[64 * hf:64 * hf + 64, t, rb:rb + nr], vbuf[vb][64 * hf:64 * hf + 64, t, 0:128],
                                   True, True, [KHAT, VB[vb]], [PB[ub]])
                        if g == 1 and mid is not None:
                            mid()
                        for cc in range(8):
                            c = 8 * g + cc
                            if c == 31:
                                break
                            ub = ubs[c % 2]
                            usrc = psum[ub][:, (cc // 2) * 128:(cc // 2 + 1) * 128]
                            dstS = Sf[:, (c + 1) % 4, :]
                            DB_ = SFB[((c + 1) % 4) // 2]
                            if c == 0:
                                V("tensor_copy", [PB[ub]], [DB_], out=dstS, in_=usrc)
                            else:
                                V("scalar_tensor_tensor", [PB[ub], EL, SFB[(c % 4) // 2]], [DB_], out=dstS, in0=Sf[:, c % 4, :], scalar=elast[:, c:c + 1],
                                  in1=usrc, op0=ALU.mult, op1=ALU.add)
                            ACT([DB_], [SBF], out=Sbf[:, c + 1, :], in_=dstS, func=AF.Copy)
                def unit_out(sub, bi, nwcol):
                    rb, nr, vb = sub
                    sqbuf = [tmpB, tmpC]
                    SQB = [TB, TC]

                    def tail(j):
                        par = j % 2
                        tb = j % 2
                        for q in range(4):
                            transp(psb(tb)[:, q * 128:(q + 1) * 128], onb4[par][:, q, :], [ONB4[par]], [PB[tb]])
                        finish_T(brT[:, bi, j * 512:(j + 1) * 512], BR[bi], nwcol, (sgT[:, j * 512:(j + 1) * 512], SGT), tb)
                    for j in range(4):
                        par = j % 2
                        sbk, obk = (2, 3) if par == 0 else (4, 5)
                        for q in range(4):
                            t = 4 * j + q
                            tsl = slice(t * 128, (t + 1) * 128)
                            mm(psum[sbk][:, q * 128:(q + 1) * 128], kT[rb:rb + nr, tsl], qT[rb:rb + nr, tsl], True, True, [KT, QT], [PB[sbk]])
                        V("tensor_tensor", [PB[sbk], CST], [PTGB], out=PTg4, in0=psum[sbk][:].rearrange("p (a n) -> p a n", a=4),
                          in1=cmask.unsqueeze(1).to_broadcast([128, 4, 128]), op=ALU.mult)
                        if j > 0:
                            tail(j - 1)
                        for q in range(4):
                            t = 4 * j + q
                            csl = slice(q * 128, (q + 1) * 128)
                            mm(psum[obk][:, csl], PTg4[:, q, :], vbuf[vb][:, t, 0:128], True, False, [PTGB, VB[vb]], [PB[obk]])
                            for hf in range(2):
                                c = 2 * t + hf
                                mm(psum[obk][64 * hf:64 * hf + 64, csl], qT[rb:rb + nr, t * 128 + 64 * hf:t * 128 + 64 * hf + 64], Sbf[rb:rb + nr, c, :], False, hf == 1,
                                   [QT, SBF], [PB[obk]])
                        o3 = psum[obk][:].rearrange("p (a n) -> p a n", a=4)
                        s4 = sm4[par]
                        SMp = SMB[par]
                        ACT([PB[obk]], [SQB[par]], out=sqbuf[par], in_=psum[obk][:], func=AF.Square)
                        V("reduce_sum", [SQB[par]], [SMp], out=s4[:, 0:4], in_=sqbuf[par].rearrange("p (a n) -> p a n", a=4), axis=AX.X)
                        V("tensor_scalar", [SMp], [SMp], out=s4[:, 4:8], in0=s4[:, 0:4], scalar1=1.0 / 128, scalar2=EPS, op0=ALU.mult, op1=ALU.add)
                        S.op("pool", "tensor_tensor", [SMp, CST], [SMp], out=s4[:, 12:16], in0=s4[:, 4:8], in1=nhalf.to_broadcast([128, 4]), op=ALU.pow)
                        V("tensor_tensor", [PB[obk], SMp], [ONB4[par]], out=onb4[par], in0=o3, in1=s4[:, 12:16].unsqueeze(2).to_broadcast([128, 4, 128]), op=ALU.mult)
                    tail(3)

                def sg_item(c0):
                    return (lambda: load_cols(WS, win_cols(l, c0, 128), 128),
                            lambda k: proj_fm(WS.aps[k], WS.bufs[k], 128, [0, 1], evac_silu))

                def load_small(k):
                    S.dma("pool", ch_wg, wgb[0:16, :], wg_d[l], writes=[WG])
                items.append((None, load_small))

                def evac_glr(j, b):
                    ACT([PB[b]], [GLR], out=glrT[0:16, j * 512:(j + 1) * 512], in_=psum[b][0:16, :], func=AF.Copy)
                for g in range(2):
                    items.append((lambda: load_cols(WS, win_cols(l, OFF_GLR, 16), 16),
                                  lambda k: proj_fm(WS.aps[k], WS.bufs[k], 16, [0, 1], evac_glr)))
                    def gla_gate(k, g=g):
                        for j in range(4):
                            b = j % 2
                            mm(psum[b][:], wgb[0:16, g * 128:(g + 1) * 128], glrT[0:16, j * 512:(j + 1) * 512], True, True, [WG, GLR], [PB[b]])
                            ACT([PB[b], PRM], [TA], out=tmpA, in_=psum[b][:], func=AF.Exp, bias=prm[:, pb + 4 + g:pb + 5 + g], scale=-1.0)
                            ACT([TA], [TB], out=tmpB, in_=tmpA, func=AF.Ln, bias=1.0, scale=1.0)
                            gate_tail(j, tmpB, TB, -1.0 / 16)
                    items.append((None, gla_gate))
                    items.append((lambda g=g: load_cols(WS, win_cols(l, OFF_KG + g * 128, 128), 128),
                                  lambda k: proj_fm(WS.aps[k], WS.bufs[k], 128, [0, 1], lambda j, b: k_stage(j, psum[b][:], PB[b], -1.0 / 16))))
                    items.append((lambda g=g: load_cols(WS, win_cols(l, OFF_QG + g * 128, 128), 128),
                                  lambda k: proj_fm(WS.aps[k], WS.bufs[k], 128, [0, 1], q_stage(-1.0 / 16, 0.125))))
                    for si in range(2):
                        items.append((lambda g=g, si=si: load_cols(WS, win_cols(l, OFF_VG + (2 * g + si) * 128, 128), 128),
                                      lambda k, si=si: proj_tm(WS.aps[k], WS.bufs[k], 128, [0, 1], evac_v(si))))
                    gsubs = [(0, 64, 0), (64, 64, 1)]
                    items.append((lambda g=g: load_cols(WS, win_cols(l, OFF_GOUT + (2 * g) * 128, 128), 128),
                                  lambda k: unit_state(gsubs, lambda: proj_fm(WS.aps[k], WS.bufs[k], 128, [0, 1], evac_silu))))
                    items.append((None, lambda k, g=g: unit_out(gsubs[0], 4 + 2 * g, prm[:, pb + 2:pb + 3])))
                    items.append(sg_item(OFF_GOUT + (2 * g + 1) * 128))
                    items.append((None, lambda k, g=g: unit_out(gsubs[1], 4 + 2 * g + 1, prm[:, pb + 2:pb + 3])))
                for h in range(4):
                    def hg_gate(j, b, h=h):
                        sl = slice(j * 512, (j + 1) * 512)
                        oml = prm[:, pb + 6 + h:pb + 7 + h]
                        ACT([PB[b]], [TA], out=tmpA, in_=psum[b][:], func=AF.Sigmoid)
                        V("tensor_scalar", [TA, PRM], [TB], out=tmpB, in0=tmpA, scalar1=oml, scalar2=prm[:, pb + 10 + h:pb + 11 + h], op0=ALU.mult, op1=ALU.add)
                        ACT([TB], [CUM], out=cum[:, sl], in_=tmpB, func=AF.Ln)
                        ACT([PB[b]], [TC], out=tmpC, in_=psum[b][:], func=AF.Sigmoid, scale=-1.0)
                        V("tensor_scalar", [TC, PRM], [KT], out=kT[:, sl], in0=tmpC, scalar1=oml, scalar2=None, op0=ALU.mult)

                    def hg_item(k, hg_gate=hg_gate):
                        FL["mode"] = "gh1"
                        proj_fm(WS.aps[k], WS.bufs[k], 128, [0, 1], hg_gate)
                        for j in range(4):
                            sl = slice(j * 512, (j + 1) * 512)
                            V("tensor_tensor_scan", [CST], [CUM], out=cum[:, sl], data0=rmask, data1=cum[:, sl], initial=0.0, op0=ALU.mult, op1=ALU.add)
                        ACT([CUM], [EL], out=elast[:, 0:32], in_=cum[:, 63:T:64], func=AF.Exp, scale=1.0)

                        def kchain(j):
                            sl = slice(j * 512, (j + 1) * 512)
                            ACT([CUM], [TC], out=tmpC, in_=cum[:, sl], func=AF.Exp, scale=-1.0)
                            V("tensor_tensor", [TC], [KT], out=kT[:, sl], in0=kT[:, sl], in1=tmpC, op=ALU.mult)
                            V("tensor_tensor", [KT, EL], [KHTB[j % 2]], out=khT2[j % 2].rearrange("p (a b) -> p a b", a=8), in0=kT[:, sl].rearrange("p (a b) -> p a b", a=8),
                              in1=elast[:, 8 * j:8 * j + 8].unsqueeze(2).to_broadcast([128, 8, 64]), op=ALU.mult)

                        def ktr(j):
                            pv = psb(7)
                            for q in range(4):
                                transp(pv[:, q * 128:(q + 1) * 128], khT2[j % 2][:, q * 128:(q + 1) * 128], [KHTB[j % 2]], [PB[7]])
                            ACT([PB[7]], [KHAT], out=khat[:, 4 * j:4 * j + 4, :], in_=pv[:, 0:512].rearrange("p (a n) -> p a n", a=4), func=AF.Copy)
                        PEND.append(lambda: kchain(0))
                        PEND.append(lambda: (kchain(1), ktr(0)))
                        PEND.append(lambda: (kchain(2), ktr(1)))
                        PEND.append(lambda: (kchain(3), ktr(2)))
                        PEND.append(lambda: ktr(3))
                    items.append((lambda h=h: load_cols(WS, win_cols(l, OFF_FH + h * 128, 128), 128), hg_item))
                    items.append((lambda h=h: load_cols(WS, win_cols(l, OFF_QH + h * 128, 128), 128),
                                  lambda k: proj_fm(WS.aps[k], WS.bufs[k], 128, [0, 1], q_stage(1.0, 1.0))))
                    items.append((lambda h=h: load_cols(WS, win_cols(l, OFF_IH + h * 128, 128), 128),
                                  lambda k: proj_tm(WS.aps[k], WS.bufs[k], 128, [0, 1], evac_v(0))))
                    items.append((lambda h=h: load_cols(WS, win_cols(l, OFF_HOUT + h * 128, 128), 128),
                                  lambda k: unit_state([(0, 128, 0)], lambda: proj_fm(WS.aps[k], WS.bufs[k], 128, [0, 1], evac_silu))))
                    items.append((None, lambda k, h=h: unit_out((0, 128, 0), 8 + h, prm[:, pb + 3:pb + 4])))

                if only is not None:
                    items = [it for i, it in enumerate(items) if only(i)]
                run_items(items, 2)
                flush_pending()
                if dbg and s == 0 and l == 0:
                    dump("brT", brT, [12, T], BF16, BR)
                S.barrier()
                if stop_after == "mix":
                    stop = True
                    break

                C = Carver(big, SCR_LO, SBUF_BYTES)
                mergedT = C.alloc([8, T], BF16)
                MG = [Buf("mg%d" % i) for i in range(8)]
                WG1 = Slots(C, 2, [8, 3, 128], "wg1")
                WB1 = Slots(C, 2, [3, 4, 128], "wb1")
                sig = [C.alloc([512], F32) for _ in range(3)]
                SIG = [Buf("sig%d" % i) for i in range(3)]
                acc = C.alloc([512], F32)
                ACC = Buf("acc")

                def t1_load(c):
                    def f():
                        k = WG1.next()
                        for n in range(3):
                            S.dma("pool", WG1.chs[k], WG1.aps[k][:, :, n, :], win_cols(l, OFF_MG + n * 1024 + c * 128, 128), writes=[WG1.bufs[k]])
                        k2 = WB1.next()
                        for n in range(3):
                            S.dma("pool", WB1.chs[k2], WB1.aps[k2][:, n, :, :], wbr_d[l, n][:, c * 128:(c + 1) * 128].rearrange("(k p) n -> p k n", p=128), writes=[WB1.bufs[k2]])
                        return (k, k2)
                    return f

                def t1_comp(c):
                    def f(kk):
                        k, k2 = kk
                        for j in range(4):
                            sl = slice(j * 512, (j + 1) * 512)
                            for n in range(3):
                                for kc in range(8):
                                    mm(psum[n][:], WG1.aps[k][:, kc, n, :], xT[:, kc, sl], kc == 0, kc == 7, [WG1.bufs[k]] + XT[4 * j:4 * j + 4], [PB[n]])
                                ACT([PB[n]], [SIG[n]], out=sig[n], in_=psum[n][:], func=AF.Sigmoid)
                            for n in range(3):
                                for kc in range(4):
                                    mm(psum[3 + n][:], WB1.aps[k2][:, n, kc, :], brT[:, 4 * n + kc, sl], kc == 0, kc == 3, [WB1.bufs[k2], BR[4 * n + kc]], [PB[3 + n]])
                            V("tensor_tensor", [PB[3], SIG[0]], [ACC], out=acc, in0=psum[3][:], in1=sig[0], op=ALU.mult)
                            V("tensor_tensor", [PB[4]], [SIG[1]], out=sig[1], in0=psum[4][:], in1=sig[1], op=ALU.mult)
                            V("tensor_tensor", [PB[5]], [SIG[2]], out=sig[2], in0=psum[5][:], in1=sig[2], op=ALU.mult)
                            V("tensor_tensor", [SIG[1]], [ACC], out=acc, in0=acc, in1=sig[1], op=ALU.add)
                            V("tensor_tensor", [ACC, SIG[2]], [MG[c]], out=mergedT[:, c, sl], in0=acc, in1=sig[2], op=ALU.add)
                    return f
                run_items([(t1_load(c), t1_comp(c)) for c in range(8)], 2)
                S.barrier()
                C2 = Carver(big, SCR_LO - 12 * T * 2, SCR_LO)
                woT = C2.alloc([8, D], BF16)
                lnw = C2.alloc([D], F32)
                lnb = C2.alloc([D], F32)
                st6 = C2.alloc([16, 2, 6], F32)
                mvs = [C2.alloc([8], F32) for _ in range(2)]
                xbs = [C2.alloc([D], BF16) for _ in range(2)]
                WO, LNP = Buf("wo"), Buf("lnp")
                LNBs = [Buf("lnb0"), Buf("lnb1")]
                ch_wo = S.chan("wo_%d" % S.uid())
                for hq in range(2):
                    S.dma("pool", ch_wo, woT[:, 4 * hq:4 * hq + 4, :], wout_d[l][512 * hq:512 * hq + 512, :].rearrange("(k p) n -> p k n", p=128), writes=[WO])
                S.dma("sp", ch_p, lnw, ln1w_d[l].partition_broadcast(128), writes=[LNP])
                S.dma("sp", ch_p, lnb, ln1b_d[l].partition_broadcast(128), writes=[LNP])
                def fa(t_):
                    ln_finish_a(t_, st6[:, t_, :, :], lnw, lnb, LNP, mvs[t_ % 2], xbs[t_ % 2], LNBs[t_ % 2])

                def fb(t_):
                    ln_finish_b(t_, 2 + (t_ % 2), xbs[t_ % 2], LNBs[t_ % 2])
                for tt in range(18):
                    if tt < 16:
                        bks = [(4 * (tt % 2)) + 0, (4 * (tt % 2)) + 1]
                        for hf in range(2):
                            for kc in range(8):
                                mm(psum[bks[hf]][:], mergedT[:, kc, tt * 128:(tt + 1) * 128], woT[:, kc, hf * 512:(hf + 1) * 512], kc == 0, kc == 7, [MG[kc], WO], [PB[bks[hf]]])
                            ln_pre(tt, hf, bks[hf], st6[:, tt, :, :])
                    if 0 <= tt - 2:
                        fb(tt - 2)
                    if 0 <= tt - 1 < 16:
                        fa(tt - 1)
                if dbg and s == 0 and l == 0:
                    dump("x1", xres, [16, D], F32, XR)
                S.barrier()
                if stop_after == "t1":
                    stop = True
                    break

                C = Carver(big, SCR_LO - 12 * T * 2, SBUF_BYTES)
                hT = C.alloc([32, 1024], BF16)
                HT = [Buf("hT%d" % i) for i in range(32)]
                WU = Slots(C, 3, [8, 128], "wu")
                WD = Slots(C, 3, [512], "wd")
                lnw = C.alloc([D], F32)
                lnb = C.alloc([D], F32)
                st6h = [C.alloc([8, 2, 6], F32) for _ in range(2)]
                LNQ = []
                mvs = [C.alloc([8], F32) for _ in range(2)]
                xbs = [C.alloc([D], BF16) for _ in range(2)]
                tmpR = [C.alloc([512], F32) for _ in range(2)]
                TR = [Buf("tr0"), Buf("tr1")]
                LNP = Buf("lnp2")
                LNBs = [Buf("lnb20"), Buf("lnb21")]
                S.dma("sp", ch_p, lnw, ln2w_d[l].partition_broadcast(128), writes=[LNP])
                S.dma("sp", ch_p, lnb, ln2b_d[l].partition_broadcast(128), writes=[LNP])
                for half in range(2):
                    t0 = half * 1024
                    its = []
                    for f in range(32):
                        def ldu(f=f):
                            return load_cols(WU, wup_d[l][:, f * 128:(f + 1) * 128].rearrange("(k p) n -> p k n", p=128), 128)

                        def cpu(k, f=f):
                            for jj in range(2):
                                b = jj
                                tsl = slice(t0 + jj * 512, t0 + (jj + 1) * 512)
                                for kc in range(8):
                                    mm(psum[b][:], WU.aps[k][:, kc, :], xT[:, kc, tsl], kc == 0, kc == 7,
                                       [WU.bufs[k]] + XT[(t0 + jj * 512) // 128:(t0 + jj * 512) // 128 + 4], [PB[b]])
                                ACT([PB[b]], [TR[jj]], out=tmpR[jj], in_=psum[b][:], func=AF.Relu)
                                V("tensor_tensor", [PB[b], TR[jj]], [HT[f]], out=hT[:, f, jj * 512:(jj + 1) * 512], in0=psum[b][:], in1=tmpR[jj], op=ALU.mult)
                            if f % 2 == 1 and LNQ:
                                LNQ.pop(0)()
                        its.append((ldu, cpu))
                    run_items(its, 2)
                    for chh in range(2):
                        its = []
                        for f in range(32):
                            def ldd(f=f, chh=chh):
                                k = WD.next()
                                S.dma("pool", WD.chs[k], WD.aps[k], wdn_d[l][f * 128:(f + 1) * 128, chh * 512:(chh + 1) * 512], writes=[WD.bufs[k]])
                                return k

                            def cpd(k, f=f):
                                for q in range(8):
                                    mm(psum[q][:], hT[:, f, q * 128:(q + 1) * 128], WD.aps[k], f == 0, f == 31, [HT[f], WD.bufs[k]], [PB[q]])
                            its.append((ldd, cpd))
                        run_items(its, 2)
                        for q in range(8):
                            ln_pre(half * 8 + q, chh, q, st6h[half][:, q, :, :], True, False)
                        for q in range(8):
                            ln_pre(half * 8 + q, chh, q, st6h[half][:, q, :, :], False, True)
                    for q in range(8):
                        tt = half * 8 + q

                        def lnfa(tt=tt, q=q, half=half):
                            ln_finish_a(tt, st6h[half][:, q, :, :], lnw, lnb, LNP, mvs[q % 2], xbs[q % 2], LNBs[q % 2])
                            if l == n_layers - 1 and stop_after is None:
                                out_toks.append(S.dma("sp", ch_o[tt % 2], out_d[s, tt * 128:(tt + 1) * 128, :], xres[:, tt, :], reads=[XR[tt]]))

                        def lnfb(tt=tt, q=q):
                            ln_finish_b(tt, 2 + q % 6, xbs[q % 2], LNBs[q % 2])
                        if q == 0:
                            LNQ.append(lnfa)
                        else:
                            prevb = LNQ.pop()
                            LNQ.append(lnfa)
                            LNQ.append(prevb)
                        LNQ.append(lnfb)
                while LNQ:
                    LNQ.pop(0)()
                if dbg and s == 0 and l == 0:
                    dump("x2", xres, [16, D], F32, XR)
                S.barrier()
            if stop:
                break
            S.barrier()
        S.final_wait("sp", out_toks)
        S.emit()
    return nc, list(dbg_d.keys())


_CACHE = {}


def kernel(**inputs):
    n = 8
    x = np.ascontiguousarray(inputs["x"], dtype=np.float32)
    if "nc" not in _CACHE:
        _CACHE["nc"] = build()[0]
    nc = _CACHE["nc"]
    consts = make_consts()
    names = ["w_in", "gla_w_gate", "gla_b_gate", "diff_lambda", "diff_norm_w", "gla_norm_w", "hgrn_norm_w", "hgrn_lb",
             "w_branch", "w_out", "ln1_w", "ln1_b", "w_up", "w_down", "ln2_w", "ln2_b"]
    shared = {k: np.ascontiguousarray(inputs[k], dtype=np.float32) for k in names}
    in_maps = []
    for c in range(n):
        m = dict(shared)
        m["x"] = x[NSEQ * c:NSEQ * (c + 1)]
        m["consts"] = consts
        in_maps.append(m)
    res = run_bass_kernel_spmd(nc, in_maps, core_ids=list(range(n)))
    return np.concatenate([r["out"] for r in res.results], axis=0).astype(np.float32)
```

```python
import contextlib
import numpy as np
import concourse.bass as bass
import concourse.mybir as mybir
from concourse.bass_utils import run_bass_kernel_spmd

F32 = mybir.dt.float32
BF16 = mybir.dt.bfloat16
AF = mybir.ActivationFunctionType
ALU = mybir.AluOpType
AX = mybir.AxisListType

ENGS = ("pe", "act", "dve", "pool", "sp")


class Buf:
    __slots__ = ("name", "w", "r")

    def __init__(self, name):
        self.name = name
        self.w = None
        self.r = {}


class Chan:
    __slots__ = ("key", "cnt")

    def __init__(self, key):
        self.key = key
        self.cnt = 0


class Sched:
    def __init__(self, nc, stack):
        self.nc = nc
        self.stack = stack
        self.stream = {e: [] for e in ENGS}
        self.cnt = {e: 0 for e in ENGS}
        self.seen = {e: {} for e in ENGS}
        self.sems = {}
        for e in ENGS:
            self.sems[e] = stack.enter_context(nc.semaphore("s_" + e))
        self.chans = []
        self.same_sync = True
        self._uid = 0

    def uid(self):
        self._uid += 1
        return self._uid

    def chan(self, name):
        c = Chan("ch_" + name)
        self.sems[c.key] = self.stack.enter_context(self.nc.semaphore(c.key))
        self.chans.append(c)
        return c

    def _deps(self, reads, writes):
        deps = {}

        def add(t):
            if t is None:
                return
            k, v = t
            if deps.get(k, 0) < v:
                deps[k] = v
        for b in reads:
            add(b.w)
        for b in writes:
            add(b.w)
            for k, v in b.r.items():
                add((k, v))
        return deps

    def _filter(self, eng, deps):
        waits = []
        seen = self.seen[eng]
        for k, v in deps.items():
            if k == eng and (eng == "pe" or eng == "sp" or not self.same_sync):
                continue
            if seen.get(k, 0) >= v:
                continue
            seen[k] = v
            waits.append((k, v))
        return waits

    def _mark(self, tok, reads, writes):
        k, v = tok
        for b in reads:
            if b.r.get(k, 0) < v:
                b.r[k] = v
        for b in writes:
            b.w = tok
            b.r = {}

    def op(self, eng, meth, reads=(), writes=(), *args, **kw):
        fn = (meth, args, kw)
        deps = self._deps(reads, writes)
        waits = self._filter(eng, deps)
        self.cnt[eng] += 1
        tok = (eng, self.cnt[eng])
        self.stream[eng].append((waits, fn, eng, 1))
        self._mark(tok, reads, writes)
        return tok

    def dma(self, q, chan, out, in_, reads=(), writes=(), **kw):
        deps = self._deps(reads, writes)
        if chan.cnt > 0:
            if deps.get(chan.key, 0) < chan.cnt:
                deps[chan.key] = chan.cnt
        waits = self._filter(q, deps)
        chan.cnt += 16
        tok = (chan.key, chan.cnt)
        self.stream[q].append((waits, ("dma_start", (), dict(out=out, in_=in_, **kw)), chan.key, 16))
        self._mark(tok, reads, writes)
        return tok

    def barrier(self):
        snap = {e: self.cnt[e] for e in ENGS if self.cnt[e] > 0}
        for c in self.chans:
            if c.cnt > 0:
                snap[c.key] = c.cnt
        for e in ENGS:
            deps = {k: v for k, v in snap.items() if k != e}
            waits = self._filter(e, deps)
            if waits:
                self.stream[e].append((waits, None, None, 0))

    def final_wait(self, eng, toks):
        deps = {}
        for k, v in toks:
            if deps.get(k, 0) < v:
                deps[k] = v
        waits = self._filter(eng, deps)
        if waits:
            self.stream[eng].append((waits, None, None, 0))

    def emit(self):
        nc = self.nc
        sems = self.sems
        streams = self.stream

        def run(e, lst):
            for waits, fn, key, n in lst:
                for k, v in waits:
                    e.wait_ge(sems[k], v)
                if fn is not None:
                    meth, args, kw = fn
                    ins = getattr(e, meth)(*args, **kw)
                    ins.then_inc(sems[key], n)

        with nc.Block() as block:
            @block.tensor
            def _(e):
                run(e, streams["pe"])

            @block.scalar
            def _(e):
                run(e, streams["act"])

            @block.vector
            def _(e):
                run(e, streams["dve"])

            @block.gpsimd
            def _(e):
                run(e, streams["pool"])

            @block.sync
            def _(e):
                run(e, streams["sp"])

D = 1024
T = 2048
DEPTH = 2
NSEQ = 2
IN_W = 8208
DFF = 4096
ALPHA_C = float((2 * DEPTH) ** 0.25)
EPS = 1e-5
OFF_QA, OFF_KA, OFF_VA = 0, 512, 1024
OFF_QG, OFF_KG, OFF_VG, OFF_GLR, OFF_GOUT = 1536, 1792, 2048, 2560, 2576
OFF_QH, OFF_FH, OFF_IH, OFF_HOUT, OFF_MG = 3088, 3600, 4112, 4624, 5136
SBUF_BYTES = 212800
NTAB = 19


def _prod(s):
    r = 1
    for v in s:
        r *= v
    return r


class Carver:
    def __init__(self, big, lo, hi):
        self.big, self.off, self.hi = big, lo, hi

    def alloc(self, shape, dt):
        sz = 4 if dt == F32 else 2
        nb = _prod(shape) * sz
        nb = (nb + 63) // 64 * 64
        assert self.off + nb <= self.hi, ("SBUF carve overflow", self.off, nb, self.hi)
        ap = self.big[:, self.off // 2:(self.off + nb) // 2]
        self.off += nb
        if dt == F32:
            ap = ap.bitcast(F32)
        ap = ap[:, 0:_prod(shape)]
        if len(shape) == 2:
            ap = ap.rearrange("p (a b) -> p a b", a=shape[0])
        elif len(shape) == 3:
            ap = ap.rearrange("p (a b c) -> p a b c", a=shape[0], b=shape[1])
        return ap


def make_consts():
    c = np.zeros((128, 1024), np.float32)
    c[:, 0:128] = np.eye(128, dtype=np.float32)
    j = np.arange(128)[:, None]
    i = np.arange(128)[None, :]
    c[:, 128:256] = np.where(i < j, -30000.0, 0.0)
    c[:, 256:384] = ((i >= j) & ((i // 64) == (j // 64))).astype(np.float32)
    slopes = [2.0 ** (-8.0 * (h + 1) / 4) for h in range(4)]
    for h in range(4):
        for d in range(NTAB):
            c[:, 384 + h * NTAB + d] = slopes[h] * (128.0 * (d - 15) + np.arange(128))
    c[:, 461] = -0.5
    c[:, 512:1024] = 1.0
    c[:, 512:1024:64] = 0.0
    return c


def build(n_seq=NSEQ, n_layers=DEPTH, dbg=False, stop_after=None, only=None):
    nc = bass.Bass("TRN2", target_bir_lowering=False)
    dram = {}

    def din(name, shape):
        dram[name] = nc.dram_tensor(name, list(shape), F32, kind="ExternalInput").ap()
        return dram[name]
    x_d = din("x", [NSEQ, T, D])
    w_in_d = din("w_in", [DEPTH, D, IN_W])
    wg_d = din("gla_w_gate", [DEPTH, 16, 256])
    bg_d = din("gla_b_gate", [DEPTH, 256])
    dl_d = din("diff_lambda", [DEPTH, 4, 64])
    dnw_d = din("diff_norm_w", [DEPTH, 128])
    gnw_d = din("gla_norm_w", [DEPTH, 128])
    hnw_d = din("hgrn_norm_w", [DEPTH, 128])
    hlb_d = din("hgrn_lb", [DEPTH, 512])
    wbr_d = din("w_branch", [DEPTH, 3, 512, D])
    wout_d = din("w_out", [DEPTH, D, D])
    ln1w_d = din("ln1_w", [DEPTH, D])
    ln1b_d = din("ln1_b", [DEPTH, D])
    wup_d = din("w_up", [DEPTH, D, DFF])
    wdn_d = din("w_down", [DEPTH, DFF, D])
    ln2w_d = din("ln2_w", [DEPTH, D])
    ln2b_d = din("ln2_b", [DEPTH, D])
    cst_d = din("consts", [128, 1024])
    out_d = nc.dram_tensor("out", [NSEQ, T, D], F32, kind="ExternalOutput").ap()
    dbg_d = {}

    with contextlib.ExitStack() as st:
        S = Sched(nc, st)
        big = st.enter_context(nc.sbuf_tensor("big", [128, SBUF_BYTES // 2], BF16))
        psum = [st.enter_context(nc.psum_tensor("ps%d" % i, [128, 512], F32)) for i in range(8)]
        PB = [Buf("psum%d" % i) for i in range(8)]

        def psb(i):
            return psum[i][:].bitcast(BF16)

        P = Carver(big, 0, SBUF_BYTES)
        xres = P.alloc([16, D], F32)
        xT = P.alloc([8, T], BF16)
        cst = P.alloc([1024], F32)
        identb = P.alloc([128], BF16)
        maskb = P.alloc([128], BF16)
        prm = P.alloc([64], F32)
        brT = P.alloc([12, T], BF16)
        SCR_LO = P.off
        XR = [Buf("xres%d" % i) for i in range(16)]
        XT = [Buf("xT%d" % i) for i in range(16)]
        BR = [Buf("brT%d" % i) for i in range(12)]
        CST = Buf("cst")
        PRM = Buf("prm")
        cmask = cst[:, 256:384]
        rmask = cst[:, 512:1024]
        nhalf = cst[:, 461:462]

        def tab(h, d):
            i = 384 + h * NTAB + d + 15
            return cst[:, i:i + 1]

        ch_x = [S.chan("x%d" % i) for i in range(2)]
        ch_o = [S.chan("o%d" % i) for i in range(2)]
        ch_c = S.chan("c")
        ch_p = S.chan("p")
        ch_dbg = S.chan("dbg")
        out_toks = []

        def dump(name, ap, shape, dt, reads):
            if not dbg:
                return
            t = nc.dram_tensor("dbg_" + name, [128] + list(shape), dt, kind="ExternalOutput").ap()
            dbg_d[name] = t
            out_toks.append(S.dma("sp", ch_dbg, t, ap, reads=reads))

        def ACT(reads, writes, **kw):
            S.op("act", "activation", reads, writes, **kw)

        def V(meth, reads, writes, *a, **kw):
            S.op("dve", meth, reads, writes, *a, **kw)

        def G(meth, reads, writes, *a, **kw):
            S.op("pool", meth, reads, writes, *a, **kw)

        def mm(out, lhsT, rhs, start, stop, reads, writes):
            S.op("pe", "matmul", reads, writes, out, lhsT=lhsT, rhs=rhs, start=start, stop=stop, skip_group_check=True)

        def transp(out, in_, reads, writes):
            S.op("pe", "transpose", list(reads) + [CST], writes, out=out, in_=in_, identity=identb)

        S.dma("sp", ch_c, cst, cst_d, writes=[CST])
        V("tensor_copy", [CST], [CST], out=identb, in_=cst[:, 0:128])
        V("tensor_copy", [CST], [CST], out=maskb, in_=cst[:, 128:256])
        SC0 = Carver(big, SCR_LO, SBUF_BYTES)
        dlt = SC0.alloc([2, 4, 64], F32)
        dlp = SC0.alloc([2, 2, 64], F32)
        lbt = SC0.alloc([2, 4], F32)
        SETUP = Buf("setup")
        S.dma("sp", ch_p, dlt.rearrange("p l a b -> p (l a b)"),
              dl_d.rearrange("l a b -> (l a b)").partition_broadcast(128), writes=[SETUP])
        S.dma("sp", ch_p, lbt, hlb_d.rearrange("l (h p) -> p l h", p=128), writes=[SETUP], allow_slow_non_contiguous=True)
        for l in range(DEPTH):
            b = 32 * l
            S.dma("sp", ch_p, prm[:, b + 1:b + 2], dnw_d[l].unsqueeze(1), writes=[PRM])
            S.dma("sp", ch_p, prm[:, b + 2:b + 3], gnw_d[l].unsqueeze(1), writes=[PRM])
            S.dma("sp", ch_p, prm[:, b + 3:b + 4], hnw_d[l].unsqueeze(1), writes=[PRM])
            S.dma("sp", ch_p, prm[:, b + 4:b + 6], bg_d[l].rearrange("(g p) -> p g", p=128), writes=[PRM], allow_slow_non_contiguous=True)
        for l in range(DEPTH):
            b = 32 * l
            lam_init = 0.8 - 0.6 * float(np.exp(-0.3 * l))
            V("tensor_tensor", [SETUP], [SETUP], out=dlp[:, l, 0, :], in0=dlt[:, l, 0, :], in1=dlt[:, l, 1, :], op=ALU.mult)
            V("tensor_tensor", [SETUP], [SETUP], out=dlp[:, l, 1, :], in0=dlt[:, l, 2, :], in1=dlt[:, l, 3, :], op=ALU.mult)
            V("reduce_sum", [SETUP, PRM], [PRM], out=prm[:, b + 16:b + 18], in_=dlp[:, l, :, :], axis=AX.X)
            ACT([PRM], [PRM], out=prm[:, b + 18:b + 20], in_=prm[:, b + 16:b + 18], func=AF.Exp)
            V("scalar_tensor_tensor", [PRM], [PRM], out=prm[:, b + 0:b + 1], in0=prm[:, b + 19:b + 20], scalar=-lam_init, in1=prm[:, b + 18:b + 19], op0=ALU.add, op1=ALU.subtract)
            V("tensor_scalar", [PRM], [PRM], out=prm[:, b + 1:b + 2], in0=prm[:, b + 1:b + 2], scalar1=1.0 - lam_init, scalar2=None, op0=ALU.mult)
            V("tensor_scalar", [PRM], [PRM], out=prm[:, b + 4:b + 6], in0=prm[:, b + 4:b + 6], scalar1=-1.0, scalar2=None, op0=ALU.mult)
        V("memset", [PRM], [PRM], prm[:, 6:10], 1.0)
        V("memset", [PRM], [PRM], prm[:, 10:14], 1e-30)
        V("tensor_tensor", [SETUP, PRM], [PRM], out=prm[:, 48:52], in0=lbt[:, 1, :], in1=lbt[:, 0, :], op=ALU.subtract)
        ACT([PRM], [PRM], out=prm[:, 52:56], in_=prm[:, 48:52], func=AF.Sigmoid)
        V("tensor_scalar", [PRM], [PRM], out=prm[:, 38:42], in0=prm[:, 52:56], scalar1=-1.0, scalar2=1.0, op0=ALU.mult, op1=ALU.add)
        V("tensor_scalar", [PRM], [PRM], out=prm[:, 42:46], in0=prm[:, 52:56], scalar1=1e-30, scalar2=None, op0=ALU.max)
        S.barrier()

        PEND = []

        FL = {"mode": "gh"}

        def flush_pending(n=None):
            while PEND and (n is None or n > 0):
                PEND.pop(0)()
                if n is not None:
                    n -= 1

        class Slots:
            def __init__(self, carver, n, shape, name):
                self.aps = [carver.alloc(shape, BF16) for _ in range(n)]
                self.bufs = [Buf("%s%d" % (name, i)) for i in range(n)]
                self.chs = [S.chan("%s%d_%d" % (name, i, S.uid())) for i in range(n)]
                self.i = 0

            def next(self):
                k = self.i % len(self.aps)
                self.i += 1
                return k

        def run_items(items, depth):
            loads = [i for i, it in enumerate(items) if it[0] is not None]
            handles = {}
            state = {"p": 0, "out": 0}

            def pump():
                while state["p"] < len(loads) and state["out"] < depth:
                    i = loads[state["p"]]
                    handles[i] = items[i][0]()
                    state["p"] += 1
                    state["out"] += 1
            for i, it in enumerate(items):
                pump()
                it[1](handles.get(i))
                if it[0] is not None:
                    state["out"] -= 1

        def load_cols(slots, src3, ncols):
            k = slots.next()
            S.dma("pool", slots.chs[k], slots.aps[k][:, :, 0:ncols], src3, writes=[slots.bufs[k]])
            return k

        def win_cols(l, c0, n):
            return w_in_d[l][:, c0:c0 + n].rearrange("(k p) n -> p k n", p=128)

        def proj_fm(wap, wbuf, M, banks, evac):
            for j in range(4):
                b = banks[j % len(banks)]
                for kc in range(8):
                    mm(psum[b][0:M, :], wap[:, kc, 0:M], xT[:, kc, j * 512:(j + 1) * 512], kc == 0, kc == 7,
                       [wbuf] + XT[4 * j:4 * j + 4], [PB[b]])
                evac(j, b)
                if FL["mode"] == "gh1":
                    flush_pending(1)
                elif j == 1:
                    flush_pending()

        def proj_tm(wap, wbuf, N, banks, evac):
            for g in range(4):
                b = banks[g % len(banks)]
                for q in range(4):
                    tt = 4 * g + q
                    for kc in range(8):
                        mm(psum[b][:, q * 128:q * 128 + N], xT[:, kc, tt * 128:(tt + 1) * 128], wap[:, kc, 0:N], kc == 0, kc == 7,
                           [wbuf, XT[tt]], [PB[b]])
                evac(g, b)
                if FL["mode"] == "gh1":
                    flush_pending(1)
                elif g == 1:
                    flush_pending()

        def to_xT(tt, tbank, xb, XB):
            ACT([XR[tt]], [XB], out=xb, in_=xres[:, tt, :], func=AF.Copy)
            pv = psb(tbank)
            for k in range(8):
                transp(pv[:, k * 128:(k + 1) * 128], xb[:, k * 128:(k + 1) * 128], [XB], [PB[tbank]])
            ACT([PB[tbank]], [XT[tt]], out=xT[:, :, tt * 128:(tt + 1) * 128], in_=pv.rearrange("p (k n) -> p k n", k=8), func=AF.Copy)

        def ln_pre(tt, hf, bank, st6t, do_add=True, do_stats=True):
            sl = slice(hf * 512, (hf + 1) * 512)
            xr = xres[:, tt, :]
            if do_add:
                V("scalar_tensor_tensor", [PB[bank]], [XR[tt]], out=xr[:, sl], in0=xr[:, sl], scalar=ALPHA_C, in1=psum[bank][:], op0=ALU.mult, op1=ALU.add)
            if do_stats:
                V("bn_stats", [XR[tt]], [XR[tt]], out=st6t[:, hf, :], in_=xr[:, sl])

        def ln_finish_a(tt, st6t, lnw, lnb, LNP, mv, xb, LNB):
            xr = xres[:, tt, :]
            V("bn_aggr", [XR[tt]], [LNB], out=mv[:, 0:2], in_=st6t.rearrange("p a b -> p (a b)"))
            V("tensor_scalar", [LNB], [LNB], out=mv[:, 2:3], in0=mv[:, 1:2], scalar1=EPS, scalar2=None, op0=ALU.add)
            ACT([LNB], [LNB], out=mv[:, 3:4], in_=mv[:, 2:3], func=AF.Sqrt)
            V("reciprocal", [LNB], [LNB], out=mv[:, 4:5], in_=mv[:, 3:4])
            V("scalar_tensor_tensor", [LNB, LNP], [XR[tt]], out=xr, in0=xr, scalar=mv[:, 0:1], in1=lnw, op0=ALU.subtract, op1=ALU.mult)
            V("scalar_tensor_tensor", [LNB, LNP], [XR[tt]], out=xr, in0=xr, scalar=mv[:, 4:5], in1=lnb, op0=ALU.mult, op1=ALU.add)
            ACT([XR[tt]], [LNB], out=xb, in_=xres[:, tt, :], func=AF.Copy)

        def ln_finish_b(tt, tbank, xb, LNB):
            pv = psb(tbank)
            for k in range(8):
                transp(pv[:, k * 128:(k + 1) * 128], xb[:, k * 128:(k + 1) * 128], [LNB], [PB[tbank]])
            ACT([PB[tbank]], [XT[tt]], out=xT[:, :, tt * 128:(tt + 1) * 128], in_=pv.rearrange("p (k n) -> p k n", k=8), func=AF.Copy)

        for s in range(n_seq):
            C = Carver(big, SCR_LO, SBUF_BYTES)
            xb0 = C.alloc([D], BF16)
            XB0 = Buf("xb0")
            for tt in range(16):
                S.dma("sp", ch_x[tt % 2], xres[:, tt, :], x_d[s, tt * 128:(tt + 1) * 128, :], writes=[XR[tt]])
            for tt in range(16):
                to_xT(tt, tt % 2, xb0, XB0)
            S.barrier()
            stop = False
            for l in range(n_layers):
                pb = 32 * l
                C = Carver(big, SCR_LO, SBUF_BYTES)
                WS = Slots(C, 3, [8, 128], "ws")
                vbuf = [C.alloc([16, 130], BF16) for _ in range(2)]
                VB = [Buf("v0"), Buf("v1")]
                qT = C.alloc([T], BF16)
                kT = C.alloc([T], BF16)
                QT, KT = Buf("qT"), Buf("kT")
                sm = C.alloc([64], F32)
                SMB = [Buf("sm%d" % i) for i in range(4)]
                C_shared = C.off
                CA = Carver(big, C_shared, SBUF_BYTES)
                onb = CA.alloc([4, 128], BF16)
                ONBB = [Buf("onb%d" % i) for i in range(4)]
                qTp = CA.alloc([2, T], BF16)
                QTP = Buf("qTp")
                PT = [CA.alloc([512], BF16) for _ in range(3)]
                PTB = [Buf("pt%d" % i) for i in range(3)]
                ost = [CA.alloc([2, 129], F32) for _ in range(4)]
                OST = [Buf("ost%d" % i) for i in range(4)]
                o1 = [CA.alloc([128], F32) for _ in range(4)]
                sqj4 = [CA.alloc([128], F32) for _ in range(4)]
                OFB = [Buf("ofin%d" % i) for i in range(4)]
                CG = Carver(big, C_shared, SBUF_BYTES)
                cum = CG.alloc([T], F32)
                tmpA = CG.alloc([512], F32)
                tmpB = CG.alloc([512], F32)
                tmpC = CG.alloc([512], F32)
                khT0 = CG.alloc([512], BF16)
                khat = CG.alloc([16, 128], BF16)
                Sbf = CG.alloc([32, 128], BF16)
                Sf = CG.alloc([4, 128], F32)
                sgT = CG.alloc([T], BF16)
                elast = CG.alloc([32], F32)
                wgb = CG.alloc([256], BF16)
                PTg4 = CG.alloc([4, 128], BF16)
                onb4 = [CG.alloc([4, 128], BF16) for _ in range(2)]
                ONB4 = [Buf("onb4_0"), Buf("onb4_1")]
                sm4 = [sm[:, 0:16], sm[:, 16:32]]
                CUM, TA, TB, TC, KHT, KHAT, SBF, SGT, EL, GLR, WG, SQG = [Buf(n) for n in
                    ["cum", "tA", "tB", "tC", "khT", "khat", "sbf", "sgT", "elast", "glrT", "wgb", "sqg"]]
                glrT = sgT
                GLR = SGT
                SFB = [Buf("sf0"), Buf("sf1")]
                PTGB = Buf("ptg4")
                khT2 = [khT0, PTg4.rearrange("p a n -> p (a n)")]
                KHTB = [Buf("khT0"), PTGB]
                ch_wg = S.chan("wg_%d" % S.uid())

                for vb in range(2):
                    V("memset", [], [VB[vb]], vbuf[vb][:, :, 128:130], 1.0)
                V("memset", [], [QTP], qTp[64:128, 0, :], 0.0)
                V("memset", [], [QTP], qTp[0:64, 1, :], 0.0)

                items = []
                FL["mode"] = "gh"

                def evac_copy_bf16(dst, DB, eng):
                    def f(j, b):
                        if eng == "act":
                            ACT([PB[b]], [DB], out=dst[:, j * 512:(j + 1) * 512], in_=psum[b][:], func=AF.Copy)
                        else:
                            V("tensor_copy", [PB[b]], [DB], out=dst[:, j * 512:(j + 1) * 512], in_=psum[b][:])
                    return f

                def evac_v(vb):
                    def f(g, b):
                        ACT([PB[b]], [VB[vb]], out=vbuf[vb][:, 4 * g:4 * g + 4, 0:128], in_=psum[b][:].rearrange("p (a n) -> p a n", a=4), func=AF.Copy)
                    return f

                def rms_chain(src, SRC, qs, k0, junk):
                    SM = SMB[qs]
                    ACT([SRC], [SM], out=junk, in_=src, func=AF.Square, accum_out=sm[:, k0:k0 + 1])
                    V("tensor_scalar", [SM], [SM], out=sm[:, k0 + 1:k0 + 2], in0=sm[:, k0:k0 + 1], scalar1=1.0 / 128, scalar2=EPS, op0=ALU.mult, op1=ALU.add)
                    ACT([SM], [SM], out=sm[:, k0 + 2:k0 + 3], in_=sm[:, k0 + 1:k0 + 2], func=AF.Sqrt)
                    V("reciprocal", [SM], [SM], out=sm[:, k0 + 3:k0 + 4], in_=sm[:, k0 + 2:k0 + 3])
                    V("tensor_scalar", [SRC, SM], [ONBB[qs]], out=onb[:, qs, :], in0=src, scalar1=sm[:, k0 + 3:k0 + 4], scalar2=None, op0=ALU.mult)

                def onb_transpose(qs, tb):
                    transp(psb(tb)[:, qs * 128:(qs + 1) * 128], onb[:, qs, :], [ONBB[qs]], [PB[tb]])

                def finish_T(dst, DB, wcol, gate, tb=0):
                    src = psb(tb)[:, 0:512]
                    if gate is None:
                        V("tensor_scalar", [PB[tb], PRM], [DB], out=dst, in0=src, scalar1=wcol, scalar2=None, op0=ALU.mult)
                    else:
                        gap, GB = gate
                        V("scalar_tensor_tensor", [PB[tb], PRM, GB], [DB], out=dst, in0=src, scalar=wcol, in1=gap, op0=ALU.mult, op1=ALU.mult)

                def evac_qpad(j, b):
                    sl = slice(j * 512, (j + 1) * 512)
                    ACT([PB[b]], [QTP], out=qTp[0:64, 0, sl], in_=psum[b][0:64, :], func=AF.Copy)
                    V("tensor_copy", [PB[b]], [QTP], out=qTp[64:128, 1, sl], in_=psum[b][64:128, :])

                def attn_head(h):
                    hb = h
                    for qt in range(4):
                        steps = [(m, jb) for jb in range(4 * (qt + 1)) for m in (0, 1)]

                        def emit_S(idx):
                            m, jb = steps[idx]
                            bk = SBK[idx % 3]
                            r = jb - 4 * qt
                            c0 = 128 * r if r > 0 else 0
                            mm(psum[bk][:, c0:512], kT[:, jb * 128:(jb + 1) * 128], qTp[:, m, qt * 512 + c0:qt * 512 + 512],
                               True, r < 0, [KT, QTP], [PB[bk]])
                            if r >= 0:
                                mm(psum[bk][:, c0:c0 + 128], identb, maskb, False, True, [CST], [PB[bk]])

                        def emit_rest(idx):
                            m, jb = steps[idx]
                            bk = SBK[idx % 3]
                            pt = idx % 3
                            r = jb - 4 * qt
                            c0 = 128 * r if r > 0 else 0
                            if h == 0:
                                for u in range(2):
                                    a, bnd = max(c0, 256 * u), 256 * u + 256
                                    if a >= bnd:
                                        continue
                                    d = jb - 4 * qt - 2 * u
                                    ACT([PB[bk], CST], [PTB[pt]], out=PT[pt][:, a:bnd], in_=psum[bk][:, a:bnd], func=AF.Exp, bias=tab(0, d), scale=0.125)
                            else:
                                d = jb - 4 * qt
                                ACT([PB[bk], CST], [PTB[pt]], out=PT[pt][:, c0:512], in_=psum[bk][:, c0:512], func=AF.Exp, bias=tab(h, d), scale=0.125)
                            for qs in range(max(r, 0), 4):
                                ob = 4 + 2 * m + qs // 2
                                oc = (qs % 2) * 256
                                mm(psum[ob][:, oc:oc + 129], PT[pt][:, qs * 128:(qs + 1) * 128], vbuf[0][:, jb, 0:129],
                                   (jb == 0 and qs % 2 == 0), (jb == 4 * qt + qs), [PTB[pt], VB[0]], [PB[ob]])
                        n = len(steps)
                        SBK = [2, 3, 0]
                        emit_S(0)
                        emit_S(1)
                        for idx in range(n):
                            if idx + 2 < n:
                                emit_S(idx + 2)
                            emit_rest(idx)
                            if idx == 7:
                                flush_pending()
                        QS = range(4)
                        oc_ = lambda qs: (qs % 2) * 256
                        b0_ = lambda qs: 4 + qs // 2
                        b1_ = lambda qs: 6 + qs // 2
                        for bq in range(4):
                            src_o = psum[4 + bq][:].rearrange("p (a n) -> p a n", a=2)[:, :, 0:129]
                            if bq % 2 == 0:
                                ACT([PB[4 + bq]], [OST[bq]], out=ost[bq], in_=src_o, func=AF.Copy)
                            else:
                                V("tensor_copy", [PB[4 + bq]], [OST[bq]], out=ost[bq], in_=src_o)
                        s0_ = lambda qs: ost[qs // 2]
                        s1_ = lambda qs: ost[2 + qs // 2]
                        S0_ = lambda qs: OST[qs // 2]
                        S1_ = lambda qs: OST[2 + qs // 2]
                        for qs in QS:
                            V("reciprocal", [S0_(qs)], [SMB[qs]], out=sm[:, 8 * qs:8 * qs + 1], in_=s0_(qs)[:, qs % 2, 128:129])
                            V("reciprocal", [S1_(qs)], [SMB[qs]], out=sm[:, 8 * qs + 1:8 * qs + 2], in_=s1_(qs)[:, qs % 2, 128:129])
                        for qs in QS:
                            V("tensor_tensor", [SMB[qs], PRM], [SMB[qs]], out=sm[:, 8 * qs + 2:8 * qs + 3], in0=sm[:, 8 * qs + 1:8 * qs + 2], in1=prm[:, pb:pb + 1], op=ALU.mult)
                        for qs in QS:
                            V("tensor_scalar", [S0_(qs), SMB[qs]], [OFB[qs]], out=o1[qs], in0=s0_(qs)[:, qs % 2, 0:128], scalar1=sm[:, 8 * qs:8 * qs + 1], scalar2=None, op0=ALU.mult)
                        for qs in QS:
                            V("scalar_tensor_tensor", [S1_(qs), SMB[qs]], [OFB[qs]], out=o1[qs], in0=s1_(qs)[:, qs % 2, 0:128], scalar=sm[:, 8 * qs + 2:8 * qs + 3], in1=o1[qs], op0=ALU.mult, op1=ALU.add)
                        for qs in QS:
                            V("scalar_tensor_tensor", [OFB[qs]], [SMB[qs]], out=sqj4[qs], in0=o1[qs], scalar=1.0, in1=o1[qs], op0=ALU.mult, op1=ALU.mult, accum_out=sm[:, 8 * qs + 3:8 * qs + 4])
                        for qs in QS:
                            V("tensor_scalar", [SMB[qs]], [SMB[qs]], out=sm[:, 8 * qs + 4:8 * qs + 5], in0=sm[:, 8 * qs + 3:8 * qs + 4], scalar1=1.0 / 128, scalar2=EPS, op0=ALU.mult, op1=ALU.add)
                        for qs in QS:
                            S.op("pool", "tensor_tensor", [SMB[qs], CST], [SMB[qs]], out=sm[:, 8 * qs + 6:8 * qs + 7], in0=sm[:, 8 * qs + 4:8 * qs + 5], in1=nhalf, op=ALU.pow)
                        for qs in QS:
                            V("tensor_scalar", [OFB[qs], SMB[qs]], [ONBB[qs]], out=onb[:, qs, :], in0=o1[qs], scalar1=sm[:, 8 * qs + 6:8 * qs + 7], scalar2=None, op0=ALU.mult)

                        def deferred(qt=qt):
                            for qs in range(4):
                                transp(psb(1)[:, qs * 128:(qs + 1) * 128], onb[:, qs, :], [ONBB[qs]], [PB[1]])
                            finish_T(brT[:, hb, qt * 512:(qt + 1) * 512], BR[hb], prm[:, pb + 1:pb + 2], None, 1)
                        PEND.append(deferred)

                for h in range(4):
                    items.append((lambda h=h: load_cols(WS, win_cols(l, OFF_QA + h * 128, 128), 128),
                                  lambda k: proj_fm(WS.aps[k], WS.bufs[k], 128, [0, 1], evac_qpad)))
                    items.append((lambda h=h: load_cols(WS, win_cols(l, OFF_KA + h * 128, 128), 128),
                                  lambda k: proj_fm(WS.aps[k], WS.bufs[k], 128, [0, 1], evac_copy_bf16(kT, KT, "dve"))))
                    items.append((lambda h=h: load_cols(WS, win_cols(l, OFF_VA + h * 128, 128), 128),
                                  lambda k: proj_tm(WS.aps[k], WS.bufs[k], 128, [0, 1], evac_v(0))))
                    items.append((None, lambda k, h=h: attn_head(h)))

                def k_stage(j, kap, KB, sc):
                    sl = slice(j * 512, (j + 1) * 512)
                    ACT([CUM], [TC], out=tmpC, in_=cum[:, sl], func=AF.Exp, scale=-sc)
                    V("tensor_tensor", [KB, TC], [KT], out=kT[:, sl], in0=kap, in1=tmpC, op=ALU.mult)
                    if len(PEND) >= 2:
                        PEND.pop(0)()
                    V("tensor_tensor", [KT, EL], [KHTB[j % 2]], out=khT2[j % 2].rearrange("p (a b) -> p a b", a=8), in0=kT[:, sl].rearrange("p (a b) -> p a b", a=8),
                      in1=elast[:, 8 * j:8 * j + 8].unsqueeze(2).to_broadcast([128, 8, 64]), op=ALU.mult)
                    def dtr(j=j):
                        pv = psb(7)
                        for q in range(4):
                            transp(pv[:, q * 128:(q + 1) * 128], khT2[j % 2][:, q * 128:(q + 1) * 128], [KHTB[j % 2]], [PB[7]])
                        ACT([PB[7]], [KHAT], out=khat[:, 4 * j:4 * j + 4, :], in_=pv[:, 0:512].rearrange("p (a n) -> p a n", a=4), func=AF.Copy)
                    PEND.append(dtr)

                def gate_tail(j, csrc, CB, sc):
                    sl = slice(j * 512, (j + 1) * 512)
                    V("tensor_tensor_scan", [CB, CST], [CUM], out=cum[:, sl], data0=rmask, data1=csrc, initial=0.0, op0=ALU.mult, op1=ALU.add)
                    ACT([CUM], [EL], out=elast[:, 8 * j:8 * j + 8], in_=cum[:, j * 512 + 63:(j + 1) * 512:64], func=AF.Exp, scale=sc)

                def q_stage(sc, qscale):
                    def f(j, b):
                        sl = slice(j * 512, (j + 1) * 512)
                        ACT([CUM], [TA], out=tmpA, in_=cum[:, sl], func=AF.Exp, scale=sc)
                        V("scalar_tensor_tensor", [PB[b], TA], [QT], out=qT[:, sl], in0=psum[b][:], scalar=qscale, in1=tmpA, op0=ALU.mult, op1=ALU.mult)
                    return f

                def evac_silu(j, b):
                    ACT([PB[b]], [SGT], out=sgT[:, j * 512:(j + 1) * 512], in_=psum[b][:], func=AF.Silu)

                def unit_state(subs, mid=None):
                    flush_pending()
                    V("memset", [], [SBF], Sbf[:, 0, :], 0.0)
                    for g in range(4):
                        ubs = [4 + 2 * (g % 2), 5 + 2 * (g % 2)]
                        for cc in range(8):
                            c = 8 * g + cc
                            t, hf = c // 2, c % 2
                            ub = ubs[hf]
                            for (rb, nr, vb) in subs:
                                mm(psum[ub][rb:rb + nr, (cc // 2) * 128:(cc // 2 + 1) * 128], khat[64 * hf:64 * hf + 64, t, rb:rb + nr], vbuf[vb][64 * hf:64 * hf + 64, t, 0:128],
                                   True, True, [KHAT, VB[vb]], [PB[ub]])
                        if g == 1 and mid is not None:
                            mid()
                        for cc in range(8):
                            c = 8 * g + cc
                            if c == 31:
                                break
                            ub = ubs[c % 2]
                            usrc = psum[ub][:, (cc // 2) * 128:(cc // 2 + 1) * 128]
                            dstS = Sf[:, (c + 1) % 4, :]
                            DB_ = SFB[((c + 1) % 4) // 2]
                            if c == 0:
                                V("tensor_copy", [PB[ub]], [DB_], out=dstS, in_=usrc)
                            else:
                                V("scalar_tensor_tensor", [PB[ub], EL, SFB[(c % 4) // 2]], [DB_], out=dstS, in0=Sf[:, c % 4, :], scalar=elast[:, c:c + 1],
                                  in1=usrc, op0=ALU.mult, op1=ALU.add)
                            ACT([DB_], [SBF], out=Sbf[:, c + 1, :], in_=dstS, func=AF.Copy)
                def unit_out(sub, bi, nwcol):
                    rb, nr, vb = sub
                    sqbuf = [tmpB, tmpC]
                    SQB = [TB, TC]

                    def tail(j):
                        par = j % 2
                        tb = j % 2
                        for q in range(4):
                            transp(psb(tb)[:, q * 128:(q + 1) * 128], onb4[par][:, q, :], [ONB4[par]], [PB[tb]])
                        finish_T(brT[:, bi, j * 512:(j + 1) * 512], BR[bi], nwcol, (sgT[:, j * 512:(j + 1) * 512], SGT), tb)
                    for j in range(4):
                        par = j % 2
                        sbk, obk = (2, 3) if par == 0 else (4, 5)
                        for q in range(4):
                            t = 4 * j + q
                            tsl = slice(t * 128, (t + 1) * 128)
                            mm(psum[sbk][:, q * 128:(q + 1) * 128], kT[rb:rb + nr, tsl], qT[rb:rb + nr, tsl], True, True, [KT, QT], [PB[sbk]])
                        V("tensor_tensor", [PB[sbk], CST], [PTGB], out=PTg4, in0=psum[sbk][:].rearrange("p (a n) -> p a n", a=4),
                          in1=cmask.unsqueeze(1).to_broadcast([128, 4, 128]), op=ALU.mult)
                        if j > 0:
                            tail(j - 1)
                        for q in range(4):
                            t = 4 * j + q
                            csl = slice(q * 128, (q + 1) * 128)
                            mm(psum[obk][:, csl], PTg4[:, q, :], vbuf[vb][:, t, 0:128], True, False, [PTGB, VB[vb]], [PB[obk]])
                            for hf in range(2):
                                c = 2 * t + hf
                                mm(psum[obk][64 * hf:64 * hf + 64, csl], qT[rb:rb + nr, t * 128 + 64 * hf:t * 128 + 64 * hf + 64], Sbf[rb:rb + nr, c, :], False, hf == 1,
                                   [QT, SBF], [PB[obk]])
                        o3 = psum[obk][:].rearrange("p (a n) -> p a n", a=4)
                        s4 = sm4[par]
                        SMp = SMB[par]
                        ACT([PB[obk]], [SQB[par]], out=sqbuf[par], in_=psum[obk][:], func=AF.Square)
                        V("reduce_sum", [SQB[par]], [SMp], out=s4[:, 0:4], in_=sqbuf[par].rearrange("p (a n) -> p a n", a=4), axis=AX.X)
                        V("tensor_scalar", [SMp], [SMp], out=s4[:, 4:8], in0=s4[:, 0:4], scalar1=1.0 / 128, scalar2=EPS, op0=ALU.mult, op1=ALU.add)
                        S.op("pool", "tensor_tensor", [SMp, CST], [SMp], out=s4[:, 12:16], in0=s4[:, 4:8], in1=nhalf.to_broadcast([128, 4]), op=ALU.pow)
                        V("tensor_tensor", [PB[obk], SMp], [ONB4[par]], out=onb4[par], in0=o3, in1=s4[:, 12:16].unsqueeze(2).to_broadcast([128, 4, 128]), op=ALU.mult)
                    tail(3)

                def sg_item(c0):
                    return (lambda: load_cols(WS, win_cols(l, c0, 128), 128),
                            lambda k: proj_fm(WS.aps[k], WS.bufs[k], 128, [0, 1], evac_silu))

                def load_small(k):
                    S.dma("pool", ch_wg, wgb[0:16, :], wg_d[l], writes=[WG])
                items.append((None, load_small))

                def evac_glr(j, b):
                    ACT([PB[b]], [GLR], out=glrT[0:16, j * 512:(j + 1) * 512], in_=psum[b][0:16, :], func=AF.Copy)
                for g in range(2):
                    items.append((lambda: load_cols(WS, win_cols(l, OFF_GLR, 16), 16),
                                  lambda k: proj_fm(WS.aps[k], WS.bufs[k], 16, [0, 1], evac_glr)))
                    def gla_gate(k, g=g):
                        for j in range(4):
                            b = j % 2
                            mm(psum[b][:], wgb[0:16, g * 128:(g + 1) * 128], glrT[0:16, j * 512:(j + 1) * 512], True, True, [WG, GLR], [PB[b]])
                            ACT([PB[b], PRM], [TA], out=tmpA, in_=psum[b][:], func=AF.Exp, bias=prm[:, pb + 4 + g:pb + 5 + g], scale=-1.0)
                            ACT([TA], [TB], out=tmpB, in_=tmpA, func=AF.Ln, bias=1.0, scale=1.0)
                            gate_tail(j, tmpB, TB, -1.0 / 16)
                    items.append((None, gla_gate))
                    items.append((lambda g=g: load_cols(WS, win_cols(l, OFF_KG + g * 128, 128), 128),
                                  lambda k: proj_fm(WS.aps[k], WS.bufs[k], 128, [0, 1], lambda j, b: k_stage(j, psum[b][:], PB[b], -1.0 / 16))))
                    items.append((lambda g=g: load_cols(WS, win_cols(l, OFF_QG + g * 128, 128), 128),
                                  lambda k: proj_fm(WS.aps[k], WS.bufs[k], 128, [0, 1], q_stage(-1.0 / 16, 0.125))))
                    for si in range(2):
                        items.append((lambda g=g, si=si: load_cols(WS, win_cols(l, OFF_VG + (2 * g + si) * 128, 128), 128),
                                      lambda k, si=si: proj_tm(WS.aps[k], WS.bufs[k], 128, [0, 1], evac_v(si))))
                    gsubs = [(0, 64, 0), (64, 64, 1)]
                    items.append((lambda g=g: load_cols(WS, win_cols(l, OFF_GOUT + (2 * g) * 128, 128), 128),
                                  lambda k: unit_state(gsubs, lambda: proj_fm(WS.aps[k], WS.bufs[k], 128, [0, 1], evac_silu))))
                    items.append((None, lambda k, g=g: unit_out(gsubs[0], 4 + 2 * g, prm[:, pb + 2:pb + 3])))
                    items.append(sg_item(OFF_GOUT + (2 * g + 1) * 128))
                    items.append((None, lambda k, g=g: unit_out(gsubs[1], 4 + 2 * g + 1, prm[:, pb + 2:pb + 3])))
                for h in range(4):
                    def hg_gate(j, b, h=h):
                        sl = slice(j * 512, (j + 1) * 512)
                        oml = prm[:, pb + 6 + h:pb + 7 + h]
                        ACT([PB[b]], [TA], out=tmpA, in_=psum[b][:], func=AF.Sigmoid)
                        V("tensor_scalar", [TA, PRM], [TB], out=tmpB, in0=tmpA, scalar1=oml, scalar2=prm[:, pb + 10 + h:pb + 11 + h], op0=ALU.mult, op1=ALU.add)
                        ACT([TB], [CUM], out=cum[:, sl], in_=tmpB, func=AF.Ln)
                        ACT([PB[b]], [TC], out=tmpC, in_=psum[b][:], func=AF.Sigmoid, scale=-1.0)
                        V("tensor_scalar", [TC, PRM], [KT], out=kT[:, sl], in0=tmpC, scalar1=oml, scalar2=None, op0=ALU.mult)

                    def hg_item(k, hg_gate=hg_gate):
                        FL["mode"] = "gh1"
                        proj_fm(WS.aps[k], WS.bufs[k], 128, [0, 1], hg_gate)
                        for j in range(4):
                            sl = slice(j * 512, (j + 1) * 512)
                            V("tensor_tensor_scan", [CST], [CUM], out=cum[:, sl], data0=rmask, data1=cum[:, sl], initial=0.0, op0=ALU.mult, op1=ALU.add)
                        ACT([CUM], [EL], out=elast[:, 0:32], in_=cum[:, 63:T:64], func=AF.Exp, scale=1.0)

                        def kchain(j):
                            sl = slice(j * 512, (j + 1) * 512)
                            ACT([CUM], [TC], out=tmpC, in_=cum[:, sl], func=AF.Exp, scale=-1.0)
                            V("tensor_tensor", [TC], [KT], out=kT[:, sl], in0=kT[:, sl], in1=tmpC, op=ALU.mult)
                            V("tensor_tensor", [KT, EL], [KHTB[j % 2]], out=khT2[j % 2].rearrange("p (a b) -> p a b", a=8), in0=kT[:, sl].rearrange("p (a b) -> p a b", a=8),
                              in1=elast[:, 8 * j:8 * j + 8].unsqueeze(2).to_broadcast([128, 8, 64]), op=ALU.mult)

                        def ktr(j):
                            pv = psb(7)
                            for q in range(4):
                                transp(pv[:, q * 128:(q + 1) * 128], khT2[j % 2][:, q * 128:(q + 1) * 128], [KHTB[j % 2]], [PB[7]])
                            ACT([PB[7]], [KHAT], out=khat[:, 4 * j:4 * j + 4, :], in_=pv[:, 0:512].rearrange("p (a n) -> p a n", a=4), func=AF.Copy)
                        PEND.append(lambda: kchain(0))
                        PEND.append(lambda: (kchain(1), ktr(0)))
                        PEND.append(lambda: (kchain(2), ktr(1)))
                        PEND.append(lambda: (kchain(3), ktr(2)))
                        PEND.append(lambda: ktr(3))
                    items.append((lambda h=h: load_cols(WS, win_cols(l, OFF_FH + h * 128, 128), 128), hg_item))
                    items.append((lambda h=h: load_cols(WS, win_cols(l, OFF_QH + h * 128, 128), 128),
                                  lambda k: proj_fm(WS.aps[k], WS.bufs[k], 128, [0, 1], q_stage(1.0, 1.0))))
                    items.append((lambda h=h: load_cols(WS, win_cols(l, OFF_IH + h * 128, 128), 128),
                                  lambda k: proj_tm(WS.aps[k], WS.bufs[k], 128, [0, 1], evac_v(0))))
                    items.append((lambda h=h: load_cols(WS, win_cols(l, OFF_HOUT + h * 128, 128), 128),
                                  lambda k: unit_state([(0, 128, 0)], lambda: proj_fm(WS.aps[k], WS.bufs[k], 128, [0, 1], evac_silu))))
                    items.append((None, lambda k, h=h: unit_out((0, 128, 0), 8 + h, prm[:, pb + 3:pb + 4])))

                if only is not None:
                    items = [it for i, it in enumerate(items) if only(i)]
                run_items(items, 2)
                flush_pending()
                if dbg and s == 0 and l == 0:
                    dump("brT", brT, [12, T], BF16, BR)
                S.barrier()
                if stop_after == "mix":
                    stop = True
                    break

                C = Carver(big, SCR_LO, SBUF_BYTES)
                mergedT = C.alloc([8, T], BF16)
                MG = [Buf("mg%d" % i) for i in range(8)]
                WG1 = Slots(C, 2, [8, 3, 128], "wg1")
                WB1 = Slots(C, 2, [3, 4, 128], "wb1")
                sig = [C.alloc([512], F32) for _ in range(3)]
                SIG = [Buf("sig%d" % i) for i in range(3)]
                acc = C.alloc([512], F32)
                ACC = Buf("acc")

                def t1_load(c):
                    def f():
                        k = WG1.next()
                        for n in range(3):
                            S.dma("pool", WG1.chs[k], WG1.aps[k][:, :, n, :], win_cols(l, OFF_MG + n * 1024 + c * 128, 128), writes=[WG1.bufs[k]])
                        k2 = WB1.next()
                        for n in range(3):
                            S.dma("pool", WB1.chs[k2], WB1.aps[k2][:, n, :, :], wbr_d[l, n][:, c * 128:(c + 1) * 128].rearrange("(k p) n -> p k n", p=128), writes=[WB1.bufs[k2]])
                        return (k, k2)
                    return f

                def t1_comp(c):
                    def f(kk):
                        k, k2 = kk
                        for j in range(4):
                            sl = slice(j * 512, (j + 1) * 512)
                            for n in range(3):
                                for kc in range(8):
                                    mm(psum[n][:], WG1.aps[k][:, kc, n, :], xT[:, kc, sl], kc == 0, kc == 7, [WG1.bufs[k]] + XT[4 * j:4 * j + 4], [PB[n]])
                                ACT([PB[n]], [SIG[n]], out=sig[n], in_=psum[n][:], func=AF.Sigmoid)
                            for n in range(3):
                                for kc in range(4):
                                    mm(psum[3 + n][:], WB1.aps[k2][:, n, kc, :], brT[:, 4 * n + kc, sl], kc == 0, kc == 3, [WB1.bufs[k2], BR[4 * n + kc]], [PB[3 + n]])
                            V("tensor_tensor", [PB[3], SIG[0]], [ACC], out=acc, in0=psum[3][:], in1=sig[0], op=ALU.mult)
                            V("tensor_tensor", [PB[4]], [SIG[1]], out=sig[1], in0=psum[4][:], in1=sig[1], op=ALU.mult)
                            V("tensor_tensor", [PB[5]], [SIG[2]], out=sig[2], in0=psum[5][:], in1=sig[2], op=ALU.mult)
                            V("tensor_tensor", [SIG[1]], [ACC], out=acc, in0=acc, in1=sig[1], op=ALU.add)
                            V("tensor_tensor", [ACC, SIG[2]], [MG[c]], out=mergedT[:, c, sl], in0=acc, in1=sig[2], op=ALU.add)
                    return f
                run_items([(t1_load(c), t1_comp(c)) for c in range(8)], 2)
                S.barrier()
                C2 = Carver(big, SCR_LO - 12 * T * 2, SCR_LO)
                woT = C2.alloc([8, D], BF16)
                lnw = C2.alloc([D], F32)
                lnb = C2.alloc([D], F32)
                st6 = C2.alloc([16, 2, 6], F32)
                mvs = [C2.alloc([8], F32) for _ in range(2)]
                xbs = [C2.alloc([D], BF16) for _ in range(2)]
                WO, LNP = Buf("wo"), Buf("lnp")
                LNBs = [Buf("lnb0"), Buf("lnb1")]
                ch_wo = S.chan("wo_%d" % S.uid())
                for hq in range(2):
                    S.dma("pool", ch_wo, woT[:, 4 * hq:4 * hq + 4, :], wout_d[l][512 * hq:512 * hq + 512, :].rearrange("(k p) n -> p k n", p=128), writes=[WO])
                S.dma("sp", ch_p, lnw, ln1w_d[l].partition_broadcast(128), writes=[LNP])
                S.dma("sp", ch_p, lnb, ln1b_d[l].partition_broadcast(128), writes=[LNP])
                def fa(t_):
                    ln_finish_a(t_, st6[:, t_, :, :], lnw, lnb, LNP, mvs[t_ % 2], xbs[t_ % 2], LNBs[t_ % 2])

                def fb(t_):
                    ln_finish_b(t_, 2 + (t_ % 2), xbs[t_ % 2], LNBs[t_ % 2])
                for tt in range(18):
                    if tt < 16:
                        bks = [(4 * (tt % 2)) + 0, (4 * (tt % 2)) + 1]
                        for hf in range(2):
                            for kc in range(8):
                                mm(psum[bks[hf]][:], mergedT[:, kc, tt * 128:(tt + 1) * 128], woT[:, kc, hf * 512:(hf + 1) * 512], kc == 0, kc == 7, [MG[kc], WO], [PB[bks[hf]]])
                            ln_pre(tt, hf, bks[hf], st6[:, tt, :, :])
                    if 0 <= tt - 2:
                        fb(tt - 2)
                    if 0 <= tt - 1 < 16:
                        fa(tt - 1)
                if dbg and s == 0 and l == 0:
                    dump("x1", xres, [16, D], F32, XR)
                S.barrier()
                if stop_after == "t1":
                    stop = True
                    break

                C = Carver(big, SCR_LO - 12 * T * 2, SBUF_BYTES)
                hT = C.alloc([32, 1024], BF16)
                HT = [Buf("hT%d" % i) for i in range(32)]
                WU = Slots(C, 3, [8, 128], "wu")
                WD = Slots(C, 3, [512], "wd")
                lnw = C.alloc([D], F32)
                lnb = C.alloc([D], F32)
                st6h = [C.alloc([8, 2, 6], F32) for _ in range(2)]
                LNQ = []
                mvs = [C.alloc([8], F32) for _ in range(2)]
                xbs = [C.alloc([D], BF16) for _ in range(2)]
                tmpR = [C.alloc([512], F32) for _ in range(4)]
                TR = [Buf("tr%d" % i) for i in range(4)]
                LNP = Buf("lnp2")
                LNBs = [Buf("lnb20"), Buf("lnb21")]
                S.dma("sp", ch_p, lnw, ln2w_d[l].partition_broadcast(128), writes=[LNP])
                S.dma("sp", ch_p, lnb, ln2b_d[l].partition_broadcast(128), writes=[LNP])
                for half in range(2):
                    t0 = half * 1024
                    its = []
                    for f in range(32):
                        def ldu(f=f):
                            return load_cols(WU, wup_d[l][:, f * 128:(f + 1) * 128].rearrange("(k p) n -> p k n", p=128), 128)

                        def cpu(k, f=f):
                            for jj in range(2):
                                b = jj
                                tsl = slice(t0 + jj * 512, t0 + (jj + 1) * 512)
                                for kc in range(8):
                                    mm(psum[b][:], WU.aps[k][:, kc, :], xT[:, kc, tsl], kc == 0, kc == 7,
                                       [WU.bufs[k]] + XT[(t0 + jj * 512) // 128:(t0 + jj * 512) // 128 + 4], [PB[b]])
                                ri = (2 * f + jj) % 4
                                ACT([PB[b]], [TR[ri]], out=tmpR[ri], in_=psum[b][:], func=AF.Relu)
                                V("tensor_tensor", [TR[ri]], [HT[f]], out=hT[:, f, jj * 512:(jj + 1) * 512], in0=tmpR[ri], in1=tmpR[ri], op=ALU.mult)
                            if f % 2 == 1 and LNQ:
                                LNQ.pop(0)()
                        its.append((ldu, cpu))
                    run_items(its, 2)
                    for chh in range(2):
                        its = []
                        for f in range(32):
                            def ldd(f=f, chh=chh):
                                k = WD.next()
                                S.dma("pool", WD.chs[k], WD.aps[k], wdn_d[l][f * 128:(f + 1) * 128, chh * 512:(chh + 1) * 512], writes=[WD.bufs[k]])
                                return k

                            def cpd(k, f=f):
                                for q in range(8):
                                    mm(psum[q][:], hT[:, f, q * 128:(q + 1) * 128], WD.aps[k], f == 0, f == 31, [HT[f], WD.bufs[k]], [PB[q]])
                            its.append((ldd, cpd))
                        run_items(its, 2)
                        for q in range(8):
                            ln_pre(half * 8 + q, chh, q, st6h[half][:, q, :, :], True, False)
                        for q in range(8):
                            ln_pre(half * 8 + q, chh, q, st6h[half][:, q, :, :], False, True)
                    for q in range(8):
                        tt = half * 8 + q

                        def lnfa(tt=tt, q=q, half=half):
                            ln_finish_a(tt, st6h[half][:, q, :, :], lnw, lnb, LNP, mvs[q % 2], xbs[q % 2], LNBs[q % 2])
                            if l == n_layers - 1 and stop_after is None:
                                out_toks.append(S.dma("sp", ch_o[tt % 2], out_d[s, tt * 128:(tt + 1) * 128, :], xres[:, tt, :], reads=[XR[tt]]))

                        def lnfb(tt=tt, q=q):
                            ln_finish_b(tt, 2 + q % 6, xbs[q % 2], LNBs[q % 2])
                        if q == 0:
                            LNQ.append(lnfa)
                        else:
                            prevb = LNQ.pop()
                            LNQ.append(lnfa)
                            LNQ.append(prevb)
                        LNQ.append(lnfb)
                while LNQ:
                    LNQ.pop(0)()
                if dbg and s == 0 and l == 0:
                    dump("x2", xres, [16, D], F32, XR)
                S.barrier()
            if stop:
                break
            S.barrier()
        S.final_wait("sp", out_toks)
        S.emit()
    return nc, list(dbg_d.keys())


_CACHE = {}


def kernel(**inputs):
    n = 8
    x = np.ascontiguousarray(inputs["x"], dtype=np.float32)
    if "nc" not in _CACHE:
        _CACHE["nc"] = build()[0]
    nc = _CACHE["nc"]
    consts = make_consts()
    names = ["w_in", "gla_w_gate", "gla_b_gate", "diff_lambda", "diff_norm_w", "gla_norm_w", "hgrn_norm_w", "hgrn_lb",
             "w_branch", "w_out", "ln1_w", "ln1_b", "w_up", "w_down", "ln2_w", "ln2_b"]
    shared = {k: np.ascontiguousarray(inputs[k], dtype=np.float32) for k in names}
    in_maps = []
    for c in range(n):
        m = dict(shared)
        m["x"] = x[NSEQ * c:NSEQ * (c + 1)]
        m["consts"] = consts
        in_maps.append(m)
    res = run_bass_kernel_spmd(nc, in_maps, core_ids=list(range(n)))
    return np.concatenate([r["out"] for r in res.results], axis=0).astype(np.float32)
```
